# Optimizing a Trainium2 kernel written in Bass

```python
import jax
import jax.numpy as jnp
from jax import lax
import numpy as np

D_MODEL = 1024
BATCH = 16
SEQ = 2048
DEPTH = 4

GRID_W = 64
CTX_LEN = 256
N_MIXERS = 3
N_ATTN_LAYERS = (DEPTH + 2) // 3
N_CONV_LAYERS = (DEPTH + 1) // 3
N_NAT_LAYERS = DEPTH // 3

A_HEAD_DIM = 64
A_HEADS = D_MODEL // A_HEAD_DIM
A_KV_HEADS = A_HEADS // 4
A_GROUP = A_HEADS // A_KV_HEADS
A_Q = A_HEADS * A_HEAD_DIM
A_KV = A_KV_HEADS * A_HEAD_DIM
A_WINDOW = 128
A_BLOCK = 128
ROPE_BASE = 10000.0
CONV_WIDTH = 3
NA_HEAD_DIM = 64
NA_HEADS = D_MODEL // NA_HEAD_DIM
NA_KH_MAX = 8
NA_KW = 16
N_GROUPS = 4
EXPERTS_PER_GROUP = 8
N_EXPERTS = N_GROUPS * EXPERTS_PER_GROUP
TOP_K = 2
EXPERT_FF = D_MODEL // 2
MOE_BLOCK = 128
ALPHA = (2 * DEPTH) ** 0.25
BETA = (8 * DEPTH) ** -0.25
LN_EPS = 1e-5
NEG_INF = -1e30

kernel_name = 'hybrid_dit_window_conv_nat_hmoe'


def layer_norm(x, g, b):
    xf = x.astype(jnp.float32)
    mu = jnp.mean(xf, axis=-1, keepdims=True)
    var = jnp.mean(jnp.square(xf - mu), axis=-1, keepdims=True)
    return ((xf - mu) * lax.rsqrt(var + LN_EPS) * g + b).astype(x.dtype)


def modulate(x, shift, scale):
    return x * (1.0 + scale) + shift


def axial_rope(x, rows, cols):
    half = x.shape[-1] // 2
    quarter = half // 2
    inv_freq = ROPE_BASE ** (-jnp.arange(quarter, dtype=jnp.float32) / quarter)

    def rotate(xp, pos):
        ang = pos.astype(jnp.float32)[:, None] * inv_freq
        cos = jnp.cos(ang)[None, :, None, :]
        sin = jnp.sin(ang)[None, :, None, :]
        x1 = xp[..., :quarter].astype(jnp.float32)
        x2 = xp[..., quarter:].astype(jnp.float32)
        return jnp.concatenate([x1 * cos - x2 * sin, x1 * sin + x2 * cos], axis=-1)

    out = jnp.concatenate([rotate(x[..., :half], rows), rotate(x[..., half:], cols)], axis=-1)
    return out.astype(x.dtype)


def sink_softmax(s, sink):
    sk = jnp.broadcast_to(sink, s.shape[:-1] + (1,))
    p = jax.nn.softmax(jnp.concatenate([s, sk], axis=-1), axis=-1)
    return p[..., :-1]


def windowed_gqa(h_lat, h_ctx, w_qkv, w_o, sink, with_ctx_out):
    B, S, _ = h_lat.shape
    L = h_ctx.shape[1]
    nb = S // A_BLOCK
    span = 3 * A_BLOCK
    scale = A_HEAD_DIM ** -0.5

    def split_qkv(h):
        qkv = h @ w_qkv
        n = h.shape[1]
        q = qkv[..., :A_Q].reshape(B, n, A_HEADS, A_HEAD_DIM)
        k = qkv[..., A_Q:A_Q + A_KV].reshape(B, n, A_KV_HEADS, A_HEAD_DIM)
        v = qkv[..., A_Q + A_KV:].reshape(B, n, A_KV_HEADS, A_HEAD_DIM)
        return q, k, v

    q, k, v = split_qkv(h_lat)
    qc, kc, vc = split_qkv(h_ctx)
    t = jnp.arange(S)
    q = axial_rope(q, t // GRID_W, t % GRID_W)
    k = axial_rope(k, t // GRID_W, t % GRID_W)
    sink_f = sink.astype(jnp.float32).reshape(A_KV_HEADS, A_GROUP, 1, 1)

    pad = ((0, 0), (A_BLOCK, A_BLOCK), (0, 0), (0, 0))
    kp = jnp.pad(k, pad)
    vp = jnp.pad(v, pad)
    qb = q.reshape(B, nb, A_BLOCK, A_KV_HEADS, A_GROUP, A_HEAD_DIM).transpose(1, 0, 2, 3, 4, 5)
    rel = jnp.arange(span)[None, :] - A_BLOCK - jnp.arange(A_BLOCK)[:, None]
    band = jnp.abs(rel) <= A_WINDOW
    ctx_ok = jnp.ones((A_BLOCK, L), dtype=bool)

    def block(args):
        b, qi = args
        kb = lax.dynamic_slice_in_dim(kp, b * A_BLOCK, span, axis=1)
        vb = lax.dynamic_slice_in_dim(vp, b * A_BLOCK, span, axis=1)
        kpos = b * A_BLOCK - A_BLOCK + jnp.arange(span)
        valid = band & ((kpos >= 0) & (kpos < S))[None, :]
        mask = jnp.concatenate([valid, ctx_ok], axis=1)
        k_all = jnp.concatenate([kb, kc], axis=1)
        v_all = jnp.concatenate([vb, vc], axis=1)
        s = jnp.einsum('bqkgd,bskd->bkgqs', qi, k_all).astype(jnp.float32) * scale
        p = sink_softmax(jnp.where(mask, s, NEG_INF), sink_f)
        return jnp.einsum('bkgqs,bskd->bqkgd', p.astype(v_all.dtype), v_all)

    o = lax.map(block, (jnp.arange(nb), qb))
    o_lat = o.transpose(1, 0, 2, 3, 4, 5).reshape(B, S, A_Q) @ w_o
    if not with_ctx_out:
        return o_lat, None
    qcg = qc.reshape(B, L, A_KV_HEADS, A_GROUP, A_HEAD_DIM)
    s = jnp.einsum('blkgd,bmkd->bkglm', qcg, kc).astype(jnp.float32) * scale
    p = sink_softmax(s, sink_f)
    o_ctx = jnp.einsum('bkglm,bmkd->blkgd', p.astype(vc.dtype), vc).reshape(B, L, A_Q) @ w_o
    return o_lat, o_ctx


def depthwise_conv(u, w):
    return lax.conv_general_dilated(
        u, w[:, None, :].astype(u.dtype), window_strides=(1,),
        padding=[(CONV_WIDTH // 2, CONV_WIDTH // 2)],
        dimension_numbers=('NWC', 'WIO', 'NWC'), feature_group_count=u.shape[-1])


def gated_short_conv(h_lat, h_ctx, w_in, conv_w, w_out, with_ctx_out):
    def run(h):
        gb, gc, u = jnp.split(h @ w_in, 3, axis=-1)
        return (gb * depthwise_conv(gc * u, conv_w)) @ w_out
    return run(h_lat), (run(h_ctx) if with_ctx_out else None)


def neighbourhood_attention(h_lat, h_ctx, w_qkv, w_o, rpb, with_ctx_out):
    B, S, _ = h_lat.shape
    L = h_ctx.shape[1]
    rows = S // GRID_W
    kh = min(NA_KH_MAX, rows)
    ncb = GRID_W // NA_KW
    span = 2 * NA_KW
    scale = NA_HEAD_DIM ** -0.5

    def split_qkv(h):
        q, k, v = jnp.split(h @ w_qkv, 3, axis=-1)
        shp = (B, h.shape[1], NA_HEADS, NA_HEAD_DIM)
        return q.reshape(shp), k.reshape(shp), v.reshape(shp)

    q, k, v = split_qkv(h_lat)
    qc, kc, vc = split_qkv(h_ctx)
    grid = (B, rows, GRID_W, NA_HEADS, NA_HEAD_DIM)
    qg = jnp.moveaxis(q.reshape(grid), 1, 0)
    kg = k.reshape(grid)
    vg = v.reshape(grid)

    cb = jnp.arange(ncb)
    key_cols = jnp.clip(cb * NA_KW - NA_KW // 2, 0, GRID_W - span)[:, None] + jnp.arange(span)
    q_cols = cb[:, None] * NA_KW + jnp.arange(NA_KW)
    q_start = jnp.clip(q_cols - NA_KW // 2, 0, GRID_W - NA_KW)
    col_ok = (key_cols[:, None, :] >= q_start[:, :, None]) & (key_cols[:, None, :] < q_start[:, :, None] + NA_KW)
    loc_mask = jnp.broadcast_to(col_ok[:, None, :, None, :], (ncb, 1, NA_KW, kh, span)).reshape(ncb, 1, NA_KW, kh * span)
    dc_idx = jnp.clip(key_cols[:, None, :] - q_cols[:, :, None] + NA_KW - 1, 0, 2 * NA_KW - 2)
    rpb_f = rpb.astype(jnp.float32)

    def row_step(args):
        r, q_row = args
        rs = jnp.clip(r - kh // 2, 0, rows - kh)

        def gather(t):
            t_rows = lax.dynamic_slice_in_dim(t, rs, kh, axis=1)
            t_blk = t_rows[:, :, key_cols]
            return t_blk.transpose(0, 2, 1, 3, 4, 5).reshape(B, ncb, kh * span, NA_HEADS, NA_HEAD_DIM)

        kb = gather(kg)
        vb = gather(vg)
        qb = q_row.reshape(B, ncb, NA_KW, NA_HEADS, NA_HEAD_DIM)
        dr_idx = rs + jnp.arange(kh) - r + NA_KH_MAX - 1
        bias = rpb_f[:, dr_idx][:, :, dc_idx]
        bias = bias.transpose(2, 0, 3, 1, 4).reshape(ncb, NA_HEADS, NA_KW, kh * span)
        s_loc = jnp.einsum('bnqhd,bnkhd->bnhqk', qb, kb).astype(jnp.float32) * scale + bias
        s_ctx = jnp.einsum('bnqhd,bmhd->bnhqm', qb, kc).astype(jnp.float32) * scale
        s = jnp.concatenate([jnp.where(loc_mask, s_loc, NEG_INF), s_ctx], axis=-1)
        p = jax.nn.softmax(s, axis=-1).astype(vb.dtype)
        o = (jnp.einsum('bnhqk,bnkhd->bnqhd', p[..., :kh * span], vb)
             + jnp.einsum('bnhqm,bmhd->bnqhd', p[..., kh * span:], vc))
        return o.reshape(B, GRID_W, NA_HEADS * NA_HEAD_DIM)

    o = lax.map(row_step, (jnp.arange(rows), qg))
    o_lat = jnp.moveaxis(o, 0, 1).reshape(B, S, D_MODEL) @ w_o
    if not with_ctx_out:
        return o_lat, None
    s = jnp.einsum('blhd,bmhd->bhlm', qc, kc).astype(jnp.float32) * scale
    p = jax.nn.softmax(s, axis=-1).astype(vc.dtype)
    o_ctx = jnp.einsum('bhlm,bmhd->blhd', p, vc).reshape(B, L, D_MODEL) @ w_o
    return o_lat, o_ctx


def hier_moe(h, w_group, b_group, w_expert, b_expert, w_gate, w_up, w_down):
    T, D = h.shape
    g_logits = (h @ w_group).astype(jnp.float32) + b_group.astype(jnp.float32)
    g_idx = jnp.argmax(g_logits, axis=-1)
    g_prob = jnp.take_along_axis(jax.nn.softmax(g_logits, axis=-1), g_idx[:, None], axis=-1)
    e_logits = ((h @ w_expert).astype(jnp.float32) + b_expert.astype(jnp.float32)).reshape(T, N_GROUPS, EXPERTS_PER_GROUP)
    e_logits = jnp.take_along_axis(e_logits, g_idx[:, None, None], axis=1)[:, 0]
    top_v, top_i = lax.top_k(e_logits, TOP_K)
    gates = (jax.nn.softmax(top_v, axis=-1) * g_prob).reshape(-1)
    eid = (g_idx[:, None] * EXPERTS_PER_GROUP + top_i).reshape(-1)
    tok = jnp.repeat(jnp.arange(T), TOP_K)
    n_assign = T * TOP_K

    order = jnp.argsort(eid)
    s_eid, s_tok, s_gate = eid[order], tok[order], gates[order]
    counts = jnp.bincount(eid, length=N_EXPERTS)
    pcounts = (counts + MOE_BLOCK - 1) // MOE_BLOCK * MOE_BLOCK
    offs = jnp.cumsum(counts) - counts
    pend = jnp.cumsum(pcounts)
    poffs = pend - pcounts
    dest = poffs[s_eid] + jnp.arange(n_assign) - offs[s_eid]
    n_blocks = -(-n_assign // MOE_BLOCK) + N_EXPERTS
    xs = jnp.zeros((n_blocks * MOE_BLOCK, D), h.dtype).at[dest].set(h[s_tok])
    blk_e = jnp.minimum(jnp.searchsorted(pend, jnp.arange(n_blocks) * MOE_BLOCK, side='right'), N_EXPERTS - 1)

    def expert_block(args):
        xb, e = args
        return (jax.nn.silu(xb @ w_gate[e]) * (xb @ w_up[e])) @ w_down[e]

    ys = lax.map(expert_block, (xs.reshape(n_blocks, MOE_BLOCK, D), blk_e)).reshape(n_blocks * MOE_BLOCK, D)
    return jax.ops.segment_sum(ys[dest] * s_gate[:, None].astype(ys.dtype), s_tok, num_segments=T)


def setup_inputs(seed: int = 0) -> dict:
    key = jax.random.key(seed)
    ks = iter(jax.random.split(key, 32))

    def nrm(shape, scale):
        return scale * jax.random.normal(next(ks), shape, jnp.float32)

    D = D_MODEL
    inv = D ** -0.5
    a_col = jnp.concatenate([jnp.ones((A_Q + A_KV,), jnp.float32), jnp.full((A_KV,), BETA, jnp.float32)])
    n_col = jnp.concatenate([jnp.ones((2 * D,), jnp.float32), jnp.full((D,), BETA, jnp.float32)])
    return {
        'x': nrm((BATCH, SEQ, D), 1.0),
        'c': nrm((BATCH, D), 1.0),
        'ctx': nrm((BATCH, CTX_LEN, D), 1.0),
        'c_ctx': nrm((D,), 1.0),
        'w_ada': nrm((DEPTH, D, 6 * D), 0.5 * inv),
        'b_ada': nrm((DEPTH, 6 * D), 0.02),
        'ln1_g': 1.0 + nrm((DEPTH, D), 0.02),
        'ln1_b': nrm((DEPTH, D), 0.02),
        'ln2_g': 1.0 + nrm((DEPTH, D), 0.02),
        'ln2_b': nrm((DEPTH, D), 0.02),
        'attn_w_qkv': nrm((N_ATTN_LAYERS, D, A_Q + 2 * A_KV), inv) * a_col,
        'attn_w_o': nrm((N_ATTN_LAYERS, A_Q, D), BETA * A_Q ** -0.5),
        'attn_sink': nrm((N_ATTN_LAYERS, A_HEADS), 0.5),
        'conv_w_in': nrm((N_CONV_LAYERS, D, 3 * D), inv),
        'conv_w': nrm((N_CONV_LAYERS, CONV_WIDTH, D), CONV_WIDTH ** -0.5),
        'conv_w_out': nrm((N_CONV_LAYERS, D, D), BETA * inv),
        'nat_w_qkv': nrm((N_NAT_LAYERS, D, 3 * D), inv) * n_col,
        'nat_w_o': nrm((N_NAT_LAYERS, D, D), BETA * inv),
        'nat_rpb': nrm((N_NAT_LAYERS, NA_HEADS, 2 * NA_KH_MAX - 1, 2 * NA_KW - 1), 0.1),
        'router_w_group': nrm((DEPTH, D, N_GROUPS), inv),
        'router_b_group': nrm((DEPTH, N_GROUPS), 0.01),
        'router_w_expert': nrm((DEPTH, D, N_EXPERTS), inv),
        'router_b_expert': nrm((DEPTH, N_EXPERTS), 0.01),
        'expert_w_gate': nrm((DEPTH, N_EXPERTS, D, EXPERT_FF), inv),
        'expert_w_up': nrm((DEPTH, N_EXPERTS, D, EXPERT_FF), inv),
        'expert_w_down': nrm((DEPTH, N_EXPERTS, EXPERT_FF, D), BETA * EXPERT_FF ** -0.5),
    }


def reference(x, c, ctx, c_ctx, w_ada, b_ada, ln1_g, ln1_b, ln2_g, ln2_b,
              attn_w_qkv, attn_w_o, attn_sink, conv_w_in, conv_w, conv_w_out,
              nat_w_qkv, nat_w_o, nat_rpb,
              router_w_group, router_b_group, router_w_expert, router_b_expert,
              expert_w_gate, expert_w_up, expert_w_down):
    B, S, D = x.shape
    xc = ctx
    silu_c = jax.nn.silu(c)
    silu_cc = jax.nn.silu(c_ctx)
    for i in range(DEPTH):
        last = i == DEPTH - 1
        j = i // N_MIXERS
        kind = i % N_MIXERS
        mod_lat = jnp.split((silu_c @ w_ada[i] + b_ada[i])[:, None, :], 6, axis=-1)
        mod_ctx = jnp.split(silu_cc @ w_ada[i] + b_ada[i], 6, axis=-1)

        h_lat = modulate(x, mod_lat[0], mod_lat[1])
        h_ctx = modulate(xc, mod_ctx[0], mod_ctx[1])
        if kind == 0:
            o_lat, o_ctx = windowed_gqa(h_lat, h_ctx, attn_w_qkv[j], attn_w_o[j], attn_sink[j], not last)
        elif kind == 1:
            o_lat, o_ctx = gated_short_conv(h_lat, h_ctx, conv_w_in[j], conv_w[j], conv_w_out[j], not last)
        else:
            o_lat, o_ctx = neighbourhood_attention(h_lat, h_ctx, nat_w_qkv[j], nat_w_o[j], nat_rpb[j], not last)
        x = layer_norm(ALPHA * x + mod_lat[2] * o_lat, ln1_g[i], ln1_b[i])

        h_lat = modulate(x, mod_lat[3], mod_lat[4]).reshape(B * S, D)
        moe_args = (router_w_group[i], router_b_group[i], router_w_expert[i], router_b_expert[i],
                    expert_w_gate[i], expert_w_up[i], expert_w_down[i])
        if last:
            y_lat = hier_moe(h_lat, *moe_args)
        else:
            xc = layer_norm(ALPHA * xc + mod_ctx[2] * o_ctx, ln1_g[i], ln1_b[i])
            h_ctx = modulate(xc, mod_ctx[3], mod_ctx[4]).reshape(-1, D)
            y = hier_moe(jnp.concatenate([h_lat, h_ctx], axis=0), *moe_args)
            y_lat = y[:B * S]
            xc = layer_norm(ALPHA * xc + mod_ctx[5] * y[B * S:].reshape(xc.shape), ln2_g[i], ln2_b[i])
        x = layer_norm(ALPHA * x + mod_lat[5] * y_lat.reshape(B, S, D), ln2_g[i], ln2_b[i])
    return x
```

```python
import numpy as np
from contextlib import ExitStack
import ml_dtypes
import concourse.bass as bass
import concourse.mybir as mybir
from concourse.bass_utils import run_bass_kernel_spmd

F32 = mybir.dt.float32
BF16 = mybir.dt.bfloat16
I32 = mybir.dt.int32
AF = mybir.ActivationFunctionType
ALU = mybir.AluOpType
AX = mybir.AxisListType.X

NCORES = 8
D = 1024
SEQ = 2048
LCTX = 256
DEPTH = 4
NB = 2
TPB = 18
NT = NB * TPB
NEXP = 32
FF = 512
CAP = 1024
NSLOT = NEXP * CAP
ALPHA = (2 * DEPTH) ** 0.25
INV_ALPHA = 1.0 / ALPHA
EPS2 = 1e-5 / (ALPHA * ALPHA)
BIG = 1.0e30
ENGS = ("pe", "act", "dve", "pool", "sp")
import os as _os
KDBG = int(_os.environ.get("KDBG", "0"))


class _Op:
    __slots__ = ("idx", "eng", "fn", "is_dma", "key", "dma_val", "signal", "milestone", "waits", "group")

    def __init__(self, idx, eng, fn, is_dma, key):
        self.group = None
        self.idx = idx
        self.eng = eng
        self.fn = fn
        self.is_dma = is_dma
        self.key = key
        self.dma_val = 0
        self.signal = False
        self.milestone = 0
        self.waits = []


class Sched:
    def __init__(self, nc, es):
        self.nc = nc
        self.es = es
        self.esem = {e: es.enter_context(nc.semaphore("s_" + e)) for e in ENGS}
        self.ksem = {}
        self.key_count = {}
        self.eng_base = {e: 0 for e in ENGS}
        self._reset()

    def _reset(self):
        self.ops = []
        self.per_eng = {e: [] for e in ENGS}
        self.last_w = {}
        self.readers = {}
        self.seen = {e: {f: -1 for f in ENGS} for e in ENGS}
        self.seen_dma = {e: {} for e in ENGS}
        self.eng_group = {e: None for e in ENGS}
        self.seen_saved = {e: None for e in ENGS}

    cur_group = None
    cnt_ap = None

    def _add(self, eng, fn, reads, writes, is_dma, key, group=None):
        o = _Op(len(self.ops), eng, fn, is_dma, key)
        o.group = group
        if group != self.eng_group[eng]:
            if self.eng_group[eng] is not None:
                self.seen[eng], self.seen_dma[eng] = self.seen_saved[eng]
            if group is not None:
                self.seen_saved[eng] = (dict(self.seen[eng]), dict(self.seen_dma[eng]))
            self.eng_group[eng] = group
        deps = set()
        for b in reads:
            w = self.last_w.get(b)
            if w is not None:
                deps.add(w)
        for b in writes:
            w = self.last_w.get(b)
            if w is not None:
                deps.add(w)
            for r in self.readers.get(b, ()):
                deps.add(r)
        for b in reads:
            self.readers.setdefault(b, []).append(o.idx)
        for b in writes:
            self.last_w[b] = o.idx
            self.readers[b] = []
        for di in sorted(deps):
            d = self.ops[di]
            if d.is_dma:
                if self.seen_dma[eng].get(d.key, 0) >= d.dma_val:
                    continue
                self.seen_dma[eng][d.key] = d.dma_val
                o.waits.append(("dma", d.key, d.dma_val))
            else:
                if d.eng == eng and not is_dma and eng == "pe":
                    continue
                if self.seen[eng][d.eng] >= d.idx:
                    continue
                self.seen[eng][d.eng] = d.idx
                d.signal = True
                o.waits.append(("eng", d.eng, d.idx))
        if is_dma:
            if key not in self.ksem:
                self.ksem[key] = self.es.enter_context(self.nc.semaphore("k%d" % len(self.ksem)))
                self.key_count[key] = 0
            self.key_count[key] += 16
            o.dma_val = self.key_count[key]
        self.ops.append(o)
        self.per_eng[eng].append(o)
        return o

    _cap = None

    def op(self, eng, fn, reads=(), writes=()):
        if self._cap is not None:
            self._cap.append((eng, fn, tuple(reads), tuple(writes), False, None, self.cur_group))
            return None
        return self._add(eng, fn, tuple(reads), tuple(writes), False, None, self.cur_group)

    def dma(self, eng, fn, key, reads=(), writes=()):
        if self._cap is not None:
            self._cap.append((eng, fn, tuple(reads), tuple(writes), True, key, self.cur_group))
            return None
        return self._add(eng, fn, tuple(reads), tuple(writes), True, key, self.cur_group)

    def capture(self, f, *a, **kw):
        assert self._cap is None
        self._cap = []
        try:
            r = f(*a, **kw)
        finally:
            c = self._cap
            self._cap = None
        return c, r

    def replay(self, items):
        for it in items:
            self._add(*it)

    limit = None
    nflush = 0

    def flush(self):
        self.nflush += 1
        if self.limit is not None and self.nflush > self.limit:
            for o in self.ops:
                if o.is_dma:
                    self.key_count[o.key] -= 16
            self._reset()
            return
        nc = self.nc
        ops = self.ops
        final = {}
        self.eng_base_prev = dict(self.eng_base)
        for e in ENGS:
            lst = [o for o in self.per_eng[e] if not o.is_dma]
            if e != "sp" and lst:
                lst[-1].signal = True
            n = self.eng_base[e]
            for o in self.per_eng[e]:
                if o.signal and not o.is_dma:
                    n += 1
                    o.milestone = n
            self.eng_base[e] = n
            final[e] = n
        esem, ksem = self.esem, self.ksem
        keyvals = dict(self.key_count)
        per_eng = self.per_eng

        regs = self.__dict__.setdefault("_regs", {})

        def emit_one(ename, eng, o):
            for w in o.waits:
                if w[0] == "dma":
                    eng.wait_ge(ksem[w[1]], w[2])
                else:
                    eng.wait_ge(esem[w[1]], ops[w[2]].milestone)
            inst = o.fn(eng)
            if o.is_dma:
                inst.then_inc(ksem[o.key], 16)
            elif o.signal:
                inst.then_inc(esem[ename], 1)

        def emit(ename, eng):
            lst = per_eng[ename]
            ms = self.eng_base_prev[ename]
            k = 0
            while k < len(lst):
                o = lst[k]
                if o.group is None:
                    emit_one(ename, eng, o)
                    if o.signal and not o.is_dma:
                        ms = o.milestone
                    k += 1
                    continue
                k2 = k
                while k2 < len(lst) and lst[k2].group == o.group:
                    k2 += 1
                run = lst[k:k2]
                if ename not in regs:
                    regs[ename] = eng.alloc_register("cnt_" + ename)
                r = regs[ename]
                eng.reg_load(r, self.cnt_ap(o.group[0]))
                nsig = sum(1 for x in run if x.signal and not x.is_dma)
                with eng.If_lt(r, o.group[1]):
                    if nsig:
                        if ms > 0:
                            eng.wait_ge(esem[ename], ms)
                        eng.sem_inc(esem[ename], nsig)
                    for x in run:
                        if x.is_dma:
                            if x.dma_val > 16:
                                eng.wait_ge(ksem[x.key], x.dma_val - 16)
                            eng.sem_inc(ksem[x.key], 16)
                with eng.Else():
                    for x in run:
                        emit_one(ename, eng, x)
                ms += nsig
                k = k2
            for kk, v in keyvals.items():
                if v > 0:
                    eng.wait_ge(ksem[kk], v)
            for f in ENGS:
                if f != ename and f != "sp" and final[f] > 0:
                    eng.wait_ge(esem[f], final[f])

        with nc.Block() as block:
            @block.tensor
            def _(eng):
                emit("pe", eng)

            @block.scalar
            def _(eng):
                emit("act", eng)

            @block.vector
            def _(eng):
                emit("dve", eng)

            @block.gpsimd
            def _(eng):
                emit("pool", eng)

            @block.sync
            def _(eng):
                emit("sp", eng)
        self._reset()


def _rope_tables():
    p = np.arange(128)
    j = p % 16
    inv_freq = (10000.0 ** (-(j.astype(np.float64)) / 16.0))
    t = np.arange(SEQ)
    rows = t // 64
    cols = t % 64
    pos = np.where(((p % 64) < 32)[:, None], rows[None, :], cols[None, :]).astype(np.float64)
    ang = pos * inv_freq[:, None]
    ang = (pos.astype(np.float32) * inv_freq.astype(np.float32)[:, None]).astype(np.float32)
    cosT = np.cos(ang).astype(np.float32)
    sinT = np.sin(ang).astype(np.float32)
    sign = np.where((p % 32) < 16, -1.0, 1.0).astype(np.float32)[:, None]
    sinT = sinT * sign
    perm = np.where((p % 32) < 16, p + 16, p - 16)
    Pm = np.zeros((128, 128), np.float32)
    Pm[perm, p] = 1.0
    return cosT, sinT, Pm


def _band_masks():
    k = np.arange(128)[:, None]
    q = np.arange(128)[None, :]
    prev = (k >= q).astype(np.float32)
    nxt = (k <= q).astype(np.float32)
    return np.stack([prev, nxt], 0)


def _nat_patterns():
    pats = {}
    plist = []
    chunks = {}
    for qt in range(16):
        r0, r1 = 2 * qt, 2 * qt + 1
        rs0 = min(max(r0 - 4, 0), 24)
        rs1 = min(max(r1 - 4, 0), 24)
        lo = rs0 // 2
        hi = (rs1 + 7) // 2
        chunks[qt] = list(range(lo, hi + 1))
        for kc in chunks[qt]:
            interior = 2 <= qt <= 13
            key = ("i", kc - qt) if interior else ("b", qt, kc)
            if key not in pats:
                pats[key] = len(plist)
                plist.append((qt, kc))
            pats[(qt, kc)] = pats[key]
    npat = len(plist)
    dr = np.zeros((npat, 128, 128), np.int64)
    dc = np.zeros((npat, 128, 128), np.int64)
    mk = np.zeros((npat, 128, 128), np.float32)
    kk = np.arange(128)
    for pi, (qt, kc) in enumerate(plist):
        kr = 2 * kc + kk // 64
        kcol = kk % 64
        qr = 2 * qt + kk // 64
        qcol = kk % 64
        rs = np.clip(qr - 4, 0, 24)
        qs = np.clip(qcol - 8, 0, 48)
        vrow = (kr[:, None] >= rs[None, :]) & (kr[:, None] < rs[None, :] + 8)
        vcol = (kcol[:, None] >= qs[None, :]) & (kcol[:, None] < qs[None, :] + 16)
        mk[pi] = (vrow & vcol).astype(np.float32)
        dr[pi] = np.clip(kr[:, None] - qr[None, :] + 7, 0, 14)
        dc[pi] = np.clip(kcol[:, None] - qcol[None, :] + 15, 0, 30)
    return pats, chunks, dr, dc, mk, npat


_NATP = _nat_patterns()
NPAT = _NATP[5]


def build_program(limit=None, debug=False):
    nc = bass.Bass("TRN2", target_bir_lowering=False)

    _uid = [0]

    def un(name):
        _uid[0] += 1
        return "%s_u%d" % (name, _uid[0])

    def din(name, shape, dt=F32):
        return nc.dram_tensor(name, list(shape), dt, kind="ExternalInput").ap()

    x_in = din("x", [NB, SEQ, D])
    ctx_in = din("ctx", [NB, LCTX, D])
    c_in = din("c", [NB, D])
    cctx_in = din("c_ctx", [1, D])
    w_ada = din("w_ada", [DEPTH, D, 6 * D])
    b_ada = din("b_ada", [DEPTH, 6 * D])
    ln1_g = din("ln1_g", [DEPTH, D])
    ln1_b = din("ln1_b", [DEPTH, D])
    ln2_g = din("ln2_g", [DEPTH, D])
    ln2_b = din("ln2_b", [DEPTH, D])
    attn_wqk = din("attn_wqk", [2, D, 1536])
    attn_wv = din("attn_wv", [2, D, 256])
    attn_wo = din("attn_w_o", [2, D, D])
    attn_sink = din("attn_sink", [2, 16])
    conv_w_in = din("conv_w_in", [1, D, 3 * D])
    conv_w = din("conv_w", [1, 3, D])
    conv_w_out = din("conv_w_out", [1, D, D])
    nat_wqk = din("nat_wqk", [1, D, 2048])
    nat_wv = din("nat_wv", [1, D, 1024])
    nat_wo = din("nat_w_o", [1, D, D])
    nat_bias = din("nat_bias", [NPAT, 128, 16, 128])
    router_w = din("router_w", [DEPTH, D, 36])
    router_b = din("router_b", [DEPTH, 36])
    w_gate = din("expert_w_gate", [DEPTH, NEXP, D, FF])
    w_up = din("expert_w_up", [DEPTH, NEXP, D, FF])
    w_down = din("expert_w_down", [DEPTH, NEXP, FF, D])
    k_ident = din("k_ident", [128, 128])
    k_cos = din("k_cos", [128, SEQ])
    k_sin = din("k_sin", [128, SEQ])
    k_pm = din("k_pm", [128, 128])
    k_band = din("k_band", [2, 128, 128])
    k_natmask = din("k_natmask", [NPAT, 128, 128])
    k_ustrict = din("k_ustrict", [128, 128])
    k_eoff = din("k_eoff", [1, 32])

    out = nc.dram_tensor("out", [NB, SEQ, D], F32, kind="ExternalOutput").ap()

    X = nc.dram_tensor("X_scr", [NT * 128, D], F32).ap()
    MODR = nc.dram_tensor("MODR_scr", [DEPTH, 3, 6, D], F32).ap()
    XS = nc.dram_tensor("XS_scr", [NSLOT, D], BF16).ap()
    YS = nc.dram_tensor("YS_scr", [NSLOT, D], F32).ap()
    ETAB = nc.dram_tensor("ETAB_scr", [NPAT, 128, 16, 128], BF16).ap()
    QT = nc.dram_tensor("QT_scr", [128, 8, 2304], BF16).ap()

    DBG = {}
    if debug:
        DBG["T"] = nc.dram_tensor("dbgT", [4, 128, D], F32, kind="ExternalOutput").ap()
        DBG["O"] = nc.dram_tensor("dbgO", [128, D], BF16, kind="ExternalOutput").ap()
        DBG["Den"] = nc.dram_tensor("dbgDen", [128, 16], F32, kind="ExternalOutput").ap()
        DBG["H2"] = nc.dram_tensor("dbgH2", [128, D], BF16, kind="ExternalOutput").ap()
        DBG["RT"] = nc.dram_tensor("dbgRT", [128, 256], F32, kind="ExternalOutput").ap()
        DBG["tile"] = 0
        DBG["OA"] = nc.dram_tensor("dbgOA", [128, 3, 512], F32, kind="ExternalOutput").ap()
        DBG["SK"] = nc.dram_tensor("dbgSK", [128, 16], F32, kind="ExternalOutput").ap()

    def dbg_store(key, idx, ap, bid, tt):
        if not DBG or tt != DBG["tile"] or DBG.get("done_" + key + str(idx)):
            return
        DBG["done_" + key + str(idx)] = True
        dst = DBG[key][idx] if idx is not None else DBG[key]
        S.dma("sp", lambda e: e.dma_start(out=dst, in_=ap), "dbg" + key + str(idx), reads=[bid])

    with ExitStack() as ges:
        S = Sched(nc, ges)
        S.limit = limit

        def gsb(name, shape, dt):
            return ges.enter_context(nc.sbuf_tensor(un(name), list(shape), dt))

        identf = gsb("identf", [128, 128], F32)
        identb = gsb("identb", [128, 128], BF16)
        ustrict = gsb("ustrict", [128, 128], BF16)
        onesb = gsb("onesb", [128, 128], BF16)
        zerob = gsb("zerob", [128, 512], BF16)
        ones13 = gsb("ones13", [1, 4], F32)
        csT = gsb("csT", [128, 8, 3], F32)
        eoff = gsb("eoff", [128, 32], F32)
        destI = gsb("destI", [128, NT, 2], I32)
        gates = gsb("gates", [128, NT, 2], F32)
        cnt = gsb("cnt", [128, 32], F32)
        cnti = gsb("cnti", [128, 32], I32)
        S.cnt_ap = lambda idx: cnti[0:1, idx:idx + 1]

        _preg = {}

        def breg(e):
            if "r" not in _preg:
                _preg["r"] = e.to_reg(NSLOT - 1)
            return _preg["r"]

        def tile_src(layer, tt):
            b, j = divmod(tt, TPB)
            if layer == 0:
                if j < 16:
                    return x_in[b, j * 128:(j + 1) * 128, :]
                return ctx_in[b, (j - 16) * 128:(j - 15) * 128, :]
            return X[tt * 128:(tt + 1) * 128, :]

        def phase_init():
            with ExitStack() as es:
                tmpf = es.enter_context(nc.sbuf_tensor(un("init_tmp"), [128, 128], F32))
                S.dma("sp", lambda e: e.dma_start(out=identf[:], in_=k_ident), "c0", writes=["identf"])
                S.op("dve", lambda e: e.tensor_copy(out=identb[:], in_=identf[:]), reads=["identf"], writes=["identb"])
                S.dma("sp", lambda e: e.dma_start(out=tmpf[:], in_=k_ustrict), "c1", writes=["tmpf"])
                S.op("dve", lambda e: e.tensor_copy(out=ustrict[:], in_=tmpf[:]), reads=["tmpf"], writes=["ustrict"])
                S.op("pool", lambda e: e.memset(onesb[:], 1.0), writes=["onesb"])
                S.op("pool", lambda e: e.memset(zerob[:], 0.0), writes=["zerob"])
                S.op("pool", lambda e: e.memset(ones13[:], 1.0), writes=["ones13"])
                for v in range(3):
                    srcv = c_in[v, :] if v < 2 else cctx_in[0, :]
                    S.dma("sp", lambda e, v=v, srcv=srcv: e.dma_start(out=csT[:, :, v], in_=srcv.rearrange("(c p) -> p c", p=128),
                                                                       allow_slow_non_contiguous=True), "c2", writes=["csT"])
                S.op("act", lambda e: e.activation(out=csT[:], in_=csT[:], func=AF.Silu), reads=["csT"], writes=["csT"])
                S.dma("sp", lambda e: e.dma_start(out=eoff[:], in_=k_eoff[0, :].partition_broadcast(128)), "c4", writes=["eoff"])
                S.flush()

        def phase_ada(i):
            with ExitStack() as es:
                def sb(name, shape, dt):
                    return es.enter_context(nc.sbuf_tensor(un(name), list(shape), dt))
                wst = [sb("ada_w%d" % k, [128, 8, 512], F32) for k in range(2)]
                brow = sb("ada_b", [1, 6 * D], F32)
                modrow = sb("ada_mod", [3, 6 * D], F32)
                lng = sb("ada_lng", [3, 2, D], F32)
                outrow = sb("ada_out", [3, 6, D], F32)
                tmp = sb("ada_tmp", [3, D], F32)
                pm = [es.enter_context(nc.psum_tensor(un("ada_ps%d" % k), [3, 512], F32)) for k in range(2)]
                S.dma("sp", lambda e: e.dma_start(out=brow[:], in_=b_ada[i:i + 1, :]), "ab", writes=["brow"])
                S.dma("sp", lambda e: e.dma_start(out=lng[:, 0, :], in_=ln1_g[i, :].partition_broadcast(3)), "al", writes=["lng"])
                S.dma("sp", lambda e: e.dma_start(out=lng[:, 1, :], in_=ln1_b[i, :].partition_broadcast(3)), "al", writes=["lng"])
                wv = w_ada[i].rearrange("(c p) n -> p c n", p=128)
                for n in range(12):
                    k = n % 2
                    S.dma("sp", lambda e, n=n, k=k: e.dma_start(out=wst[k][:], in_=wv[:, :, n * 512:(n + 1) * 512]),
                          "aw%d" % k, writes=["wst%d" % k])
                    for c in range(8):
                        S.op("pe", lambda e, c=c, k=k: e.matmul(pm[k][:], lhsT=csT[:, c, :], rhs=wst[k][:, c, :],
                                                                 start=(c == 0), stop=False),
                             reads=["csT", "wst%d" % k], writes=["pm%d" % k])
                    S.op("pe", lambda e, n=n, k=k: e.matmul(pm[k][:], lhsT=ones13[0:1, 0:3], rhs=brow[0:1, n * 512:(n + 1) * 512],
                                                             start=False, stop=True),
                         reads=["ones13", "brow"], writes=["pm%d" % k])
                    S.op("act", lambda e, n=n, k=k: e.copy(out=modrow[:, n * 512:(n + 1) * 512], in_=pm[k][:]),
                         reads=["pm%d" % k], writes=["modrow"])

                def m(k):
                    return modrow[:, k * D:(k + 1) * D]
                S.op("dve", lambda e: e.tensor_copy(out=outrow[:, 0, :], in_=m(0)), reads=["modrow"], writes=["o0"])
                S.op("dve", lambda e: e.tensor_scalar_add(out=outrow[:, 1, :], in0=m(1), scalar1=1.0), reads=["modrow"], writes=["o1"])
                S.op("dve", lambda e: e.tensor_scalar_mul(out=outrow[:, 2, :], in0=m(2), scalar1=INV_ALPHA), reads=["modrow"], writes=["o2"])
                S.op("dve", lambda e: e.tensor_scalar_add(out=tmp[:], in0=m(4), scalar1=1.0), reads=["modrow"], writes=["tmp"])
                S.op("dve", lambda e: e.tensor_tensor(out=outrow[:, 3, :], in0=tmp[:], in1=lng[:, 0, :], op=ALU.mult),
                     reads=["tmp", "lng"], writes=["o3"])
                S.op("dve", lambda e: e.tensor_tensor(out=outrow[:, 4, :], in0=tmp[:], in1=lng[:, 1, :], op=ALU.mult),
                     reads=["tmp", "lng"], writes=["o4"])
                S.op("dve", lambda e: e.tensor_tensor(out=outrow[:, 4, :], in0=outrow[:, 4, :], in1=m(3), op=ALU.add),
                     reads=["o4", "modrow"], writes=["o4"])
                S.op("dve", lambda e: e.tensor_scalar_mul(out=outrow[:, 5, :], in0=m(5), scalar1=INV_ALPHA), reads=["modrow"], writes=["o5"])
                S.dma("sp", lambda e: e.dma_start(out=MODR[i], in_=outrow[:]), "ao",
                      reads=["o0", "o1", "o2", "o3", "o4", "o5"], writes=["MODR"])
                S.flush()

        class Rot:
            def __init__(self, es, name, n, shape, dt, psum=False):
                self.bufs = []
                for k in range(n):
                    if psum:
                        t = es.enter_context(nc.psum_tensor(un("%s%d" % (name, k)), list(shape), dt))
                    else:
                        t = es.enter_context(nc.sbuf_tensor(un("%s%d" % (name, k)), list(shape), dt))
                    self.bufs.append((t, "%s%d" % (name, k)))
                self.i = 0

            def next(self):
                r = self.bufs[self.i % len(self.bufs)]
                self.i += 1
                return r

        def layer_norm_core(es_bufs, t, t_id, xn, xn_id):
            st, mv, rs = es_bufs
            stt, st_id = st.next()
            mvt, mv_id = mv.next()
            rst, rs_id = rs.next()
            S.op("dve", lambda e: e.bn_stats(out=stt[:, 0, :], in_=t[:, 0:512]), reads=[t_id], writes=[st_id])
            S.op("dve", lambda e: e.bn_stats(out=stt[:, 1, :], in_=t[:, 512:1024]), reads=[t_id], writes=[st_id])
            S.op("dve", lambda e: e.bn_aggr(out=mvt[:], in_=stt[:].rearrange("p a b -> p (a b)")), reads=[st_id], writes=[mv_id])
            S.op("dve", lambda e: e.tensor_scalar_add(out=rst[:, 0:1], in0=mvt[:, 1:2], scalar1=EPS2), reads=[mv_id], writes=[rs_id])
            S.op("act", lambda e: e.activation(out=rst[:, 0:1], in_=rst[:, 0:1], func=AF.Ln), reads=[rs_id], writes=[rs_id])
            S.op("act", lambda e: e.activation(out=rst[:, 0:1], in_=rst[:, 0:1], func=AF.Exp, scale=-0.5), reads=[rs_id], writes=[rs_id])
            S.op("dve", lambda e: e.scalar_tensor_tensor(out=rst[:, 1:2], in0=mvt[:, 0:1], scalar=-1.0, in1=rst[:, 0:1],
                                                         op0=ALU.mult, op1=ALU.mult), reads=[mv_id, rs_id], writes=[rs_id])
            S.op("act", lambda e: e.activation(out=xn[:], in_=t[:], func=AF.Identity, scale=rst[:, 0:1], bias=rst[:, 1:2]),
                 reads=[t_id, rs_id], writes=[xn_id])

        class MixCtx:
            pass

        def mixer_common_alloc(es, i):
            M = MixCtx()

            def sb(name, shape, dt):
                return es.enter_context(nc.sbuf_tensor(un(name), list(shape), dt))
            M.G1 = sb("mx_G1", [128, D], F32)
            M.A2 = sb("mx_A2", [128, D], F32)
            M.S2 = sb("mx_S2", [128, D], F32)
            M.lg = sb("mx_lg", [128, D], F32)
            M.lb = sb("mx_lb", [128, D], F32)
            M.wr = sb("mx_wr", [128, 8, 36], BF16)
            M.wrf = sb("mx_wrf", [128, 8, 36], F32)
            M.rb = sb("mx_rb", [128, 36], F32)
            M.xt = Rot(es, "mx_xt", 2, [128, D], F32)
            M.t = Rot(es, "mx_t", 2, [128, D], F32)
            M.xn = Rot(es, "mx_xn", 2, [128, D], F32)
            M.h2 = Rot(es, "mx_h2", 2, [128, D], BF16)
            M.h2T = Rot(es, "mx_h2T", 2, [128, 8, 128], BF16)
            M.st = Rot(es, "mx_st", 2, [128, 2, 6], F32)
            M.mv = Rot(es, "mx_mv", 2, [128, 2], F32)
            M.rs = Rot(es, "mx_rs", 2, [128, 2], F32)
            M.rt = Rot(es, "mx_rt", 2, [128, 256], F32)
            M.selb = Rot(es, "mx_sel", 2, [128, 32], BF16)
            M.cur_v = None
            S.dma("sp", lambda e: e.dma_start(out=M.wrf[:], in_=router_w[i].rearrange("(c p) n -> p c n", p=128)), "mwr", writes=["wrf"])
            S.op("pool", lambda e: e.tensor_copy(out=M.wr[:], in_=M.wrf[:]), reads=["wrf"], writes=["wr"])
            S.dma("sp", lambda e: e.dma_start(out=M.rb[:], in_=router_b[i, :].partition_broadcast(128)), "mrb", writes=["rb"])
            S.dma("sp", lambda e: e.dma_start(out=M.lg[:], in_=ln1_g[i, :].partition_broadcast(128)), "mlg", writes=["lg"])
            S.dma("sp", lambda e: e.dma_start(out=M.lb[:], in_=ln1_b[i, :].partition_broadcast(128)), "mlb", writes=["lb"])
            return M

        def load_variant(M, i, v):
            if M.cur_v == v:
                return
            M.cur_v = v
            S.dma("sp", lambda e: e.dma_start(out=M.G1[:], in_=MODR[i, v, 2, :].partition_broadcast(128)), "mG1", writes=["G1"])
            S.dma("sp", lambda e: e.dma_start(out=M.A2[:], in_=MODR[i, v, 3, :].partition_broadcast(128)), "mA2", writes=["A2"])
            S.dma("sp", lambda e: e.dma_start(out=M.S2[:], in_=MODR[i, v, 4, :].partition_broadcast(128)), "mS2", writes=["S2"])

        def out_stage(M, P, i, tt, po, po_ids):
            xt, xt_id = M.xt.next()
            t, t_id = M.t.next()
            xn, xn_id = M.xn.next()
            h2, h2_id = M.h2.next()
            h2T, h2T_id = M.h2T.next()
            rt, rt_id = M.rt.next()
            selb, selb_id = M.selb.next()
            src = tile_src(i, tt)
            S.dma("sp", lambda e: e.dma_start(out=xt[:], in_=src), xt_id, writes=[xt_id])
            S.op("dve", lambda e: e.tensor_tensor(out=t[:], in0=po, in1=M.G1[:], op=ALU.mult), reads=list(po_ids) + ["G1"], writes=[t_id])
            dbg_store("T", 0, t[:], t_id, tt)
            S.op("dve", lambda e: e.tensor_tensor(out=t[:], in0=t[:], in1=xt[:], op=ALU.add), reads=[t_id, xt_id], writes=[t_id])
            dbg_store("T", 1, t[:], t_id, tt)
            layer_norm_core((M.st, M.mv, M.rs), t, t_id, xn, xn_id)
            dbg_store("T", 2, xn[:], xn_id, tt)
            S.op("dve", lambda e: e.tensor_tensor(out=t[:], in0=xn[:], in1=M.A2[:], op=ALU.mult), reads=[xn_id, "A2"], writes=[t_id])
            S.op("dve", lambda e: e.tensor_tensor(out=h2[:], in0=t[:], in1=M.S2[:], op=ALU.add), reads=[t_id, "S2"], writes=[h2_id])
            dbg_store("H2", None, h2[:], h2_id, tt)
            S.op("dve", lambda e: e.tensor_tensor(out=xn[:], in0=xn[:], in1=M.lg[:], op=ALU.mult), reads=[xn_id, "lg"], writes=[xn_id])
            S.op("dve", lambda e: e.tensor_tensor(out=xn[:], in0=xn[:], in1=M.lb[:], op=ALU.add), reads=[xn_id, "lb"], writes=[xn_id])
            S.dma("sp", lambda e: e.dma_start(out=X[tt * 128:(tt + 1) * 128, :], in_=xn[:]), "st_" + xn_id, reads=[xn_id], writes=["X%d" % tt])
            for hf4 in range(2):
                for c in range(4):
                    S.op("pe", lambda e, c=c, hf4=hf4: e.transpose(out=P.ptb[:, c, :], in_=h2[:, (hf4 * 4 + c) * 128:(hf4 * 4 + c + 1) * 128], identity=identb[:]),
                         reads=[h2_id, "identb"], writes=["ptb"])
                S.op("act", lambda e, hf4=hf4: e.copy(out=h2T[:, hf4 * 4:(hf4 + 1) * 4, :], in_=P.ptb), reads=["ptb"], writes=[h2T_id])
            for c in range(8):
                S.op("pe", lambda e, c=c: e.matmul(P.plg[:, 0:36], lhsT=h2T[:, c, :], rhs=M.wr[:, c, :], start=(c == 0), stop=(c == 7)),
                     reads=[h2T_id, "wr"], writes=["plg"])
            if not (KDBG & 2):
                route(M, P, tt, rt, rt_id, selb, selb_id)
            dbg_store("RT", None, rt[:], rt_id, tt)
            for k in range(2 if not (KDBG & 6) else 0):
                S.dma("pool", lambda e, k=k: e.indirect_dma_start(
                    out=XS, out_offset=bass.IndirectOffsetOnAxis(ap=destI[:, tt, k:k + 1], axis=0),
                    in_=h2[:], in_offset=None, bounds_check=breg(e), oob_is_err=False),
                    "sc_" + h2_id + str(k), reads=[h2_id, "destI%d" % tt], writes=["XS"])

        def route(M, P, tt, rt, rt_id, selb, selb_id):
            lgt = rt[:, 0:36]
            gl = rt[:, 0:4]
            el = rt[:, 4:36]
            gm = rt[:, 40:44]
            pen = rt[:, 44:48]
            em = rt[:, 48:80]
            oh1 = rt[:, 80:112]
            em2 = rt[:, 112:144]
            oh2 = rt[:, 144:176]
            slot = rt[:, 176:208]
            prod = rt[:, 208:240]
            sc = rt[:, 240:256]
            gex = rt[:, 36:40]
            W = [rt_id]
            R = [rt_id]

            def dv(fn, extra_r=(), extra_w=()):
                S.op("dve", fn, reads=R + list(extra_r), writes=W + list(extra_w))
            dv(lambda e: e.tensor_tensor(out=lgt, in0=P.plg[:, 0:36], in1=M.rb[:], op=ALU.add), extra_r=["plg", "rb"])
            dv(lambda e: e.reduce_max(out=sc[:, 0:1], in_=gl, axis=AX))
            dv(lambda e: e.tensor_scalar(out=gm, in0=gl, scalar1=sc[:, 0:1], scalar2=None, op0=ALU.is_ge))
            dv(lambda e: e.tensor_scalar_mul(out=sc[:, 1:2], in0=sc[:, 0:1], scalar1=-1.0))
            S.op("act", lambda e: e.activation(out=gex, in_=gl, func=AF.Exp, bias=sc[:, 1:2], scale=1.0, accum_out=sc[:, 2:3]),
                 reads=R, writes=W)
            dv(lambda e: e.reciprocal(out=sc[:, 3:4], in_=sc[:, 2:3]))
            dv(lambda e: e.tensor_scalar(out=pen, in0=gm, scalar1=BIG, scalar2=-BIG, op0=ALU.mult, op1=ALU.add))
            dv(lambda e: e.tensor_tensor(out=em.rearrange("p (g k) -> p g k", k=8), in0=el.rearrange("p (g k) -> p g k", k=8),
                                         in1=pen.unsqueeze(2).to_broadcast([128, 4, 8]), op=ALU.add))
            dv(lambda e: e.reduce_max(out=sc[:, 4:5], in_=em, axis=AX))
            dv(lambda e: e.tensor_scalar(out=oh1, in0=em, scalar1=sc[:, 4:5], scalar2=None, op0=ALU.is_ge))
            dv(lambda e: e.scalar_tensor_tensor(out=em2, in0=oh1, scalar=-BIG, in1=em, op0=ALU.mult, op1=ALU.add))
            dv(lambda e: e.reduce_max(out=sc[:, 5:6], in_=em2, axis=AX))
            dv(lambda e: e.tensor_scalar(out=oh2, in0=em2, scalar1=sc[:, 5:6], scalar2=None, op0=ALU.is_ge))
            dv(lambda e: e.tensor_tensor(out=sc[:, 6:7], in0=sc[:, 5:6], in1=sc[:, 4:5], op=ALU.subtract))
            S.op("act", lambda e: e.activation(out=sc[:, 7:8], in_=sc[:, 6:7], func=AF.Exp), reads=R, writes=W)
            dv(lambda e: e.tensor_scalar_add(out=sc[:, 7:8], in0=sc[:, 7:8], scalar1=1.0))
            dv(lambda e: e.reciprocal(out=sc[:, 8:9], in_=sc[:, 7:8]))
            dv(lambda e: e.tensor_tensor(out=gates[:, tt, 0:1], in0=sc[:, 8:9], in1=sc[:, 3:4], op=ALU.mult), extra_w=["gates%d" % tt])
            dv(lambda e: e.tensor_tensor(out=gates[:, tt, 1:2], in0=sc[:, 3:4], in1=gates[:, tt, 0:1], op=ALU.subtract),
               extra_r=["gates%d" % tt], extra_w=["gates%d" % tt])
            dv(lambda e: e.tensor_tensor(out=selb[:], in0=oh1, in1=oh2, op=ALU.add), extra_w=[selb_id])
            S.op("pe", lambda e: e.matmul(P.plg[:, 64:96], lhsT=ustrict[:], rhs=selb[:], start=True, stop=True),
                 reads=[selb_id, "ustrict"], writes=["prk"])
            S.op("pe", lambda e: e.matmul(P.plg[:, 128:160], lhsT=onesb[:], rhs=selb[:], start=True, stop=True),
                 reads=[selb_id, "onesb"], writes=["ptot"])
            dv(lambda e: e.tensor_tensor(out=slot, in0=P.plg[:, 64:96], in1=cnt[:], op=ALU.add), extra_r=["prk", "cnt"])
            dv(lambda e: e.tensor_scalar(out=prod, in0=slot, scalar1=float(CAP), scalar2=4.0e6, op0=ALU.is_ge, op1=ALU.mult))
            dv(lambda e: e.tensor_tensor(out=slot, in0=slot, in1=prod, op=ALU.add))
            dv(lambda e: e.tensor_tensor(out=slot, in0=slot, in1=eoff[:], op=ALU.add), extra_r=["eoff"])
            dv(lambda e: e.tensor_tensor(out=cnt[:], in0=cnt[:], in1=P.plg[:, 128:160], op=ALU.add), extra_r=["ptot", "cnt"], extra_w=["cnt"])
            dv(lambda e: e.tensor_tensor(out=prod, in0=oh1, in1=slot, op=ALU.mult))
            dv(lambda e: e.reduce_sum(out=sc[:, 9:10], in_=prod, axis=AX))
            dv(lambda e: e.tensor_tensor(out=prod, in0=oh2, in1=slot, op=ALU.mult))
            dv(lambda e: e.reduce_sum(out=sc[:, 10:11], in_=prod, axis=AX))
            dv(lambda e: e.tensor_copy(out=destI[:, tt, :], in_=sc[:, 9:11]), extra_w=["destI%d" % tt])

        def make_hT(es, P, i, b, hT, scal):
            xr = Rot(es, "hx_x", 3, [128, D], F32)
            for seg, v in ((0, b), (1, 2)):
                S.dma("sp", lambda e, v=v: e.dma_start(out=scal[:], in_=MODR[i, v, 0:2, :].rearrange("k (c p) -> p k c", p=128),
                                                        allow_slow_non_contiguous=True), "hsc", writes=["scal"])
                tiles = range(16) if seg == 0 else range(16, 18)
                for j in tiles:
                    tt = b * TPB + j
                    xt, xid = xr.next()
                    src = tile_src(i, tt)
                    S.dma("sp", lambda e, xt=xt, src=src: e.dma_start(out=xt[:], in_=src), xid, reads=["X%d" % tt], writes=[xid])
                    for c in range(8):
                        S.op("pe", lambda e, c=c, xt=xt: e.transpose(out=P.ptf[:, c, :], in_=xt[:, c * 128:(c + 1) * 128], identity=identf[:]),
                             reads=[xid, "identf"], writes=["ptf"])
                    for c in range(8):
                        if c % 2 == 0:
                            S.op("act", lambda e, c=c, j=j: e.activation(out=hT[:, c, j * 128:(j + 1) * 128], in_=P.ptf[:, c, :], func=AF.Identity,
                                                                          scale=scal[:, 1, c:c + 1], bias=scal[:, 0, c:c + 1]),
                                 reads=["ptf", "scal"], writes=["hT"])
                        else:
                            S.op("dve", lambda e, c=c, j=j: e.tensor_scalar(out=hT[:, c, j * 128:(j + 1) * 128], in0=P.ptf[:, c, :],
                                                                             scalar1=scal[:, 1, c:c + 1], scalar2=scal[:, 0, c:c + 1],
                                                                             op0=ALU.mult, op1=ALU.add),
                                 reads=["ptf", "scal"], writes=["hT"])

        HB = [(0, h) if h < 6 else ((1, h - 6) if h < 11 else (2, h - 11)) for h in range(16)]
        TOKG = [(0, 512), (512, 512), (1024, 512), (1536, 512), (2048, 256)]

        class PsumSet:
            pass

        def alloc_psum(es):
            P = PsumSet()

            def ps(name, shape, dt):
                return es.enter_context(nc.psum_tensor(un(name), list(shape), dt))
            P.ptf = ps("ps_ptf", [128, 8, 128], F32)
            P.pA = ps("ps_A", [128, 512], F32)
            P.pB = ps("ps_B", [128, 512], F32)
            P.oacc = ps("ps_oacc", [128, 3, 512], F32)
            P.b7 = ps("ps_b7", [128, 512], F32)
            P.ptb = P.b7[:, 0:256].bitcast(BF16).rearrange("p (c t) -> p c t", t=128)
            P.plg = P.b7[:, 256:512]
            P.pC = P.oacc[:, 0, :]
            return P

        def phase_attn(i, j, kind, last):
            is_a = kind == 0
            nkv = 4 if is_a else 16
            nqk_cols = 1536 if is_a else 2048
            nk_chunks = 4 if is_a else 8
            nv_cols = 256 if is_a else 1024
            wqk = attn_wqk[j] if is_a else nat_wqk[j]
            wv = attn_wv[j] if is_a else nat_wv[j]
            wo = attn_wo[j] if is_a else nat_wo[j]
            scale = 0.125
            if not is_a:
                pats, nat_chunks = _NATP[0], _NATP[1]
            for b in range(NB):
                with ExitStack() as es:
                    def sb(name, shape, dt):
                        return es.enter_context(nc.sbuf_tensor(un(name), list(shape), dt))
                    P = alloc_psum(es)
                    kT = sb("at_kT", [128, nk_chunks, 2304], BF16)
                    va = sb("at_va", [128, TPB, nkv, 65], BF16)
                    S.op("pool", lambda e: e.memset(va[:], 1.0), writes=["va"])
                    with ExitStack() as es1:
                        def sb1(name, shape, dt):
                            return es1.enter_context(nc.sbuf_tensor(un(name), list(shape), dt))
                        hT = sb1("at_hT", [128, 8, 2304], BF16)
                        scal = sb1("at_scal", [128, 2, 8], F32)
                        wstg = Rot(es1, "at_wst", 2, [128, 8, 256], F32)
                        wbf = Rot(es1, "at_wbf", 2, [128, 8, 256], BF16)
                        qst = Rot(es1, "at_qst", 2, [128, 512], BF16)
                        if is_a:
                            raw = Rot(es1, "at_raw", 2, [128, 512], BF16)
                            tm1 = Rot(es1, "at_tm1", 2, [128, 512], F32)
                            tm2 = Rot(es1, "at_tm2", 2, [128, 512], F32)
                            cosT = sb1("at_cos", [128, SEQ], F32)
                            sinT = sb1("at_sin", [128, SEQ], F32)
                            pmf = sb1("at_pmf", [128, 128], F32)
                            pmb = sb1("at_pmb", [128, 128], BF16)
                            S.dma("sp", lambda e: e.dma_start(out=cosT[:], in_=k_cos), "rc", writes=["cosT"])
                            S.dma("sp", lambda e: e.dma_start(out=sinT[:], in_=k_sin), "rs", writes=["sinT"])
                            S.dma("sp", lambda e: e.dma_start(out=pmf[:], in_=k_pm), "rp", writes=["pmf"])
                            S.op("dve", lambda e: e.tensor_copy(out=pmb[:], in_=pmf[:]), reads=["pmf"], writes=["pmb"])
                        make_hT(es1, P, i, b, hT, scal)
                        wqk_v = wqk.rearrange("(c p) n -> p c n", p=128)
                        wv_v = wv.rearrange("(c p) n -> p c n", p=128)
                        pab = [(P.pA, "pA"), (P.pB, "pB")]
                        pcount = 0
                        for g in range(nqk_cols // 256):
                            wst, wst_id = wstg.next()
                            wb, wb_id = wbf.next()
                            S.dma("sp", lambda e, wst=wst, g=g: e.dma_start(out=wst[:], in_=wqk_v[:, :, g * 256:(g + 1) * 256]),
                                  wst_id, writes=[wst_id])
                            S.op("pool", lambda e, wst=wst, wb=wb: e.tensor_copy(out=wb[:], in_=wst[:]), reads=[wst_id], writes=[wb_id])
                            for cc in range(2):
                                chunk = g * 2 + cc
                                isq = chunk < 8
                                dchunk = chunk if isq else chunk - 8
                                for (t0, tn) in TOKG:
                                    if isq:
                                        qs_, dst_id = qst.next()
                                        dst_ap = qs_[:, 0:tn]
                                    else:
                                        dst_ap = kT[:, dchunk, t0:t0 + tn]
                                        dst_id = "kT"
                                    pp, pp_id = pab[pcount % 2]
                                    pcount += 1
                                    for c in range(8):
                                        S.op("pe", lambda e, pp=pp, wb=wb, cc=cc, c=c, t0=t0, tn=tn: e.matmul(
                                            pp[:, 0:tn], lhsT=wb[:, c, cc * 128:(cc + 1) * 128], rhs=hT[:, c, t0:t0 + tn],
                                            start=(c == 0), stop=(c == 7)), reads=[wb_id, "hT"], writes=[pp_id])
                                    rope = is_a and t0 < SEQ
                                    if not rope:
                                        S.op("act", lambda e, pp=pp, dst_ap=dst_ap, tn=tn: e.copy(
                                            out=dst_ap, in_=pp[:, 0:tn]), reads=[pp_id], writes=[dst_id])
                                    else:
                                        rw, rw_id = raw.next()
                                        a1, a1_id = tm1.next()
                                        a2, a2_id = tm2.next()
                                        S.op("act", lambda e, pp=pp, rw=rw: e.copy(out=rw[:], in_=pp[:]), reads=[pp_id], writes=[rw_id])
                                        S.op("pe", lambda e, rw=rw: e.matmul(P.pC, lhsT=pmb[:], rhs=rw[:], start=True, stop=True),
                                             reads=[rw_id, "pmb"], writes=["pC"])
                                        S.op("dve", lambda e, a1=a1, t0=t0: e.tensor_tensor(out=a1[:], in0=P.pC, in1=sinT[:, t0:t0 + 512], op=ALU.mult),
                                             reads=["pC", "sinT"], writes=[a1_id])
                                        S.op("pool", lambda e, a2=a2, rw=rw, t0=t0: e.tensor_tensor(out=a2[:], in0=rw[:], in1=cosT[:, t0:t0 + 512], op=ALU.mult),
                                             reads=[rw_id, "cosT"], writes=[a2_id])
                                        S.op("dve", lambda e, a1=a1, a2=a2, dst_ap=dst_ap: e.tensor_tensor(
                                            out=dst_ap, in0=a1[:], in1=a2[:], op=ALU.add),
                                            reads=[a1_id, a2_id], writes=[dst_id])
                                    if isq:
                                        S.dma("sp", lambda e, dst_ap=dst_ap, dchunk=dchunk, t0=t0, tn=tn: e.dma_start(
                                            out=QT[:, dchunk, t0:t0 + tn], in_=dst_ap), "st" + dst_id, reads=[dst_id], writes=["QT"])
                        for g in range(nv_cols // 256):
                            ncol = 256
                            wst, wst_id = wstg.next()
                            wb, wb_id = wbf.next()
                            S.dma("sp", lambda e, wst=wst, g=g, ncol=ncol: e.dma_start(out=wst[:, :, 0:ncol], in_=wv_v[:, :, g * 256:g * 256 + ncol]),
                                  wst_id, writes=[wst_id])
                            S.op("pool", lambda e, wst=wst, wb=wb, ncol=ncol: e.tensor_copy(out=wb[:, :, 0:ncol], in_=wst[:, :, 0:ncol]),
                                 reads=[wst_id], writes=[wb_id])
                            nh = ncol // 64
                            for jt in range(TPB):
                                pp, pp_id = pab[pcount % 2]
                                pcount += 1
                                for c in range(8):
                                    S.op("pe", lambda e, pp=pp, wb=wb, c=c, jt=jt, ncol=ncol: e.matmul(
                                        pp[:, 0:ncol], lhsT=hT[:, c, jt * 128:(jt + 1) * 128], rhs=wb[:, c, 0:ncol],
                                        start=(c == 0), stop=(c == 7)), reads=[wb_id, "hT"], writes=[pp_id])
                                S.op("act", lambda e, pp=pp, jt=jt, g=g, nh=nh, ncol=ncol: e.copy(
                                    out=va[:, jt, g * 4:g * 4 + nh, 0:64], in_=pp[:, 0:ncol].rearrange("p (h d) -> p h d", d=64)),
                                    reads=[pp_id], writes=["va"])
                        S.flush()
                    with ExitStack() as es2:
                        def sb2(name, shape, dt):
                            return es2.enter_context(nc.sbuf_tensor(un(name), list(shape), dt))
                        M = mixer_common_alloc(es2, i)
                        wob = sb2("at_wob", [128, 8, D], BF16)
                        wstg = Rot(es2, "at_wst2_", 2, [128, 8, 256], F32)
                        for g in range(4):
                            wst, wst_id = wstg.next()
                            S.dma("sp", lambda e, wst=wst, g=g: e.dma_start(out=wst[:], in_=wo.rearrange("(c p) n -> p c n", p=128)[:, :, g * 256:(g + 1) * 256]),
                                  wst_id, writes=[wst_id])
                            S.op("pool", lambda e, wst=wst, g=g: e.tensor_copy(out=wob[:, :, g * 256:(g + 1) * 256], in_=wst[:]),
                                 reads=[wst_id], writes=["wob"])
                        sinkx = sb2("at_sink", [128, 16], F32)
                        if is_a:
                            S.dma("sp", lambda e: e.dma_start(out=sinkx[:], in_=attn_sink[j, :].partition_broadcast(128)), "snk", writes=["sinkx"])
                            S.op("act", lambda e: e.activation(out=sinkx[:], in_=sinkx[:], func=AF.Exp), reads=["sinkx"], writes=["sinkx"])
                            bandf = sb2("at_bandf", [128, 2, 128], F32)
                            bandb = sb2("at_bandb", [128, 2, 128], BF16)
                            S.dma("sp", lambda e: e.dma_start(out=bandf[:], in_=k_band.rearrange("m k q -> k m q")), "bnd", writes=["bandf"])
                            S.op("dve", lambda e: e.tensor_copy(out=bandb[:], in_=bandf[:]), reads=["bandf"], writes=["bandb"])
                        else:
                            S.op("pool", lambda e: e.memset(sinkx[:], 0.0), writes=["sinkx"])
                            etr = Rot(es2, "at_et", 2, [128, 16, 128], BF16)
                        ptr_ = Rot(es2, "at_pt", 4, [128, 512], BF16)
                        otok = Rot(es2, "at_otok", 2, [128, D], BF16)
                        oTr = Rot(es2, "at_oT", 2, [128, 8, 128], BF16)
                        den = Rot(es2, "at_den", 2, [128, 16], F32)
                        pab = [(P.pA, "pA"), (P.pB, "pB")]
                        pcount = 0
                        qtiles = list(range(16)) + ([] if last else [16, 17])
                        qtr = Rot(es2, "at_qt", 2, [128, 16, 128], BF16)
                        for (qb_, qb_id) in qtr.bufs:
                            S.op("pool", lambda e, qb_=qb_: e.memset(qb_[:], 0.0), writes=[qb_id])
                        def kcs_for(jq):
                            if jq >= 16:
                                return [(16, None), (17, None)]
                            if is_a:
                                kcs = []
                                if jq > 0:
                                    kcs.append((jq - 1, ("band", 0)))
                                kcs.append((jq, None))
                                if jq < 15:
                                    kcs.append((jq + 1, ("band", 1)))
                                return kcs + [(16, None), (17, None)]
                            return [(kc, ("nat", pats[(jq, kc)])) for kc in nat_chunks[jq]] + [(16, None), (17, None)]

                        pstate = {"n": 0}

                        def rec_core(jq):
                            qT, qT_id = qtr.next()
                            qTv = qT[:].rearrange("p (j two) q -> p j two q", two=2)
                            kcs = kcs_for(jq)

                            def pro():
                                S.dma("sp", lambda e: e.dma_start(out=qTv[0:64, :, 0, :], in_=QT[0:64, :, jq * 128:(jq + 1) * 128]),
                                      qT_id, reads=["QT"], writes=[qT_id])
                                S.dma("sp", lambda e: e.dma_start(out=qTv[64:128, :, 1, :], in_=QT[64:128, :, jq * 128:(jq + 1) * 128]),
                                      qT_id, reads=["QT"], writes=[qT_id])
                                for bank in range(3):
                                    S.op("pe", lambda e, bank=bank: e.matmul(P.oacc[:, bank, :], lhsT=zerob[:, 0:128], rhs=zerob[:], start=True, stop=True),
                                         reads=["zerob"], writes=["oacc"])
                            prol, _ = S.capture(pro)
                            steps = []
                            for ci, (kc, msk) in enumerate(kcs):
                                et = None
                                et_id = None
                                if msk is not None and msk[0] == "nat":
                                    et, et_id = etr.next()
                                for hg in range(4):
                                    pp, pp_id = pab[pstate["n"] % 2]
                                    pstate["n"] += 1
                                    pt, pt_id = ptr_.next()

                                    def s_part(ci=ci, kc=kc, msk=msk, hg=hg, pp=pp, pp_id=pp_id, et=et, et_id=et_id):
                                        if et is not None and hg == 0:
                                            S.dma("sp", lambda e: e.dma_start(out=et[:], in_=ETAB[msk[1]]), et_id, reads=["ETAB"], writes=[et_id])
                                        for hh in range(4):
                                            h = hg * 4 + hh
                                            kch = hg if is_a else h // 2
                                            S.op("pe", lambda e, hh=hh, kch=kch, h=h: e.matmul(
                                                pp[:, hh * 128:(hh + 1) * 128], lhsT=kT[:, kch, kc * 128:(kc + 1) * 128],
                                                rhs=qT[:, h, :], start=True, stop=True),
                                                reads=[qT_id, "kT"], writes=[pp_id])

                                    def r_part(ci=ci, kc=kc, msk=msk, hg=hg, pp=pp, pp_id=pp_id, pt=pt, pt_id=pt_id, et=et, et_id=et_id, n=len(kcs)):
                                        S.op("act", lambda e: e.activation(out=pt[:], in_=pp[:], func=AF.Exp, scale=scale),
                                             reads=[pp_id], writes=[pt_id])
                                        if msk is not None:
                                            ptv = pt[:].rearrange("p (h q) -> p h q", q=128)
                                            if msk[0] == "band":
                                                S.op("pool", lambda e: e.tensor_tensor(out=ptv, in0=ptv,
                                                     in1=bandb[:, msk[1], :].unsqueeze(1).to_broadcast([128, 4, 128]), op=ALU.mult),
                                                     reads=[pt_id, "bandb"], writes=[pt_id])
                                            else:
                                                S.op("pool", lambda e: e.tensor_tensor(out=ptv, in0=ptv, in1=et[:, hg * 4:(hg + 1) * 4, :], op=ALU.mult),
                                                     reads=[pt_id, et_id], writes=[pt_id])
                                        for hh in range(4):
                                            h = hg * 4 + hh
                                            kvh = hg if is_a else h
                                            bank, off = HB[h]
                                            S.op("pe", lambda e, hh=hh, kvh=kvh, bank=bank, off=off: e.matmul(
                                                P.oacc[:, bank, off * 65:(off + 1) * 65], lhsT=pt[:, hh * 128:(hh + 1) * 128],
                                                rhs=va[:, kc, kvh, :], start=False, stop=(ci == n - 1)),
                                                reads=[pt_id, "va"], writes=["oacc"])
                                    sl, _ = S.capture(s_part)
                                    rl, _ = S.capture(r_part)
                                    steps.append((sl, rl))
                            return prol, steps

                        def core_units(steps):
                            units = []
                            n = len(steps)
                            for k in range(min(2, n)):
                                units.append(steps[k][0])
                            for k in range(n):
                                units.append(steps[k][1])
                                if k + 2 < n:
                                    units.append(steps[k + 2][0])
                            return units

                        def norm(jq):
                            tt = b * TPB + jq
                            dn, dn_id = den.next()
                            ot, ot_id = otok.next()
                            oT, oT_id = oTr.next()
                            for bank in range(3):
                                nh = (6, 5, 5)[bank]
                                h0 = (0, 6, 11)[bank]
                                S.op("dve", lambda e, bank=bank, nh=nh, h0=h0: e.tensor_tensor(
                                    out=dn[:, h0:h0 + nh], in0=P.oacc[:, bank, 0:nh * 65].rearrange("p (h d) -> p h d", d=65)[:, :, 64],
                                    in1=sinkx[:, h0:h0 + nh], op=ALU.add), reads=["oacc", "sinkx"], writes=[dn_id])
                            S.op("dve", lambda e: e.reciprocal(out=dn[:], in_=dn[:]), reads=[dn_id], writes=[dn_id])
                            for bank in range(3):
                                nh = (6, 5, 5)[bank]
                                h0 = (0, 6, 11)[bank]
                                S.op("dve", lambda e, bank=bank, nh=nh, h0=h0: e.tensor_tensor(
                                    out=ot[:, h0 * 64:(h0 + nh) * 64].rearrange("p (h d) -> p h d", d=64),
                                    in0=P.oacc[:, bank, 0:nh * 65].rearrange("p (h d) -> p h d", d=65)[:, :, 0:64],
                                    in1=dn[:, h0:h0 + nh].unsqueeze(2).to_broadcast([128, nh, 64]), op=ALU.mult),
                                    reads=["oacc", dn_id], writes=[ot_id])
                            return ot, ot_id, oT, oT_id

                        def tail(jq, ot, ot_id, oT, oT_id):
                            tt = b * TPB + jq
                            load_variant(M, i, b if jq < 16 else 2)
                            for hf4 in range(2):
                                for c in range(4):
                                    S.op("pe", lambda e, c=c, hf4=hf4: e.transpose(out=P.ptb[:, c, :], in_=ot[:, (hf4 * 4 + c) * 128:(hf4 * 4 + c + 1) * 128], identity=identb[:]),
                                         reads=[ot_id, "identb"], writes=["ptb"])
                                S.op("act", lambda e, hf4=hf4: e.copy(out=oT[:, hf4 * 4:(hf4 + 1) * 4, :], in_=P.ptb), reads=["ptb"], writes=[oT_id])
                            po = P.ptf[:].rearrange("p c t -> p (c t)")
                            for hf in range(2):
                                for c in range(8):
                                    S.op("pe", lambda e, c=c, hf=hf: e.matmul(po[:, hf * 512:(hf + 1) * 512], lhsT=oT[:, c, :],
                                                                              rhs=wob[:, c, hf * 512:(hf + 1) * 512],
                                                                              start=(c == 0), stop=(c == 7)),
                                         reads=[oT_id, "wob"], writes=["ptf"])
                            out_stage(M, P, i, tt, po, ["ptf"])

                        def interleave(units, tl):
                            out_ = []
                            nu = max(1, len(units))
                            per = -(-len(tl) // nu)
                            ti = 0
                            for u in units:
                                out_.extend(u)
                                out_.extend(tl[ti:ti + per])
                                ti += per
                            out_.extend(tl[ti:])
                            return out_

                        prol, steps = rec_core(qtiles[0])
                        S.replay(prol)
                        for u in core_units(steps):
                            S.replay(u)
                        for qi, jq in enumerate(qtiles):
                            nl, nr = S.capture(norm, jq)
                            S.replay(nl)
                            tl, _ = S.capture(tail, jq, *nr)
                            if qi + 1 < len(qtiles):
                                prol, steps = rec_core(qtiles[qi + 1])
                                S.replay(prol)
                                S.replay(interleave(core_units(steps), tl))
                            else:
                                S.replay(tl)
                        S.flush()

        def phase_nat_table():
            with ExitStack() as es:
                bt = Rot(es, "nt_b", 2, [128, 16, 128], F32)
                mt = Rot(es, "nt_m", 2, [128, 128], F32)
                eo = Rot(es, "nt_e", 2, [128, 16, 128], BF16)
                for pid in range(NPAT):
                    b_, b_id = bt.next()
                    m_, m_id = mt.next()
                    e_, e_id = eo.next()
                    S.dma("sp", lambda e, b_=b_, pid=pid: e.dma_start(out=b_[:], in_=nat_bias[pid]), b_id, writes=[b_id])
                    S.dma("sp", lambda e, m_=m_, pid=pid: e.dma_start(out=m_[:], in_=k_natmask[pid]), m_id, writes=[m_id])
                    S.op("act", lambda e, b_=b_: e.activation(out=b_[:], in_=b_[:], func=AF.Exp), reads=[b_id], writes=[b_id])
                    S.op("dve", lambda e, b_=b_, m_=m_, e_=e_: e.tensor_tensor(out=e_[:], in0=b_[:], in1=m_[:].unsqueeze(1).to_broadcast([128, 16, 128]),
                                                                              op=ALU.mult), reads=[b_id, m_id], writes=[e_id])
                    S.dma("sp", lambda e, e_=e_, pid=pid: e.dma_start(out=ETAB[pid], in_=e_[:]), "st" + e_id, reads=[e_id], writes=["ETAB"])
                S.flush()

        def phase_conv(i, j, last):
            win = conv_w_in[j].rearrange("(c p) n -> p c n", p=128)
            for b in range(NB):
                with ExitStack() as es:
                    def sb(name, shape, dt):
                        return es.enter_context(nc.sbuf_tensor(un(name), list(shape), dt))
                    P = alloc_psum(es)
                    zT = sb("cv_zT", [128, 8, 2304], BF16)
                    with ExitStack() as es1:
                        def sb1(name, shape, dt):
                            return es1.enter_context(nc.sbuf_tensor(un(name), list(shape), dt))
                        hT = sb1("cv_hT", [128, 8, 2304], BF16)
                        scal = sb1("cv_scal", [128, 2, 8], F32)
                        cw = sb1("cv_cw", [128, 3, 8], F32)
                        S.dma("sp", lambda e: e.dma_start(out=cw[:], in_=conv_w[j].rearrange("k (c p) -> p k c", p=128), allow_slow_non_contiguous=True),
                              "cw", writes=["cw"])
                        wstg = Rot(es1, "cv_wst", 2, [128, 8, 3, 128], F32)
                        wbf = Rot(es1, "cv_wbf", 2, [128, 8, 3, 128], BF16)
                        pbuf = sb1("cv_p", [128, 2308], F32)
                        gbuf = sb1("cv_g", [128, 2304], F32)
                        ubuf = Rot(es1, "cv_u", 2, [128, 512], F32)
                        acc = sb1("cv_acc", [128, 2304], F32)
                        S.op("pool", lambda e: e.memset(pbuf[:], 0.0), writes=["pbuf"])
                        make_hT(es1, P, i, b, hT, scal)
                        pbanks = [(P.pA, "pA"), (P.pB, "pB"), (P.pC, "pC")]
                        for c in range(8):
                            wst, wst_id = wstg.next()
                            wb, wb_id = wbf.next()
                            for k3 in range(3):
                                S.dma("sp", lambda e, wst=wst, k3=k3, c=c: e.dma_start(out=wst[:, :, k3, :], in_=win[:, :, k3 * D + c * 128:k3 * D + (c + 1) * 128]),
                                      wst_id, writes=[wst_id])
                            S.op("pool", lambda e, wst=wst, wb=wb: e.tensor_copy(out=wb[:], in_=wst[:]), reads=[wst_id], writes=[wb_id])
                            for (t0, tn) in TOKG:
                                poff = 1 + t0 if t0 < SEQ else 2051
                                for k3 in range(3):
                                    pp, pp_id = pbanks[k3]
                                    for kk in range(8):
                                        S.op("pe", lambda e, pp=pp, wb=wb, k3=k3, kk=kk, t0=t0, tn=tn: e.matmul(
                                            pp[:, 0:tn], lhsT=wb[:, kk, k3, :], rhs=hT[:, kk, t0:t0 + tn], start=(kk == 0), stop=(kk == 7)),
                                            reads=[wb_id, "hT"], writes=[pp_id])
                                ub, ub_id = ubuf.next()
                                S.op("act", lambda e, ub=ub, tn=tn: e.copy(out=ub[:, 0:tn], in_=P.pC[:, 0:tn]), reads=["pC"], writes=[ub_id])
                                S.op("act", lambda e, t0=t0, tn=tn: e.copy(out=gbuf[:, t0:t0 + tn], in_=P.pA[:, 0:tn]), reads=["pA"], writes=["gbuf"])
                                S.op("dve", lambda e, ub=ub, tn=tn, poff=poff: e.tensor_tensor(out=pbuf[:, poff:poff + tn], in0=P.pB[:, 0:tn], in1=ub[:, 0:tn], op=ALU.mult),
                                     reads=["pB", ub_id], writes=["pbuf"])
                            for (o0, p0, n) in ((0, 1, SEQ), (SEQ, 2051, LCTX)):
                                S.op("dve", lambda e, c=c, o0=o0, p0=p0, n=n: e.tensor_scalar(out=acc[:, o0:o0 + n], in0=pbuf[:, p0:p0 + n], scalar1=cw[:, 1, c:c + 1],
                                                                                             scalar2=None, op0=ALU.mult), reads=["pbuf", "cw"], writes=["acc"])
                                S.op("dve", lambda e, c=c, o0=o0, p0=p0, n=n: e.scalar_tensor_tensor(out=acc[:, o0:o0 + n], in0=pbuf[:, p0 - 1:p0 - 1 + n], scalar=cw[:, 0, c:c + 1],
                                                                                                      in1=acc[:, o0:o0 + n], op0=ALU.mult, op1=ALU.add),
                                     reads=["pbuf", "cw", "acc"], writes=["acc"])
                                S.op("dve", lambda e, c=c, o0=o0, p0=p0, n=n: e.scalar_tensor_tensor(out=acc[:, o0:o0 + n], in0=pbuf[:, p0 + 1:p0 + 1 + n], scalar=cw[:, 2, c:c + 1],
                                                                                                     in1=acc[:, o0:o0 + n], op0=ALU.mult, op1=ALU.add),
                                     reads=["pbuf", "cw", "acc"], writes=["acc"])
                            S.op("pool", lambda e, c=c: e.tensor_tensor(out=zT[:, c, :], in0=acc[:], in1=gbuf[:], op=ALU.mult),
                                 reads=["acc", "gbuf"], writes=["zT"])
                        S.flush()
                    with ExitStack() as es2:
                        def sb2(name, shape, dt):
                            return es2.enter_context(nc.sbuf_tensor(un(name), list(shape), dt))
                        M = mixer_common_alloc(es2, i)
                        wob = sb2("cv_wob", [128, 8, D], BF16)
                        wstg = Rot(es2, "cv_wst2_", 2, [128, 8, 512], F32)
                        wo = conv_w_out[j].rearrange("(c p) n -> p c n", p=128)
                        for g in range(2):
                            wst, wst_id = wstg.next()
                            S.dma("sp", lambda e, wst=wst, g=g: e.dma_start(out=wst[:], in_=wo[:, :, g * 512:(g + 1) * 512]), wst_id, writes=[wst_id])
                            S.op("pool", lambda e, wst=wst, g=g: e.tensor_copy(out=wob[:, :, g * 512:(g + 1) * 512], in_=wst[:]), reads=[wst_id], writes=["wob"])
                        qtiles = list(range(16)) + ([] if last else [16, 17])
                        po = P.ptf[:].rearrange("p c t -> p (c t)")
                        for jq in qtiles:
                            tt = b * TPB + jq
                            load_variant(M, i, b if jq < 16 else 2)
                            for hf in range(2):
                                for c in range(8):
                                    S.op("pe", lambda e, c=c, hf=hf, jq=jq: e.matmul(po[:, hf * 512:(hf + 1) * 512], lhsT=zT[:, c, jq * 128:(jq + 1) * 128],
                                                                                     rhs=wob[:, c, hf * 512:(hf + 1) * 512], start=(c == 0), stop=(c == 7)),
                                         reads=["zT", "wob"], writes=["ptf"])
                            out_stage(M, P, i, tt, po, ["ptf"])
                        S.flush()

        def phase_experts(i):
            NTB = CAP // 128
            NNT = CAP // 512
            with ExitStack() as es:
                def ps(name, shape, dt):
                    return es.enter_context(nc.psum_tensor(un(name), list(shape), dt))
                wg = Rot(es, "ex_wg", 3, [128, 8, 512], BF16)
                wu = Rot(es, "ex_wu", 3, [128, 8, 512], BF16)
                wd = Rot(es, "ex_wd", 3, [128, 4, D], BF16)
                xs = Rot(es, "ex_xs", 3, [128, NTB, D], BF16)
                xsT = Rot(es, "ex_xsT", 2, [128, 8, CAP], BF16)
                sg = Rot(es, "ex_sg", 2, [128, 512], F32)
                aT = Rot(es, "ex_aT", 2, [128, 4, CAP], BF16)
                ys = Rot(es, "ex_ys", 2, [128, D], F32)
                ptb = [(ps("ex_ptb%d" % k, [128, 8, 128], BF16), "ptb%d" % k) for k in range(2)]
                pg = [(ps("ex_pg%d" % k, [128, 512], F32), "pg%d" % k) for k in range(2)]
                pu = [(ps("ex_pu%d" % k, [128, 512], F32), "pu%d" % k) for k in range(2)]
                py = ps("ex_py", [128, D], F32)
                st8 = {"nptb": 0, "npg": 0}

                def prep(ex):
                    wgb, wg_id = wg.next()
                    wub, wu_id = wu.next()
                    wdb, wd_id = wd.next()
                    xsb, xs_id = xs.next()
                    for (src, dstb, dst_id) in ((w_gate[i, ex].rearrange("(c p) n -> p c n", p=128), wgb, wg_id),
                                                (w_up[i, ex].rearrange("(c p) n -> p c n", p=128), wub, wu_id),
                                                (w_down[i, ex].rearrange("(c p) n -> p c n", p=128), wdb, wd_id)):
                        S.dma("pool", lambda e, dstb=dstb, src=src: e.dma_start(out=dstb[:], in_=src), "ld" + dst_id, writes=[dst_id])
                    S.dma("sp", lambda e, xsb=xsb, ex=ex: e.dma_start(out=xsb[:], in_=XS[ex * CAP:(ex + 1) * CAP, :].rearrange("(j p) d -> p j d", p=128)),
                          xs_id, reads=["XS"], writes=[xs_id])
                    return dict(wgb=wgb, wg_id=wg_id, wub=wub, wu_id=wu_id, wdb=wdb, wd_id=wd_id, xsb=xsb, xs_id=xs_id)

                def late_cast(pr):
                    return

                def compute(ex, pr):
                    wgb, wg_id, wub, wu_id, wdb, wd_id, xsb, xs_id = (pr[k] for k in ("wgb", "wg_id", "wub", "wu_id", "wdb", "wd_id", "xsb", "xs_id"))
                    xTb, xT_id = xsT.next()
                    aTb, aT_id = aT.next()
                    QR = 256
                    for q in range(CAP // QR):
                        S.cur_group = (ex, q * QR + 1) if q > 0 else None
                        c0 = q * QR
                        for jt in range(2 * q, 2 * q + 2):
                            pt, pt_id = ptb[st8["nptb"] % 2]
                            st8["nptb"] += 1
                            for c in range(8):
                                S.op("pe", lambda e, pt=pt, jt=jt, c=c: e.transpose(out=pt[:, c, :], in_=xsb[:, jt, c * 128:(c + 1) * 128], identity=identb[:]),
                                     reads=[xs_id, "identb"], writes=[pt_id])
                            S.op("act", lambda e, pt=pt, jt=jt: e.copy(out=xTb[:, :, jt * 128:(jt + 1) * 128], in_=pt[:]), reads=[pt_id], writes=[xT_id])
                        for m_ in range(4):
                            pgb, pg_id = pg[st8["npg"] % 2]
                            pub, pu_id = pu[st8["npg"] % 2]
                            st8["npg"] += 1
                            for c in range(8):
                                S.op("pe", lambda e, pgb=pgb, c=c, m_=m_, c0=c0: e.matmul(pgb[:, 0:QR], lhsT=wgb[:, c, m_ * 128:(m_ + 1) * 128], rhs=xTb[:, c, c0:c0 + QR],
                                                                                          start=(c == 0), stop=(c == 7)), reads=[wg_id, xT_id], writes=[pg_id])
                            for c in range(8):
                                S.op("pe", lambda e, pub=pub, c=c, m_=m_, c0=c0: e.matmul(pub[:, 0:QR], lhsT=wub[:, c, m_ * 128:(m_ + 1) * 128], rhs=xTb[:, c, c0:c0 + QR],
                                                                                          start=(c == 0), stop=(c == 7)), reads=[wu_id, xT_id], writes=[pu_id])
                            sgb, sg_id = sg.next()
                            S.op("act", lambda e, sgb=sgb, pgb=pgb: e.activation(out=sgb[:, 0:QR], in_=pgb[:, 0:QR], func=AF.Silu), reads=[pg_id], writes=[sg_id])
                            S.op("dve", lambda e, sgb=sgb, pub=pub, m_=m_, c0=c0: e.tensor_tensor(out=aTb[:, m_, c0:c0 + QR], in0=pub[:, 0:QR], in1=sgb[:, 0:QR], op=ALU.mult),
                                 reads=[pu_id, sg_id], writes=[aT_id])
                        for jt in range(2 * q, 2 * q + 2):
                            for hf in range(2):
                                for m_ in range(4):
                                    S.op("pe", lambda e, jt=jt, hf=hf, m_=m_: e.matmul(py[:, hf * 512:(hf + 1) * 512], lhsT=aTb[:, m_, jt * 128:(jt + 1) * 128],
                                                                                       rhs=wdb[:, m_, hf * 512:(hf + 1) * 512], start=(m_ == 0), stop=(m_ == 3)),
                                         reads=[aT_id, wd_id], writes=["py"])
                            ysb, ys_id = ys.next()
                            if jt % 2 == 0:
                                S.op("act", lambda e, ysb=ysb: e.copy(out=ysb[:], in_=py[:]), reads=["py"], writes=[ys_id])
                            else:
                                S.op("dve", lambda e, ysb=ysb: e.tensor_copy(out=ysb[:], in_=py[:]), reads=["py"], writes=[ys_id])
                            r0 = ex * CAP + jt * 128
                            S.dma("sp", lambda e, ysb=ysb, r0=r0: e.dma_start(out=YS[r0:r0 + 128, :], in_=ysb[:]), "st" + ys_id, reads=[ys_id], writes=["YS"])
                    S.cur_group = None

                preps = {0: prep(0), 1: prep(1)}
                for ex in range(NEXP):
                    if ex + 2 < NEXP:
                        preps[ex + 2] = prep(ex + 2)
                    compute(ex, preps.pop(ex))
                S.flush()

        def phase_combine(i, last):
            with ExitStack() as es:
                def sb(name, shape, dt):
                    return es.enter_context(nc.sbuf_tensor(un(name), list(shape), dt))
                G2 = sb("cb_G2", [128, D], F32)
                lg = sb("cb_lg", [128, D], F32)
                lb = sb("cb_lb", [128, D], F32)
                y0 = Rot(es, "cb_y0", 2, [128, D], F32)
                y1 = Rot(es, "cb_y1", 2, [128, D], F32)
                xm = Rot(es, "cb_xm", 2, [128, D], F32)
                tb = Rot(es, "cb_t", 2, [128, D], F32)
                xn = Rot(es, "cb_xn", 2, [128, D], F32)
                xo = Rot(es, "cb_xo", 2, [128, D], F32)
                st = Rot(es, "cb_st", 2, [128, 2, 6], F32)
                mv = Rot(es, "cb_mv", 2, [128, 2], F32)
                rs = Rot(es, "cb_rs", 2, [128, 2], F32)
                S.dma("sp", lambda e: e.dma_start(out=lg[:], in_=ln2_g[i, :].partition_broadcast(128)), "clg", writes=["lg"])
                S.dma("sp", lambda e: e.dma_start(out=lb[:], in_=ln2_b[i, :].partition_broadcast(128)), "clb", writes=["lb"])
                cvar = {"v": None}

                def one_tile(tt):
                    b, j = divmod(tt, TPB)
                    v = b if j < 16 else 2
                    if v != cvar["v"]:
                        cvar["v"] = v
                        S.dma("sp", lambda e, v=v: e.dma_start(out=G2[:], in_=MODR[i, v, 5, :].partition_broadcast(128)), "cG2", writes=["G2"])
                    y0b, y0_id = y0.next()
                    y1b, y1_id = y1.next()
                    xmb, xm_id = xm.next()
                    t, t_id = tb.next()
                    xnb, xn_id = xn.next()
                    xob, xo_id = xo.next()
                    for k, (yb, y_id) in enumerate(((y0b, y0_id), (y1b, y1_id))):
                        S.dma("pool", lambda e, yb=yb, k=k, tt=tt: e.indirect_dma_start(
                            out=yb[:], out_offset=None, in_=YS, in_offset=bass.IndirectOffsetOnAxis(ap=destI[:, tt, k:k + 1], axis=0),
                            bounds_check=breg(e), oob_is_err=False), y_id, reads=["YS"], writes=[y_id])
                    S.dma("sp", lambda e, xmb=xmb, tt=tt: e.dma_start(out=xmb[:], in_=X[tt * 128:(tt + 1) * 128, :]), xm_id, reads=["X%d" % tt], writes=[xm_id])
                    S.op("act", lambda e, t=t, y0b=y0b, tt=tt: e.activation(out=t[:], in_=y0b[:], func=AF.Copy, scale=gates[:, tt, 0:1]),
                         reads=[y0_id], writes=[t_id])
                    S.op("dve", lambda e, t=t, y1b=y1b, tt=tt: e.scalar_tensor_tensor(out=t[:], in0=y1b[:], scalar=gates[:, tt, 1:2], in1=t[:], op0=ALU.mult, op1=ALU.add),
                         reads=[y1_id, t_id], writes=[t_id])
                    S.op("pool", lambda e, t=t: e.tensor_tensor(out=t[:], in0=t[:], in1=G2[:], op=ALU.mult), reads=[t_id, "G2"], writes=[t_id])
                    S.op("dve", lambda e, t=t, xmb=xmb: e.tensor_tensor(out=t[:], in0=t[:], in1=xmb[:], op=ALU.add), reads=[t_id, xm_id], writes=[t_id])
                    layer_norm_core((st, mv, rs), t, t_id, xnb, xn_id)
                    S.op("dve", lambda e, xob=xob, xnb=xnb: e.tensor_tensor(out=xob[:], in0=xnb[:], in1=lg[:], op=ALU.mult), reads=[xn_id, "lg"], writes=[xo_id])
                    S.op("pool", lambda e, xob=xob: e.tensor_tensor(out=xob[:], in0=xob[:], in1=lb[:], op=ALU.add), reads=[xo_id, "lb"], writes=[xo_id])
                    if last:
                        dst = out[b, j * 128:(j + 1) * 128, :]
                    else:
                        dst = X[tt * 128:(tt + 1) * 128, :]
                    S.dma("sp", lambda e, xob=xob, dst=dst: e.dma_start(out=dst, in_=xob[:]), "st" + xo_id, reads=[xo_id], writes=["X%d" % tt])

                tiles = [tt for tt in range(NT) if not (last and tt % TPB >= 16)]
                k = 0
                while k < len(tiles):
                    ta = tiles[k]
                    va_ = (ta // TPB) if ta % TPB < 16 else 2
                    tile_b = tiles[k + 1] if k + 1 < len(tiles) else None
                    vb_ = None if tile_b is None else ((tile_b // TPB) if tile_b % TPB < 16 else 2)
                    la, _ = S.capture(one_tile, ta)
                    if tile_b is not None and vb_ == va_:
                        lb_, _ = S.capture(one_tile, tile_b)
                        merged = []
                        for x in range(max(len(la), len(lb_))):
                            if x < len(la):
                                merged.append(la[x])
                            if x < len(lb_):
                                merged.append(lb_[x])
                        S.replay(merged)
                        k += 2
                    else:
                        S.replay(la)
                        k += 1
                S.flush()

        phase_init()
        phase_nat_table()
        for i in range(DEPTH):
            last = i == DEPTH - 1
            kind = i % 3
            j = i // 3
            phase_ada(i)
            S.op("pool", lambda e: e.memset(cnt[:], 0.0), writes=["cnt"])
            if kind == 0:
                phase_attn(i, j, 0, last)
            elif kind == 1:
                phase_conv(i, j, last)
            else:
                phase_attn(i, j, 2, last)
            S.op("dve", lambda e: e.tensor_copy(out=cnti[:], in_=cnt[:]), reads=["cnt"], writes=["cnti"])
            S.flush()
            phase_experts(i)
            phase_combine(i, last)
        if debug:
            S.limit = None
            dbgX = nc.dram_tensor("dbgX", [NT * 128, D], F32, kind="ExternalOutput").ap()
            dbgM = nc.dram_tensor("dbgM", [DEPTH, 3, 6, D], F32, kind="ExternalOutput").ap()
            dbgXS = nc.dram_tensor("dbgXS", [NSLOT, D], BF16, kind="ExternalOutput").ap()
            dbgYS = nc.dram_tensor("dbgYS", [NSLOT, D], F32, kind="ExternalOutput").ap()
            dbgQ = nc.dram_tensor("dbgQ", [128, 8, 2304], BF16, kind="ExternalOutput").ap()
            dbgD = nc.dram_tensor("dbgD", [128, NT, 2], I32, kind="ExternalOutput").ap()
            dbgG = nc.dram_tensor("dbgG", [128, NT, 2], F32, kind="ExternalOutput").ap()
            for r0 in range(0, NT * 128, 512):
                S.dma("sp", lambda e, r0=r0: e.dma_start(out=dbgX[r0:r0 + 512, :], in_=X[r0:r0 + 512, :]), "d0")
            S.dma("sp", lambda e: e.dma_start(out=dbgM, in_=MODR), "d1")
            for r0 in range(0, NSLOT, 512):
                S.dma("sp", lambda e, r0=r0: e.dma_start(out=dbgXS[r0:r0 + 512, :], in_=XS[r0:r0 + 512, :]), "d2")
                S.dma("sp", lambda e, r0=r0: e.dma_start(out=dbgYS[r0:r0 + 512, :], in_=YS[r0:r0 + 512, :]), "d3")
            S.dma("sp", lambda e: e.dma_start(out=dbgQ, in_=QT), "d4")
            S.dma("sp", lambda e: e.dma_start(out=dbgD, in_=destI[:]), "d5")
            S.dma("sp", lambda e: e.dma_start(out=dbgG, in_=gates[:]), "d6")
            S.flush()
    return nc


_NC_CACHE = {}


def _host_constants(inputs):
    cosT, sinT, Pm = _rope_tables()
    pats, chunks, dr, dc, mk, npat = _NATP
    rpb = np.asarray(inputs["nat_rpb"], np.float32)[0]
    nat_bias = np.ascontiguousarray(np.transpose(rpb[:, dr, dc], (1, 2, 0, 3))).astype(np.float32)
    aw = np.asarray(inputs["attn_w_qkv"], np.float32)
    kcols = []
    for g in range(4):
        kcols += list(range(1024 + g * 64, 1024 + (g + 1) * 64)) * 2
    attn_wqk = np.ascontiguousarray(np.concatenate([aw[:, :, :1024], aw[:, :, kcols]], axis=2))
    attn_wv = np.ascontiguousarray(aw[:, :, 1280:1536])
    nw = np.asarray(inputs["nat_w_qkv"], np.float32)
    router_w = np.ascontiguousarray(np.concatenate([inputs["router_w_group"], inputs["router_w_expert"]], axis=2)).astype(np.float32)
    router_b = np.ascontiguousarray(np.concatenate([inputs["router_b_group"], inputs["router_b_expert"]], axis=1)).astype(np.float32)
    kk = np.arange(128)
    consts = {
        "attn_wqk": attn_wqk, "attn_wv": attn_wv,
        "nat_wqk": np.ascontiguousarray(nw[:, :, :2048]), "nat_wv": np.ascontiguousarray(nw[:, :, 2048:]),
        "nat_bias": nat_bias, "router_w": router_w, "router_b": router_b,
        "k_ident": np.eye(128, dtype=np.float32), "k_cos": cosT, "k_sin": sinT, "k_pm": Pm,
        "k_band": _band_masks(), "k_natmask": mk,
        "k_ustrict": (kk[:, None] < kk[None, :]).astype(np.float32),
        "k_eoff": (np.arange(32, dtype=np.float32) * CAP)[None, :],
    }
    return consts


def kernel(**inputs):
    if "nc" not in _NC_CACHE:
        _NC_CACHE["nc"] = build_program()
    nc = _NC_CACHE["nc"]
    consts = _host_constants(inputs)
    shared = {}
    for k in ("w_ada", "b_ada", "ln1_g", "ln1_b", "ln2_g", "ln2_b", "attn_w_o", "attn_sink", "conv_w_in", "conv_w",
              "conv_w_out", "nat_w_o", "expert_w_gate", "expert_w_up", "expert_w_down"):
        shared[k] = np.ascontiguousarray(np.asarray(inputs[k], np.float32))
    shared.update(consts)
    shared["c_ctx"] = np.ascontiguousarray(np.asarray(inputs["c_ctx"], np.float32).reshape(1, D))
    x = np.asarray(inputs["x"], np.float32)
    c = np.asarray(inputs["c"], np.float32)
    ctx = np.asarray(inputs["ctx"], np.float32)
    in_maps = []
    for core in range(NCORES):
        m = dict(shared)
        m["x"] = np.ascontiguousarray(x[core * NB:(core + 1) * NB])
        m["c"] = np.ascontiguousarray(c[core * NB:(core + 1) * NB])
        m["ctx"] = np.ascontiguousarray(ctx[core * NB:(core + 1) * NB])
        in_maps.append(m)
    res = run_bass_kernel_spmd(nc, in_maps, core_ids=list(range(NCORES)))
    return np.concatenate([r["out"] for r in res.results], axis=0).astype(np.float32)
```

```python
import numpy as np
from contextlib import ExitStack
import ml_dtypes
import concourse.bass as bass
import concourse.mybir as mybir
from concourse.bass_utils import run_bass_kernel_spmd

F32 = mybir.dt.float32
BF16 = mybir.dt.bfloat16
I32 = mybir.dt.int32
AF = mybir.ActivationFunctionType
ALU = mybir.AluOpType
AX = mybir.AxisListType.X

NCORES = 8
D = 1024
SEQ = 2048
LCTX = 256
DEPTH = 4
NB = 2
TPB = 18
NT = NB * TPB
NEXP = 32
FF = 512
CAP = 1024
NSLOT = NEXP * CAP
ALPHA = (2 * DEPTH) ** 0.25
INV_ALPHA = 1.0 / ALPHA
EPS2 = 1e-5 / (ALPHA * ALPHA)
BIG = 1.0e30
ENGS = ("pe", "act", "dve", "pool", "sp")
import os as _os
KDBG = int(_os.environ.get("KDBG", "0"))


class _Op:
    __slots__ = ("idx", "eng", "fn", "is_dma", "key", "dma_val", "signal", "milestone", "waits", "group")

    def __init__(self, idx, eng, fn, is_dma, key):
        self.group = None
        self.idx = idx
        self.eng = eng
        self.fn = fn
        self.is_dma = is_dma
        self.key = key
        self.dma_val = 0
        self.signal = False
        self.milestone = 0
        self.waits = []


class Sched:
    def __init__(self, nc, es):
        self.nc = nc
        self.es = es
        self.esem = {e: es.enter_context(nc.semaphore("s_" + e)) for e in ENGS}
        self.ksem = {}
        self.key_count = {}
        self.eng_base = {e: 0 for e in ENGS}
        self._reset()

    def _reset(self):
        self.ops = []
        self.per_eng = {e: [] for e in ENGS}
        self.last_w = {}
        self.readers = {}
        self.seen = {e: {f: -1 for f in ENGS} for e in ENGS}
        self.seen_dma = {e: {} for e in ENGS}
        self.eng_group = {e: None for e in ENGS}
        self.seen_saved = {e: None for e in ENGS}

    cur_group = None
    cnt_ap = None

    def _add(self, eng, fn, reads, writes, is_dma, key, group=None):
        o = _Op(len(self.ops), eng, fn, is_dma, key)
        o.group = group
        if group != self.eng_group[eng]:
            if self.eng_group[eng] is not None:
                self.seen[eng], self.seen_dma[eng] = self.seen_saved[eng]
            if group is not None:
                self.seen_saved[eng] = (dict(self.seen[eng]), dict(self.seen_dma[eng]))
            self.eng_group[eng] = group
        deps = set()
        for b in reads:
            w = self.last_w.get(b)
            if w is not None:
                deps.add(w)
        for b in writes:
            w = self.last_w.get(b)
            if w is not None:
                deps.add(w)
            for r in self.readers.get(b, ()):
                deps.add(r)
        for b in reads:
            self.readers.setdefault(b, []).append(o.idx)
        for b in writes:
            self.last_w[b] = o.idx
            self.readers[b] = []
        for di in sorted(deps):
            d = self.ops[di]
            if d.is_dma:
                if self.seen_dma[eng].get(d.key, 0) >= d.dma_val:
                    continue
                self.seen_dma[eng][d.key] = d.dma_val
                o.waits.append(("dma", d.key, d.dma_val))
            else:
                if d.eng == eng and not is_dma and eng == "pe":
                    continue
                if self.seen[eng][d.eng] >= d.idx:
                    continue
                self.seen[eng][d.eng] = d.idx
                d.signal = True
                o.waits.append(("eng", d.eng, d.idx))
        if is_dma:
            if key not in self.ksem:
                self.ksem[key] = self.es.enter_context(self.nc.semaphore("k%d" % len(self.ksem)))
                self.key_count[key] = 0
            self.key_count[key] += 16
            o.dma_val = self.key_count[key]
        self.ops.append(o)
        self.per_eng[eng].append(o)
        return o

    _cap = None

    def op(self, eng, fn, reads=(), writes=()):
        if self._cap is not None:
            self._cap.append((eng, fn, tuple(reads), tuple(writes), False, None, self.cur_group))
            return None
        return self._add(eng, fn, tuple(reads), tuple(writes), False, None, self.cur_group)

    def dma(self, eng, fn, key, reads=(), writes=()):
        if self._cap is not None:
            self._cap.append((eng, fn, tuple(reads), tuple(writes), True, key, self.cur_group))
            return None
        return self._add(eng, fn, tuple(reads), tuple(writes), True, key, self.cur_group)

    def capture(self, f, *a, **kw):
        assert self._cap is None
        self._cap = []
        try:
            r = f(*a, **kw)
        finally:
            c = self._cap
            self._cap = None
        return c, r

    def replay(self, items):
        for it in items:
            self._add(*it)

    limit = None
    nflush = 0

    def flush(self):
        self.nflush += 1
        if self.limit is not None and self.nflush > self.limit:
            for o in self.ops:
                if o.is_dma:
                    self.key_count[o.key] -= 16
            self._reset()
            return
        nc = self.nc
        ops = self.ops
        final = {}
        self.eng_base_prev = dict(self.eng_base)
        for e in ENGS:
            lst = [o for o in self.per_eng[e] if not o.is_dma]
            if e != "sp" and lst:
                lst[-1].signal = True
            n = self.eng_base[e]
            for o in self.per_eng[e]:
                if o.signal and not o.is_dma:
                    n += 1
                    o.milestone = n
            self.eng_base[e] = n
            final[e] = n
        esem, ksem = self.esem, self.ksem
        keyvals = dict(self.key_count)
        per_eng = self.per_eng

        regs = self.__dict__.setdefault("_regs", {})

        def emit_one(ename, eng, o):
            for w in o.waits:
                if w[0] == "dma":
                    eng.wait_ge(ksem[w[1]], w[2])
                else:
                    eng.wait_ge(esem[w[1]], ops[w[2]].milestone)
            inst = o.fn(eng)
            if o.is_dma:
                inst.then_inc(ksem[o.key], 16)
            elif o.signal:
                inst.then_inc(esem[ename], 1)

        def emit(ename, eng):
            lst = per_eng[ename]
            ms = self.eng_base_prev[ename]
            k = 0
            while k < len(lst):
                o = lst[k]
                if o.group is None:
                    emit_one(ename, eng, o)
                    if o.signal and not o.is_dma:
                        ms = o.milestone
                    k += 1
                    continue
                k2 = k
                while k2 < len(lst) and lst[k2].group == o.group:
                    k2 += 1
                run = lst[k:k2]
                if ename not in regs:
                    regs[ename] = eng.alloc_register("cnt_" + ename)
                r = regs[ename]
                eng.reg_load(r, self.cnt_ap(o.group[0]))
                nsig = sum(1 for x in run if x.signal and not x.is_dma)
                with eng.If_lt(r, o.group[1]):
                    if nsig:
                        if ms > 0:
                            eng.wait_ge(esem[ename], ms)
                        eng.sem_inc(esem[ename], nsig)
                    for x in run:
                        if x.is_dma:
                            if x.dma_val > 16:
                                eng.wait_ge(ksem[x.key], x.dma_val - 16)
                            eng.sem_inc(ksem[x.key], 16)
                with eng.Else():
                    for x in run:
                        emit_one(ename, eng, x)
                ms += nsig
                k = k2
            for kk, v in keyvals.items():
                if v > 0:
                    eng.wait_ge(ksem[kk], v)
            for f in ENGS:
                if f != ename and f != "sp" and final[f] > 0:
                    eng.wait_ge(esem[f], final[f])

        with nc.Block() as block:
            @block.tensor
            def _(eng):
                emit("pe", eng)

            @block.scalar
            def _(eng):
                emit("act", eng)

            @block.vector
            def _(eng):
                emit("dve", eng)

            @block.gpsimd
            def _(eng):
                emit("pool", eng)

            @block.sync
            def _(eng):
                emit("sp", eng)
        self._reset()


def _rope_tables():
    p = np.arange(128)
    j = p % 16
    inv_freq = (10000.0 ** (-(j.astype(np.float64)) / 16.0))
    t = np.arange(SEQ)
    rows = t // 64
    cols = t % 64
    pos = np.where(((p % 64) < 32)[:, None], rows[None, :], cols[None, :]).astype(np.float64)
    ang = pos * inv_freq[:, None]
    ang = (pos.astype(np.float32) * inv_freq.astype(np.float32)[:, None]).astype(np.float32)
    cosT = np.cos(ang).astype(np.float32)
    sinT = np.sin(ang).astype(np.float32)
    sign = np.where((p % 32) < 16, -1.0, 1.0).astype(np.float32)[:, None]
    sinT = sinT * sign
    perm = np.where((p % 32) < 16, p + 16, p - 16)
    Pm = np.zeros((128, 128), np.float32)
    Pm[perm, p] = 1.0
    return cosT, sinT, Pm


def _band_masks():
    k = np.arange(128)[:, None]
    q = np.arange(128)[None, :]
    prev = (k >= q).astype(np.float32)
    nxt = (k <= q).astype(np.float32)
    return np.stack([prev, nxt], 0)


def _nat_patterns():
    pats = {}
    plist = []
    chunks = {}
    for qt in range(16):
        r0, r1 = 2 * qt, 2 * qt + 1
        rs0 = min(max(r0 - 4, 0), 24)
        rs1 = min(max(r1 - 4, 0), 24)
        lo = rs0 // 2
        hi = (rs1 + 7) // 2
        chunks[qt] = list(range(lo, hi + 1))
        for kc in chunks[qt]:
            interior = 2 <= qt <= 13
            key = ("i", kc - qt) if interior else ("b", qt, kc)
            if key not in pats:
                pats[key] = len(plist)
                plist.append((qt, kc))
            pats[(qt, kc)] = pats[key]
    npat = len(plist)
    dr = np.zeros((npat, 128, 128), np.int64)
    dc = np.zeros((npat, 128, 128), np.int64)
    mk = np.zeros((npat, 128, 128), np.float32)
    kk = np.arange(128)
    for pi, (qt, kc) in enumerate(plist):
        kr = 2 * kc + kk // 64
        kcol = kk % 64
        qr = 2 * qt + kk // 64
        qcol = kk % 64
        rs = np.clip(qr - 4, 0, 24)
        qs = np.clip(qcol - 8, 0, 48)
        vrow = (kr[:, None] >= rs[None, :]) & (kr[:, None] < rs[None, :] + 8)
        vcol = (kcol[:, None] >= qs[None, :]) & (kcol[:, None] < qs[None, :] + 16)
        mk[pi] = (vrow & vcol).astype(np.float32)
        dr[pi] = np.clip(kr[:, None] - qr[None, :] + 7, 0, 14)
        dc[pi] = np.clip(kcol[:, None] - qcol[None, :] + 15, 0, 30)
    return pats, chunks, dr, dc, mk, npat


_NATP = _nat_patterns()
NPAT = _NATP[5]


def build_program(limit=None, debug=False):
    nc = bass.Bass("TRN2", target_bir_lowering=False)

    _uid = [0]

    def un(name):
        _uid[0] += 1
        return "%s_u%d" % (name, _uid[0])

    def din(name, shape, dt=F32):
        return nc.dram_tensor(name, list(shape), dt, kind="ExternalInput").ap()

    x_in = din("x", [NB, SEQ, D])
    ctx_in = din("ctx", [NB, LCTX, D])
    c_in = din("c", [NB, D])
    cctx_in = din("c_ctx", [1, D])
    w_ada = din("w_ada", [DEPTH, D, 6 * D])
    b_ada = din("b_ada", [DEPTH, 6 * D])
    ln1_g = din("ln1_g", [DEPTH, D])
    ln1_b = din("ln1_b", [DEPTH, D])
    ln2_g = din("ln2_g", [DEPTH, D])
    ln2_b = din("ln2_b", [DEPTH, D])
    attn_wqk = din("attn_wqk", [2, D, 1536])
    attn_wv = din("attn_wv", [2, D, 256])
    attn_wo = din("attn_w_o", [2, D, D])
    attn_sink = din("attn_sink", [2, 16])
    conv_w_in = din("conv_w_in", [1, D, 3 * D])
    conv_w = din("conv_w", [1, 3, D])
    conv_w_out = din("conv_w_out", [1, D, D])
    nat_wqk = din("nat_wqk", [1, D, 2048])
    nat_wv = din("nat_wv", [1, D, 1024])
    nat_wo = din("nat_w_o", [1, D, D])
    nat_bias = din("nat_bias", [NPAT, 128, 16, 128])
    router_w = din("router_w", [DEPTH, D, 36])
    router_b = din("router_b", [DEPTH, 36])
    w_gate = din("expert_w_gate", [DEPTH, NEXP, D, FF])
    w_up = din("expert_w_up", [DEPTH, NEXP, D, FF])
    w_down = din("expert_w_down", [DEPTH, NEXP, FF, D])
    k_ident = din("k_ident", [128, 128])
    k_cos = din("k_cos", [128, SEQ])
    k_sin = din("k_sin", [128, SEQ])
    k_pm = din("k_pm", [128, 128])
    k_band = din("k_band", [2, 128, 128])
    k_natmask = din("k_natmask", [NPAT, 128, 128])
    k_ustrict = din("k_ustrict", [128, 128])
    k_eoff = din("k_eoff", [1, 32])

    out = nc.dram_tensor("out", [NB, SEQ, D], F32, kind="ExternalOutput").ap()

    X = nc.dram_tensor("X_scr", [NT * 128, D], F32).ap()
    MODR = nc.dram_tensor("MODR_scr", [DEPTH, 3, 6, D], F32).ap()
    XS = nc.dram_tensor("XS_scr", [NSLOT, D], BF16).ap()
    YS = nc.dram_tensor("YS_scr", [NSLOT, D], F32).ap()
    ETAB = nc.dram_tensor("ETAB_scr", [NPAT, 128, 16, 128], BF16).ap()
    QT = nc.dram_tensor("QT_scr", [128, 8, 2304], BF16).ap()

    DBG = {}
    if debug:
        DBG["T"] = nc.dram_tensor("dbgT", [4, 128, D], F32, kind="ExternalOutput").ap()
        DBG["O"] = nc.dram_tensor("dbgO", [128, D], BF16, kind="ExternalOutput").ap()
        DBG["Den"] = nc.dram_tensor("dbgDen", [128, 16], F32, kind="ExternalOutput").ap()
        DBG["H2"] = nc.dram_tensor("dbgH2", [128, D], BF16, kind="ExternalOutput").ap()
        DBG["RT"] = nc.dram_tensor("dbgRT", [128, 256], F32, kind="ExternalOutput").ap()
        DBG["tile"] = 0
        DBG["OA"] = nc.dram_tensor("dbgOA", [128, 3, 512], F32, kind="ExternalOutput").ap()
        DBG["SK"] = nc.dram_tensor("dbgSK", [128, 16], F32, kind="ExternalOutput").ap()

    def dbg_store(key, idx, ap, bid, tt):
        if not DBG or tt != DBG["tile"] or DBG.get("done_" + key + str(idx)):
            return
        DBG["done_" + key + str(idx)] = True
        dst = DBG[key][idx] if idx is not None else DBG[key]
        S.dma("sp", lambda e: e.dma_start(out=dst, in_=ap), "dbg" + key + str(idx), reads=[bid])

    with ExitStack() as ges:
        S = Sched(nc, ges)
        S.limit = limit

        def gsb(name, shape, dt):
            return ges.enter_context(nc.sbuf_tensor(un(name), list(shape), dt))

        identf = gsb("identf", [128, 128], F32)
        identb = gsb("identb", [128, 128], BF16)
        ustrict = gsb("ustrict", [128, 128], BF16)
        onesb = gsb("onesb", [128, 128], BF16)
        zerob = gsb("zerob", [128, 512], BF16)
        ones13 = gsb("ones13", [1, 4], F32)
        csT = gsb("csT", [128, 8, 3], F32)
        eoff = gsb("eoff", [128, 32], F32)
        destI = gsb("destI", [128, NT, 2], I32)
        gates = gsb("gates", [128, NT, 2], F32)
        cnt = gsb("cnt", [128, 32], F32)
        cnti = gsb("cnti", [128, 32], I32)
        S.cnt_ap = lambda idx: cnti[0:1, idx:idx + 1]

        _preg = {}

        def breg(e):
            if "r" not in _preg:
                _preg["r"] = e.to_reg(NSLOT - 1)
            return _preg["r"]

        def tile_src(layer, tt):
            b, j = divmod(tt, TPB)
            if layer == 0:
                if j < 16:
                    return x_in[b, j * 128:(j + 1) * 128, :]
                return ctx_in[b, (j - 16) * 128:(j - 15) * 128, :]
            return X[tt * 128:(tt + 1) * 128, :]

        def phase_init():
            with ExitStack() as es:
                tmpf = es.enter_context(nc.sbuf_tensor(un("init_tmp"), [128, 128], F32))
                S.dma("sp", lambda e: e.dma_start(out=identf[:], in_=k_ident), "c0", writes=["identf"])
                S.op("dve", lambda e: e.tensor_copy(out=identb[:], in_=identf[:]), reads=["identf"], writes=["identb"])
                S.dma("sp", lambda e: e.dma_start(out=tmpf[:], in_=k_ustrict), "c1", writes=["tmpf"])
                S.op("dve", lambda e: e.tensor_copy(out=ustrict[:], in_=tmpf[:]), reads=["tmpf"], writes=["ustrict"])
                S.op("pool", lambda e: e.memset(onesb[:], 1.0), writes=["onesb"])
                S.op("pool", lambda e: e.memset(zerob[:], 0.0), writes=["zerob"])
                S.op("pool", lambda e: e.memset(ones13[:], 1.0), writes=["ones13"])
                for v in range(3):
                    srcv = c_in[v, :] if v < 2 else cctx_in[0, :]
                    S.dma("sp", lambda e, v=v, srcv=srcv: e.dma_start(out=csT[:, :, v], in_=srcv.rearrange("(c p) -> p c", p=128),
                                                                       allow_slow_non_contiguous=True), "c2", writes=["csT"])
                S.op("act", lambda e: e.activation(out=csT[:], in_=csT[:], func=AF.Silu), reads=["csT"], writes=["csT"])
                S.dma("sp", lambda e: e.dma_start(out=eoff[:], in_=k_eoff[0, :].partition_broadcast(128)), "c4", writes=["eoff"])
                S.flush()

        def phase_ada(i):
            with ExitStack() as es:
                def sb(name, shape, dt):
                    return es.enter_context(nc.sbuf_tensor(un(name), list(shape), dt))
                wst = [sb("ada_w%d" % k, [128, 8, 512], F32) for k in range(2)]
                brow = sb("ada_b", [1, 6 * D], F32)
                modrow = sb("ada_mod", [3, 6 * D], F32)
                lng = sb("ada_lng", [3, 2, D], F32)
                outrow = sb("ada_out", [3, 6, D], F32)
                tmp = sb("ada_tmp", [3, D], F32)
                pm = [es.enter_context(nc.psum_tensor(un("ada_ps%d" % k), [3, 512], F32)) for k in range(2)]
                S.dma("sp", lambda e: e.dma_start(out=brow[:], in_=b_ada[i:i + 1, :]), "ab", writes=["brow"])
                S.dma("sp", lambda e: e.dma_start(out=lng[:, 0, :], in_=ln1_g[i, :].partition_broadcast(3)), "al", writes=["lng"])
                S.dma("sp", lambda e: e.dma_start(out=lng[:, 1, :], in_=ln1_b[i, :].partition_broadcast(3)), "al", writes=["lng"])
                wv = w_ada[i].rearrange("(c p) n -> p c n", p=128)
                for n in range(12):
                    k = n % 2
                    S.dma("sp", lambda e, n=n, k=k: e.dma_start(out=wst[k][:], in_=wv[:, :, n * 512:(n + 1) * 512]),
                          "aw%d" % k, writes=["wst%d" % k])
                    for c in range(8):
                        S.op("pe", lambda e, c=c, k=k: e.matmul(pm[k][:], lhsT=csT[:, c, :], rhs=wst[k][:, c, :],
                                                                 start=(c == 0), stop=False),
                             reads=["csT", "wst%d" % k], writes=["pm%d" % k])
                    S.op("pe", lambda e, n=n, k=k: e.matmul(pm[k][:], lhsT=ones13[0:1, 0:3], rhs=brow[0:1, n * 512:(n + 1) * 512],
                                                             start=False, stop=True),
                         reads=["ones13", "brow"], writes=["pm%d" % k])
                    S.op("act", lambda e, n=n, k=k: e.copy(out=modrow[:, n * 512:(n + 1) * 512], in_=pm[k][:]),
                         reads=["pm%d" % k], writes=["modrow"])

                def m(k):
                    return modrow[:, k * D:(k + 1) * D]
                S.op("dve", lambda e: e.tensor_copy(out=outrow[:, 0, :], in_=m(0)), reads=["modrow"], writes=["o0"])
                S.op("dve", lambda e: e.tensor_scalar_add(out=outrow[:, 1, :], in0=m(1), scalar1=1.0), reads=["modrow"], writes=["o1"])
                S.op("dve", lambda e: e.tensor_scalar_mul(out=outrow[:, 2, :], in0=m(2), scalar1=INV_ALPHA), reads=["modrow"], writes=["o2"])
                S.op("dve", lambda e: e.tensor_scalar_add(out=tmp[:], in0=m(4), scalar1=1.0), reads=["modrow"], writes=["tmp"])
                S.op("dve", lambda e: e.tensor_tensor(out=outrow[:, 3, :], in0=tmp[:], in1=lng[:, 0, :], op=ALU.mult),
                     reads=["tmp", "lng"], writes=["o3"])
                S.op("dve", lambda e: e.tensor_tensor(out=outrow[:, 4, :], in0=tmp[:], in1=lng[:, 1, :], op=ALU.mult),
                     reads=["tmp", "lng"], writes=["o4"])
                S.op("dve", lambda e: e.tensor_tensor(out=outrow[:, 4, :], in0=outrow[:, 4, :], in1=m(3), op=ALU.add),
                     reads=["o4", "modrow"], writes=["o4"])
                S.op("dve", lambda e: e.tensor_scalar_mul(out=outrow[:, 5, :], in0=m(5), scalar1=INV_ALPHA), reads=["modrow"], writes=["o5"])
                S.dma("sp", lambda e: e.dma_start(out=MODR[i], in_=outrow[:]), "ao",
                      reads=["o0", "o1", "o2", "o3", "o4", "o5"], writes=["MODR"])
                S.flush()

        class Rot:
            def __init__(self, es, name, n, shape, dt, psum=False):
                self.bufs = []
                for k in range(n):
                    if psum:
                        t = es.enter_context(nc.psum_tensor(un("%s%d" % (name, k)), list(shape), dt))
                    else:
                        t = es.enter_context(nc.sbuf_tensor(un("%s%d" % (name, k)), list(shape), dt))
                    self.bufs.append((t, "%s%d" % (name, k)))
                self.i = 0

            def next(self):
                r = self.bufs[self.i % len(self.bufs)]
                self.i += 1
                return r

        def layer_norm_core(es_bufs, t, t_id, xn, xn_id):
            st, mv, rs = es_bufs
            stt, st_id = st.next()
            mvt, mv_id = mv.next()
            rst, rs_id = rs.next()
            S.op("dve", lambda e: e.bn_stats(out=stt[:, 0, :], in_=t[:, 0:512]), reads=[t_id], writes=[st_id])
            S.op("dve", lambda e: e.bn_stats(out=stt[:, 1, :], in_=t[:, 512:1024]), reads=[t_id], writes=[st_id])
            S.op("dve", lambda e: e.bn_aggr(out=mvt[:], in_=stt[:].rearrange("p a b -> p (a b)")), reads=[st_id], writes=[mv_id])
            S.op("dve", lambda e: e.tensor_scalar_add(out=rst[:, 0:1], in0=mvt[:, 1:2], scalar1=EPS2), reads=[mv_id], writes=[rs_id])
            S.op("act", lambda e: e.activation(out=rst[:, 0:1], in_=rst[:, 0:1], func=AF.Ln), reads=[rs_id], writes=[rs_id])
            S.op("act", lambda e: e.activation(out=rst[:, 0:1], in_=rst[:, 0:1], func=AF.Exp, scale=-0.5), reads=[rs_id], writes=[rs_id])
            S.op("dve", lambda e: e.scalar_tensor_tensor(out=rst[:, 1:2], in0=mvt[:, 0:1], scalar=-1.0, in1=rst[:, 0:1],
                                                         op0=ALU.mult, op1=ALU.mult), reads=[mv_id, rs_id], writes=[rs_id])
            S.op("act", lambda e: e.activation(out=xn[:], in_=t[:], func=AF.Identity, scale=rst[:, 0:1], bias=rst[:, 1:2]),
                 reads=[t_id, rs_id], writes=[xn_id])

        class MixCtx:
            pass

        def mixer_common_alloc(es, i):
            M = MixCtx()

            def sb(name, shape, dt):
                return es.enter_context(nc.sbuf_tensor(un(name), list(shape), dt))
            M.G1 = sb("mx_G1", [128, D], F32)
            M.A2 = sb("mx_A2", [128, D], F32)
            M.S2 = sb("mx_S2", [128, D], F32)
            M.lg = sb("mx_lg", [128, D], F32)
            M.lb = sb("mx_lb", [128, D], F32)
            M.wr = sb("mx_wr", [128, 8, 36], BF16)
            M.wrf = sb("mx_wrf", [128, 8, 36], F32)
            M.rb = sb("mx_rb", [128, 36], F32)
            M.xt = Rot(es, "mx_xt", 2, [128, D], F32)
            M.t = Rot(es, "mx_t", 2, [128, D], F32)
            M.xn = Rot(es, "mx_xn", 2, [128, D], F32)
            M.h2 = Rot(es, "mx_h2", 2, [128, D], BF16)
            M.h2T = Rot(es, "mx_h2T", 2, [128, 8, 128], BF16)
            M.st = Rot(es, "mx_st", 2, [128, 2, 6], F32)
            M.mv = Rot(es, "mx_mv", 2, [128, 2], F32)
            M.rs = Rot(es, "mx_rs", 2, [128, 2], F32)
            M.rt = Rot(es, "mx_rt", 2, [128, 256], F32)
            M.selb = Rot(es, "mx_sel", 2, [128, 32], BF16)
            M.cur_v = None
            S.dma("pool", lambda e: e.dma_start(out=M.wr[:], in_=router_w[i].rearrange("(c p) n -> p c n", p=128)), "mwr", writes=["wr"])
            S.dma("sp", lambda e: e.dma_start(out=M.rb[:], in_=router_b[i, :].partition_broadcast(128)), "mrb", writes=["rb"])
            S.dma("sp", lambda e: e.dma_start(out=M.lg[:], in_=ln1_g[i, :].partition_broadcast(128)), "mlg", writes=["lg"])
            S.dma("sp", lambda e: e.dma_start(out=M.lb[:], in_=ln1_b[i, :].partition_broadcast(128)), "mlb", writes=["lb"])
            return M

        def load_variant(M, i, v):
            if M.cur_v == v:
                return
            M.cur_v = v
            S.dma("sp", lambda e: e.dma_start(out=M.G1[:], in_=MODR[i, v, 2, :].partition_broadcast(128)), "mG1", writes=["G1"])
            S.dma("sp", lambda e: e.dma_start(out=M.A2[:], in_=MODR[i, v, 3, :].partition_broadcast(128)), "mA2", writes=["A2"])
            S.dma("sp", lambda e: e.dma_start(out=M.S2[:], in_=MODR[i, v, 4, :].partition_broadcast(128)), "mS2", writes=["S2"])

        def out_stage(M, P, i, tt, po, po_ids):
            xt, xt_id = M.xt.next()
            t, t_id = M.t.next()
            xn, xn_id = M.xn.next()
            h2, h2_id = M.h2.next()
            h2T, h2T_id = M.h2T.next()
            rt, rt_id = M.rt.next()
            selb, selb_id = M.selb.next()
            src = tile_src(i, tt)
            S.dma("sp", lambda e: e.dma_start(out=xt[:], in_=src), xt_id, writes=[xt_id])
            S.op("dve", lambda e: e.tensor_tensor(out=t[:], in0=po, in1=M.G1[:], op=ALU.mult), reads=list(po_ids) + ["G1"], writes=[t_id])
            dbg_store("T", 0, t[:], t_id, tt)
            S.op("dve", lambda e: e.tensor_tensor(out=t[:], in0=t[:], in1=xt[:], op=ALU.add), reads=[t_id, xt_id], writes=[t_id])
            dbg_store("T", 1, t[:], t_id, tt)
            layer_norm_core((M.st, M.mv, M.rs), t, t_id, xn, xn_id)
            dbg_store("T", 2, xn[:], xn_id, tt)
            S.op("dve", lambda e: e.tensor_tensor(out=t[:], in0=xn[:], in1=M.A2[:], op=ALU.mult), reads=[xn_id, "A2"], writes=[t_id])
            S.op("dve", lambda e: e.tensor_tensor(out=h2[:], in0=t[:], in1=M.S2[:], op=ALU.add), reads=[t_id, "S2"], writes=[h2_id])
            dbg_store("H2", None, h2[:], h2_id, tt)
            S.op("dve", lambda e: e.tensor_tensor(out=xn[:], in0=xn[:], in1=M.lg[:], op=ALU.mult), reads=[xn_id, "lg"], writes=[xn_id])
            S.op("dve", lambda e: e.tensor_tensor(out=xn[:], in0=xn[:], in1=M.lb[:], op=ALU.add), reads=[xn_id, "lb"], writes=[xn_id])
            S.dma("sp", lambda e: e.dma_start(out=X[tt * 128:(tt + 1) * 128, :], in_=xn[:]), "st_" + xn_id, reads=[xn_id], writes=["X%d" % tt])
            for hf4 in range(2):
                for c in range(4):
                    S.op("pe", lambda e, c=c, hf4=hf4: e.transpose(out=P.ptb[:, c, :], in_=h2[:, (hf4 * 4 + c) * 128:(hf4 * 4 + c + 1) * 128], identity=identb[:]),
                         reads=[h2_id, "identb"], writes=["ptb"])
                S.op("act", lambda e, hf4=hf4: e.copy(out=h2T[:, hf4 * 4:(hf4 + 1) * 4, :], in_=P.ptb), reads=["ptb"], writes=[h2T_id])
            for c in range(8):
                S.op("pe", lambda e, c=c: e.matmul(P.plg[:, 0:36], lhsT=h2T[:, c, :], rhs=M.wr[:, c, :], start=(c == 0), stop=(c == 7)),
                     reads=[h2T_id, "wr"], writes=["plg"])
            if not (KDBG & 2):
                route(M, P, tt, rt, rt_id, selb, selb_id)
            dbg_store("RT", None, rt[:], rt_id, tt)
            for k in range(2 if not (KDBG & 6) else 0):
                S.dma("pool", lambda e, k=k: e.indirect_dma_start(
                    out=XS, out_offset=bass.IndirectOffsetOnAxis(ap=destI[:, tt, k:k + 1], axis=0),
                    in_=h2[:], in_offset=None, bounds_check=breg(e), oob_is_err=False),
                    "sc_" + h2_id + str(k), reads=[h2_id, "destI%d" % tt], writes=["XS"])

        def route(M, P, tt, rt, rt_id, selb, selb_id):
            lgt = rt[:, 0:36]
            gl = rt[:, 0:4]
            el = rt[:, 4:36]
            gm = rt[:, 40:44]
            pen = rt[:, 44:48]
            em = rt[:, 48:80]
            oh1 = rt[:, 80:112]
            em2 = rt[:, 112:144]
            oh2 = rt[:, 144:176]
            slot = rt[:, 176:208]
            prod = rt[:, 208:240]
            sc = rt[:, 240:256]
            gex = rt[:, 36:40]
            W = [rt_id]
            R = [rt_id]

            def dv(fn, extra_r=(), extra_w=()):
                S.op("dve", fn, reads=R + list(extra_r), writes=W + list(extra_w))
            dv(lambda e: e.tensor_tensor(out=lgt, in0=P.plg[:, 0:36], in1=M.rb[:], op=ALU.add), extra_r=["plg", "rb"])
            dv(lambda e: e.reduce_max(out=sc[:, 0:1], in_=gl, axis=AX))
            dv(lambda e: e.tensor_scalar(out=gm, in0=gl, scalar1=sc[:, 0:1], scalar2=None, op0=ALU.is_ge))
            dv(lambda e: e.tensor_scalar_mul(out=sc[:, 1:2], in0=sc[:, 0:1], scalar1=-1.0))
            S.op("act", lambda e: e.activation(out=gex, in_=gl, func=AF.Exp, bias=sc[:, 1:2], scale=1.0, accum_out=sc[:, 2:3]),
                 reads=R, writes=W)
            dv(lambda e: e.reciprocal(out=sc[:, 3:4], in_=sc[:, 2:3]))
            dv(lambda e: e.tensor_scalar(out=pen, in0=gm, scalar1=BIG, scalar2=-BIG, op0=ALU.mult, op1=ALU.add))
            dv(lambda e: e.tensor_tensor(out=em.rearrange("p (g k) -> p g k", k=8), in0=el.rearrange("p (g k) -> p g k", k=8),
                                         in1=pen.unsqueeze(2).to_broadcast([128, 4, 8]), op=ALU.add))
            dv(lambda e: e.reduce_max(out=sc[:, 4:5], in_=em, axis=AX))
            dv(lambda e: e.tensor_scalar(out=oh1, in0=em, scalar1=sc[:, 4:5], scalar2=None, op0=ALU.is_ge))
            dv(lambda e: e.scalar_tensor_tensor(out=em2, in0=oh1, scalar=-BIG, in1=em, op0=ALU.mult, op1=ALU.add))
            dv(lambda e: e.reduce_max(out=sc[:, 5:6], in_=em2, axis=AX))
            dv(lambda e: e.tensor_scalar(out=oh2, in0=em2, scalar1=sc[:, 5:6], scalar2=None, op0=ALU.is_ge))
            dv(lambda e: e.tensor_tensor(out=sc[:, 6:7], in0=sc[:, 5:6], in1=sc[:, 4:5], op=ALU.subtract))
            S.op("act", lambda e: e.activation(out=sc[:, 7:8], in_=sc[:, 6:7], func=AF.Exp), reads=R, writes=W)
            dv(lambda e: e.tensor_scalar_add(out=sc[:, 7:8], in0=sc[:, 7:8], scalar1=1.0))
            dv(lambda e: e.reciprocal(out=sc[:, 8:9], in_=sc[:, 7:8]))
            dv(lambda e: e.tensor_tensor(out=gates[:, tt, 0:1], in0=sc[:, 8:9], in1=sc[:, 3:4], op=ALU.mult), extra_w=["gates%d" % tt])
            dv(lambda e: e.tensor_tensor(out=gates[:, tt, 1:2], in0=sc[:, 3:4], in1=gates[:, tt, 0:1], op=ALU.subtract),
               extra_r=["gates%d" % tt], extra_w=["gates%d" % tt])
            dv(lambda e: e.tensor_tensor(out=selb[:], in0=oh1, in1=oh2, op=ALU.add), extra_w=[selb_id])
            S.op("pe", lambda e: e.matmul(P.plg[:, 64:96], lhsT=ustrict[:], rhs=selb[:], start=True, stop=True),
                 reads=[selb_id, "ustrict"], writes=["prk"])
            S.op("pe", lambda e: e.matmul(P.plg[:, 128:160], lhsT=onesb[:], rhs=selb[:], start=True, stop=True),
                 reads=[selb_id, "onesb"], writes=["ptot"])
            dv(lambda e: e.tensor_tensor(out=slot, in0=P.plg[:, 64:96], in1=cnt[:], op=ALU.add), extra_r=["prk", "cnt"])
            dv(lambda e: e.tensor_scalar(out=prod, in0=slot, scalar1=float(CAP), scalar2=4.0e6, op0=ALU.is_ge, op1=ALU.mult))
            dv(lambda e: e.tensor_tensor(out=slot, in0=slot, in1=prod, op=ALU.add))
            dv(lambda e: e.tensor_tensor(out=slot, in0=slot, in1=eoff[:], op=ALU.add), extra_r=["eoff"])
            dv(lambda e: e.tensor_tensor(out=cnt[:], in0=cnt[:], in1=P.plg[:, 128:160], op=ALU.add), extra_r=["ptot", "cnt"], extra_w=["cnt"])
            dv(lambda e: e.tensor_tensor(out=prod, in0=oh1, in1=slot, op=ALU.mult))
            dv(lambda e: e.reduce_sum(out=sc[:, 9:10], in_=prod, axis=AX))
            dv(lambda e: e.tensor_tensor(out=prod, in0=oh2, in1=slot, op=ALU.mult))
            dv(lambda e: e.reduce_sum(out=sc[:, 10:11], in_=prod, axis=AX))
            dv(lambda e: e.tensor_copy(out=destI[:, tt, :], in_=sc[:, 9:11]), extra_w=["destI%d" % tt])

        def make_hT(es, P, i, b, hT, scal):
            xr = Rot(es, "hx_x", 3, [128, D], F32)
            for seg, v in ((0, b), (1, 2)):
                S.dma("sp", lambda e, v=v: e.dma_start(out=scal[:], in_=MODR[i, v, 0:2, :].rearrange("k (c p) -> p k c", p=128),
                                                        allow_slow_non_contiguous=True), "hsc", writes=["scal"])
                tiles = range(16) if seg == 0 else range(16, 18)
                for j in tiles:
                    tt = b * TPB + j
                    xt, xid = xr.next()
                    src = tile_src(i, tt)
                    S.dma("sp", lambda e, xt=xt, src=src: e.dma_start(out=xt[:], in_=src), xid, reads=["X%d" % tt], writes=[xid])
                    for c in range(8):
                        S.op("pe", lambda e, c=c, xt=xt: e.transpose(out=P.ptf[:, c, :], in_=xt[:, c * 128:(c + 1) * 128], identity=identf[:]),
                             reads=[xid, "identf"], writes=["ptf"])
                    for c in range(8):
                        if c % 2 == 0:
                            S.op("act", lambda e, c=c, j=j: e.activation(out=hT[:, c, j * 128:(j + 1) * 128], in_=P.ptf[:, c, :], func=AF.Identity,
                                                                          scale=scal[:, 1, c:c + 1], bias=scal[:, 0, c:c + 1]),
                                 reads=["ptf", "scal"], writes=["hT"])
                        else:
                            S.op("dve", lambda e, c=c, j=j: e.tensor_scalar(out=hT[:, c, j * 128:(j + 1) * 128], in0=P.ptf[:, c, :],
                                                                             scalar1=scal[:, 1, c:c + 1], scalar2=scal[:, 0, c:c + 1],
                                                                             op0=ALU.mult, op1=ALU.add),
                                 reads=["ptf", "scal"], writes=["hT"])

        HB = [(0, h) if h < 6 else ((1, h - 6) if h < 11 else (2, h - 11)) for h in range(16)]
        TOKG = [(0, 512), (512, 512), (1024, 512), (1536, 512), (2048, 256)]

        class PsumSet:
            pass

        def alloc_psum(es):
            P = PsumSet()

            def ps(name, shape, dt):
                return es.enter_context(nc.psum_tensor(un(name), list(shape), dt))
            P.ptf = ps("ps_ptf", [128, 8, 128], F32)
            P.pA = ps("ps_A", [128, 512], F32)
            P.pB = ps("ps_B", [128, 512], F32)
            P.oacc = ps("ps_oacc", [128, 3, 512], F32)
            P.b7 = ps("ps_b7", [128, 512], F32)
            P.ptb = P.b7[:, 0:256].bitcast(BF16).rearrange("p (c t) -> p c t", t=128)
            P.plg = P.b7[:, 256:512]
            P.pC = P.oacc[:, 0, :]
            return P

        def phase_attn(i, j, kind, last):
            is_a = kind == 0
            nkv = 4 if is_a else 16
            nqk_cols = 1536 if is_a else 2048
            nk_chunks = 4 if is_a else 8
            nv_cols = 256 if is_a else 1024
            wqk = attn_wqk[j] if is_a else nat_wqk[j]
            wv = attn_wv[j] if is_a else nat_wv[j]
            wo = attn_wo[j] if is_a else nat_wo[j]
            scale = 0.125
            if not is_a:
                pats, nat_chunks = _NATP[0], _NATP[1]
            for b in range(NB):
                with ExitStack() as es:
                    def sb(name, shape, dt):
                        return es.enter_context(nc.sbuf_tensor(un(name), list(shape), dt))
                    P = alloc_psum(es)
                    kT = sb("at_kT", [128, nk_chunks, 2304], BF16)
                    va = sb("at_va", [128, TPB, nkv, 65], BF16)
                    S.op("pool", lambda e: e.memset(va[:], 1.0), writes=["va"])
                    with ExitStack() as es1:
                        def sb1(name, shape, dt):
                            return es1.enter_context(nc.sbuf_tensor(un(name), list(shape), dt))
                        hT = sb1("at_hT", [128, 8, 2304], BF16)
                        scal = sb1("at_scal", [128, 2, 8], F32)
                        wstg = Rot(es1, "at_wst", 2, [128, 8, 256], F32)
                        wbf = Rot(es1, "at_wbf", 2, [128, 8, 256], BF16)
                        qst = Rot(es1, "at_qst", 2, [128, 512], BF16)
                        if is_a:
                            raw = Rot(es1, "at_raw", 2, [128, 512], BF16)
                            tm1 = Rot(es1, "at_tm1", 2, [128, 512], F32)
                            tm2 = Rot(es1, "at_tm2", 2, [128, 512], F32)
                            cosT = sb1("at_cos", [128, SEQ], F32)
                            sinT = sb1("at_sin", [128, SEQ], F32)
                            pmf = sb1("at_pmf", [128, 128], F32)
                            pmb = sb1("at_pmb", [128, 128], BF16)
                            S.dma("sp", lambda e: e.dma_start(out=cosT[:], in_=k_cos), "rc", writes=["cosT"])
                            S.dma("sp", lambda e: e.dma_start(out=sinT[:], in_=k_sin), "rs", writes=["sinT"])
                            S.dma("sp", lambda e: e.dma_start(out=pmf[:], in_=k_pm), "rp", writes=["pmf"])
                            S.op("dve", lambda e: e.tensor_copy(out=pmb[:], in_=pmf[:]), reads=["pmf"], writes=["pmb"])
                        make_hT(es1, P, i, b, hT, scal)
                        wqk_v = wqk.rearrange("(c p) n -> p c n", p=128)
                        wv_v = wv.rearrange("(c p) n -> p c n", p=128)
                        pab = [(P.pA, "pA"), (P.pB, "pB")]
                        pcount = 0
                        for g in range(nqk_cols // 256):
                            wst, wst_id = wstg.next()
                            wb, wb_id = wbf.next()
                            S.dma("pool", lambda e, wb=wb, g=g: e.dma_start(out=wb[:], in_=wqk_v[:, :, g * 256:(g + 1) * 256]),
                                  "ld" + wb_id, writes=[wb_id])
                            for cc in range(2):
                                chunk = g * 2 + cc
                                isq = chunk < 8
                                dchunk = chunk if isq else chunk - 8
                                for (t0, tn) in TOKG:
                                    if isq:
                                        qs_, dst_id = qst.next()
                                        dst_ap = qs_[:, 0:tn]
                                    else:
                                        dst_ap = kT[:, dchunk, t0:t0 + tn]
                                        dst_id = "kT"
                                    pp, pp_id = pab[pcount % 2]
                                    pcount += 1
                                    for c in range(8):
                                        S.op("pe", lambda e, pp=pp, wb=wb, cc=cc, c=c, t0=t0, tn=tn: e.matmul(
                                            pp[:, 0:tn], lhsT=wb[:, c, cc * 128:(cc + 1) * 128], rhs=hT[:, c, t0:t0 + tn],
                                            start=(c == 0), stop=(c == 7)), reads=[wb_id, "hT"], writes=[pp_id])
                                    rope = is_a and t0 < SEQ
                                    if not rope:
                                        S.op("act", lambda e, pp=pp, dst_ap=dst_ap, tn=tn: e.copy(
                                            out=dst_ap, in_=pp[:, 0:tn]), reads=[pp_id], writes=[dst_id])
                                    else:
                                        rw, rw_id = raw.next()
                                        a1, a1_id = tm1.next()
                                        a2, a2_id = tm2.next()
                                        S.op("act", lambda e, pp=pp, rw=rw: e.copy(out=rw[:], in_=pp[:]), reads=[pp_id], writes=[rw_id])
                                        S.op("pe", lambda e, rw=rw: e.matmul(P.pC, lhsT=pmb[:], rhs=rw[:], start=True, stop=True),
                                             reads=[rw_id, "pmb"], writes=["pC"])
                                        S.op("dve", lambda e, a1=a1, t0=t0: e.tensor_tensor(out=a1[:], in0=P.pC, in1=sinT[:, t0:t0 + 512], op=ALU.mult),
                                             reads=["pC", "sinT"], writes=[a1_id])
                                        S.op("dve", lambda e, a2=a2, rw=rw, t0=t0: e.tensor_tensor(out=a2[:], in0=rw[:], in1=cosT[:, t0:t0 + 512], op=ALU.mult),
                                             reads=[rw_id, "cosT"], writes=[a2_id])
                                        S.op("dve", lambda e, a1=a1, a2=a2, dst_ap=dst_ap: e.tensor_tensor(
                                            out=dst_ap, in0=a1[:], in1=a2[:], op=ALU.add),
                                            reads=[a1_id, a2_id], writes=[dst_id])
                                    if isq:
                                        S.dma("sp", lambda e, dst_ap=dst_ap, dchunk=dchunk, t0=t0, tn=tn: e.dma_start(
                                            out=QT[:, dchunk, t0:t0 + tn], in_=dst_ap), "st" + dst_id, reads=[dst_id], writes=["QT"])
                        for g in range(nv_cols // 256):
                            ncol = 256
                            wst, wst_id = wstg.next()
                            wb, wb_id = wbf.next()
                            S.dma("pool", lambda e, wb=wb, g=g, ncol=ncol: e.dma_start(out=wb[:, :, 0:ncol], in_=wv_v[:, :, g * 256:g * 256 + ncol]),
                                  "ld" + wb_id, writes=[wb_id])
                            nh = ncol // 64
                            for jt in range(TPB):
                                pp, pp_id = pab[pcount % 2]
                                pcount += 1
                                for c in range(8):
                                    S.op("pe", lambda e, pp=pp, wb=wb, c=c, jt=jt, ncol=ncol: e.matmul(
                                        pp[:, 0:ncol], lhsT=hT[:, c, jt * 128:(jt + 1) * 128], rhs=wb[:, c, 0:ncol],
                                        start=(c == 0), stop=(c == 7)), reads=[wb_id, "hT"], writes=[pp_id])
                                S.op("act", lambda e, pp=pp, jt=jt, g=g, nh=nh, ncol=ncol: e.copy(
                                    out=va[:, jt, g * 4:g * 4 + nh, 0:64], in_=pp[:, 0:ncol].rearrange("p (h d) -> p h d", d=64)),
                                    reads=[pp_id], writes=["va"])
                        S.flush()
                    with ExitStack() as es2:
                        def sb2(name, shape, dt):
                            return es2.enter_context(nc.sbuf_tensor(un(name), list(shape), dt))
                        M = mixer_common_alloc(es2, i)
                        wob = sb2("at_wob", [128, 8, D], BF16)
                        wstg = Rot(es2, "at_wst2_", 2, [128, 8, 256], F32)
                        for g in range(4):
                            wst, wst_id = wstg.next()
                            S.dma("pool", lambda e, g=g: e.dma_start(out=wob[:, :, g * 256:(g + 1) * 256], in_=wo.rearrange("(c p) n -> p c n", p=128)[:, :, g * 256:(g + 1) * 256]),
                                  "ldwob", writes=["wob"])
                        sinkx = sb2("at_sink", [128, 16], F32)
                        if is_a:
                            S.dma("sp", lambda e: e.dma_start(out=sinkx[:], in_=attn_sink[j, :].partition_broadcast(128)), "snk", writes=["sinkx"])
                            S.op("act", lambda e: e.activation(out=sinkx[:], in_=sinkx[:], func=AF.Exp), reads=["sinkx"], writes=["sinkx"])
                            bandf = sb2("at_bandf", [128, 2, 128], F32)
                            bandb = sb2("at_bandb", [128, 2, 128], BF16)
                            S.dma("sp", lambda e: e.dma_start(out=bandf[:], in_=k_band.rearrange("m k q -> k m q")), "bnd", writes=["bandf"])
                            S.op("dve", lambda e: e.tensor_copy(out=bandb[:], in_=bandf[:]), reads=["bandf"], writes=["bandb"])
                        else:
                            S.op("pool", lambda e: e.memset(sinkx[:], 0.0), writes=["sinkx"])
                            etr = Rot(es2, "at_et", 2, [128, 16, 128], BF16)
                        ptr_ = Rot(es2, "at_pt", 4, [128, 512], BF16)
                        otok = Rot(es2, "at_otok", 2, [128, D], BF16)
                        oTr = Rot(es2, "at_oT", 2, [128, 8, 128], BF16)
                        den = Rot(es2, "at_den", 2, [128, 16], F32)
                        pab = [(P.pA, "pA"), (P.pB, "pB")]
                        pcount = 0
                        qtiles = list(range(16)) + ([] if last else [16, 17])
                        qtr = Rot(es2, "at_qt", 2, [128, 16, 128], BF16)
                        for (qb_, qb_id) in qtr.bufs:
                            S.op("pool", lambda e, qb_=qb_: e.memset(qb_[:], 0.0), writes=[qb_id])
                        def kcs_for(jq):
                            if jq >= 16:
                                return [(16, None), (17, None)]
                            if is_a:
                                kcs = []
                                if jq > 0:
                                    kcs.append((jq - 1, ("band", 0)))
                                kcs.append((jq, None))
                                if jq < 15:
                                    kcs.append((jq + 1, ("band", 1)))
                                return kcs + [(16, None), (17, None)]
                            return [(kc, ("nat", pats[(jq, kc)])) for kc in nat_chunks[jq]] + [(16, None), (17, None)]

                        pstate = {"n": 0}

                        def rec_core(jq):
                            qT, qT_id = qtr.next()
                            qTv = qT[:].rearrange("p (j two) q -> p j two q", two=2)
                            kcs = kcs_for(jq)

                            def pro():
                                S.dma("sp", lambda e: e.dma_start(out=qTv[0:64, :, 0, :], in_=QT[0:64, :, jq * 128:(jq + 1) * 128]),
                                      qT_id, reads=["QT"], writes=[qT_id])
                                S.dma("sp", lambda e: e.dma_start(out=qTv[64:128, :, 1, :], in_=QT[64:128, :, jq * 128:(jq + 1) * 128]),
                                      qT_id, reads=["QT"], writes=[qT_id])
                                for bank in range(3):
                                    S.op("pe", lambda e, bank=bank: e.matmul(P.oacc[:, bank, :], lhsT=zerob[:, 0:128], rhs=zerob[:], start=True, stop=True),
                                         reads=["zerob"], writes=["oacc"])
                            prol, _ = S.capture(pro)
                            steps = []
                            for ci, (kc, msk) in enumerate(kcs):
                                et = None
                                et_id = None
                                if msk is not None and msk[0] == "nat":
                                    et, et_id = etr.next()
                                for hg in range(4):
                                    pp, pp_id = pab[pstate["n"] % 2]
                                    pstate["n"] += 1
                                    pt, pt_id = ptr_.next()

                                    def s_part(ci=ci, kc=kc, msk=msk, hg=hg, pp=pp, pp_id=pp_id, et=et, et_id=et_id):
                                        if et is not None and hg == 0:
                                            S.dma("sp", lambda e: e.dma_start(out=et[:], in_=ETAB[msk[1]]), et_id, reads=["ETAB"], writes=[et_id])
                                        for hh in range(4):
                                            h = hg * 4 + hh
                                            kch = hg if is_a else h // 2
                                            S.op("pe", lambda e, hh=hh, kch=kch, h=h: e.matmul(
                                                pp[:, hh * 128:(hh + 1) * 128], lhsT=kT[:, kch, kc * 128:(kc + 1) * 128],
                                                rhs=qT[:, h, :], start=True, stop=True),
                                                reads=[qT_id, "kT"], writes=[pp_id])

                                    def r_part(ci=ci, kc=kc, msk=msk, hg=hg, pp=pp, pp_id=pp_id, pt=pt, pt_id=pt_id, et=et, et_id=et_id, n=len(kcs)):
                                        S.op("act", lambda e: e.activation(out=pt[:], in_=pp[:], func=AF.Exp, scale=scale),
                                             reads=[pp_id], writes=[pt_id])
                                        if msk is not None:
                                            ptv = pt[:].rearrange("p (h q) -> p h q", q=128)
                                            if msk[0] == "band":
                                                S.op("pool", lambda e: e.tensor_tensor(out=ptv, in0=ptv,
                                                     in1=bandb[:, msk[1], :].unsqueeze(1).to_broadcast([128, 4, 128]), op=ALU.mult),
                                                     reads=[pt_id, "bandb"], writes=[pt_id])
                                            else:
                                                S.op("pool", lambda e: e.tensor_tensor(out=ptv, in0=ptv, in1=et[:, hg * 4:(hg + 1) * 4, :], op=ALU.mult),
                                                     reads=[pt_id, et_id], writes=[pt_id])
                                        for hh in range(4):
                                            h = hg * 4 + hh
                                            kvh = hg if is_a else h
                                            bank, off = HB[h]
                                            S.op("pe", lambda e, hh=hh, kvh=kvh, bank=bank, off=off: e.matmul(
                                                P.oacc[:, bank, off * 65:(off + 1) * 65], lhsT=pt[:, hh * 128:(hh + 1) * 128],
                                                rhs=va[:, kc, kvh, :], start=False, stop=(ci == n - 1)),
                                                reads=[pt_id, "va"], writes=["oacc"])
                                    sl, _ = S.capture(s_part)
                                    rl, _ = S.capture(r_part)
                                    steps.append((sl, rl))
                            return prol, steps

                        def core_units(steps):
                            units = []
                            n = len(steps)
                            for k in range(min(2, n)):
                                units.append(steps[k][0])
                            for k in range(n):
                                units.append(steps[k][1])
                                if k + 2 < n:
                                    units.append(steps[k + 2][0])
                            return units

                        def norm(jq):
                            tt = b * TPB + jq
                            dn, dn_id = den.next()
                            ot, ot_id = otok.next()
                            oT, oT_id = oTr.next()
                            for bank in range(3):
                                nh = (6, 5, 5)[bank]
                                h0 = (0, 6, 11)[bank]
                                S.op("dve", lambda e, bank=bank, nh=nh, h0=h0: e.tensor_tensor(
                                    out=dn[:, h0:h0 + nh], in0=P.oacc[:, bank, 0:nh * 65].rearrange("p (h d) -> p h d", d=65)[:, :, 64],
                                    in1=sinkx[:, h0:h0 + nh], op=ALU.add), reads=["oacc", "sinkx"], writes=[dn_id])
                            S.op("dve", lambda e: e.reciprocal(out=dn[:], in_=dn[:]), reads=[dn_id], writes=[dn_id])
                            for bank in range(3):
                                nh = (6, 5, 5)[bank]
                                h0 = (0, 6, 11)[bank]
                                S.op("dve", lambda e, bank=bank, nh=nh, h0=h0: e.tensor_tensor(
                                    out=ot[:, h0 * 64:(h0 + nh) * 64].rearrange("p (h d) -> p h d", d=64),
                                    in0=P.oacc[:, bank, 0:nh * 65].rearrange("p (h d) -> p h d", d=65)[:, :, 0:64],
                                    in1=dn[:, h0:h0 + nh].unsqueeze(2).to_broadcast([128, nh, 64]), op=ALU.mult),
                                    reads=["oacc", dn_id], writes=[ot_id])
                            return ot, ot_id, oT, oT_id

                        def tail(jq, ot, ot_id, oT, oT_id):
                            tt = b * TPB + jq
                            load_variant(M, i, b if jq < 16 else 2)
                            for hf4 in range(2):
                                for c in range(4):
                                    S.op("pe", lambda e, c=c, hf4=hf4: e.transpose(out=P.ptb[:, c, :], in_=ot[:, (hf4 * 4 + c) * 128:(hf4 * 4 + c + 1) * 128], identity=identb[:]),
                                         reads=[ot_id, "identb"], writes=["ptb"])
                                S.op("act", lambda e, hf4=hf4: e.copy(out=oT[:, hf4 * 4:(hf4 + 1) * 4, :], in_=P.ptb), reads=["ptb"], writes=[oT_id])
                            po = P.ptf[:].rearrange("p c t -> p (c t)")
                            for hf in range(2):
                                for c in range(8):
                                    S.op("pe", lambda e, c=c, hf=hf: e.matmul(po[:, hf * 512:(hf + 1) * 512], lhsT=oT[:, c, :],
                                                                              rhs=wob[:, c, hf * 512:(hf + 1) * 512],
                                                                              start=(c == 0), stop=(c == 7)),
                                         reads=[oT_id, "wob"], writes=["ptf"])
                            out_stage(M, P, i, tt, po, ["ptf"])

                        def interleave(units, tl):
                            out_ = []
                            nu = max(1, len(units))
                            per = -(-len(tl) // nu)
                            ti = 0
                            for u in units:
                                out_.extend(u)
                                out_.extend(tl[ti:ti + per])
                                ti += per
                            out_.extend(tl[ti:])
                            return out_

                        prol, steps = rec_core(qtiles[0])
                        S.replay(prol)
                        for u in core_units(steps):
                            S.replay(u)
                        for qi, jq in enumerate(qtiles):
                            nl, nr = S.capture(norm, jq)
                            S.replay(nl)
                            tl, _ = S.capture(tail, jq, *nr)
                            if qi + 1 < len(qtiles):
                                prol, steps = rec_core(qtiles[qi + 1])
                                S.replay(prol)
                                S.replay(interleave(core_units(steps), tl))
                            else:
                                S.replay(tl)
                        S.flush()

        def phase_nat_table():
            with ExitStack() as es:
                bt = Rot(es, "nt_b", 2, [128, 16, 128], F32)
                mt = Rot(es, "nt_m", 2, [128, 128], F32)
                eo = Rot(es, "nt_e", 2, [128, 16, 128], BF16)
                for pid in range(NPAT):
                    b_, b_id = bt.next()
                    m_, m_id = mt.next()
                    e_, e_id = eo.next()
                    S.dma("sp", lambda e, b_=b_, pid=pid: e.dma_start(out=b_[:], in_=nat_bias[pid]), b_id, writes=[b_id])
                    S.dma("sp", lambda e, m_=m_, pid=pid: e.dma_start(out=m_[:], in_=k_natmask[pid]), m_id, writes=[m_id])
                    S.op("act", lambda e, b_=b_: e.activation(out=b_[:], in_=b_[:], func=AF.Exp), reads=[b_id], writes=[b_id])
                    S.op("dve", lambda e, b_=b_, m_=m_, e_=e_: e.tensor_tensor(out=e_[:], in0=b_[:], in1=m_[:].unsqueeze(1).to_broadcast([128, 16, 128]),
                                                                              op=ALU.mult), reads=[b_id, m_id], writes=[e_id])
                    S.dma("sp", lambda e, e_=e_, pid=pid: e.dma_start(out=ETAB[pid], in_=e_[:]), "st" + e_id, reads=[e_id], writes=["ETAB"])
                S.flush()

        def phase_conv(i, j, last):
            win = conv_w_in[j].rearrange("(c p) n -> p c n", p=128)
            for b in range(NB):
                with ExitStack() as es:
                    def sb(name, shape, dt):
                        return es.enter_context(nc.sbuf_tensor(un(name), list(shape), dt))
                    P = alloc_psum(es)
                    zT = sb("cv_zT", [128, 8, 2304], BF16)
                    with ExitStack() as es1:
                        def sb1(name, shape, dt):
                            return es1.enter_context(nc.sbuf_tensor(un(name), list(shape), dt))
                        hT = sb1("cv_hT", [128, 8, 2304], BF16)
                        scal = sb1("cv_scal", [128, 2, 8], F32)
                        cw = sb1("cv_cw", [128, 3, 8], F32)
                        S.dma("sp", lambda e: e.dma_start(out=cw[:], in_=conv_w[j].rearrange("k (c p) -> p k c", p=128), allow_slow_non_contiguous=True),
                              "cw", writes=["cw"])
                        wstg = Rot(es1, "cv_wst", 2, [128, 8, 3, 128], F32)
                        wbf = Rot(es1, "cv_wbf", 2, [128, 8, 3, 128], BF16)
                        pbuf = sb1("cv_p", [128, 2308], F32)
                        gbuf = sb1("cv_g", [128, 2304], F32)
                        ubuf = Rot(es1, "cv_u", 2, [128, 512], F32)
                        acc = sb1("cv_acc", [128, 2304], F32)
                        S.op("pool", lambda e: e.memset(pbuf[:], 0.0), writes=["pbuf"])
                        make_hT(es1, P, i, b, hT, scal)
                        pbanks = [(P.pA, "pA"), (P.pB, "pB"), (P.pC, "pC")]
                        for c in range(8):
                            wst, wst_id = wstg.next()
                            wb, wb_id = wbf.next()
                            for k3 in range(3):
                                S.dma("pool", lambda e, wb=wb, k3=k3, c=c: e.dma_start(out=wb[:, :, k3, :], in_=win[:, :, k3 * D + c * 128:k3 * D + (c + 1) * 128]),
                                      "ld" + wb_id, writes=[wb_id])
                            for (t0, tn) in TOKG:
                                poff = 1 + t0 if t0 < SEQ else 2051
                                for k3 in range(3):
                                    pp, pp_id = pbanks[k3]
                                    for kk in range(8):
                                        S.op("pe", lambda e, pp=pp, wb=wb, k3=k3, kk=kk, t0=t0, tn=tn: e.matmul(
                                            pp[:, 0:tn], lhsT=wb[:, kk, k3, :], rhs=hT[:, kk, t0:t0 + tn], start=(kk == 0), stop=(kk == 7)),
                                            reads=[wb_id, "hT"], writes=[pp_id])
                                ub, ub_id = ubuf.next()
                                S.op("act", lambda e, ub=ub, tn=tn: e.copy(out=ub[:, 0:tn], in_=P.pC[:, 0:tn]), reads=["pC"], writes=[ub_id])
                                S.op("act", lambda e, t0=t0, tn=tn: e.copy(out=gbuf[:, t0:t0 + tn], in_=P.pA[:, 0:tn]), reads=["pA"], writes=["gbuf"])
                                S.op("dve", lambda e, ub=ub, tn=tn, poff=poff: e.tensor_tensor(out=pbuf[:, poff:poff + tn], in0=P.pB[:, 0:tn], in1=ub[:, 0:tn], op=ALU.mult),
                                     reads=["pB", ub_id], writes=["pbuf"])
                            for (o0, p0, n) in ((0, 1, SEQ), (SEQ, 2051, LCTX)):
                                S.op("dve", lambda e, c=c, o0=o0, p0=p0, n=n: e.tensor_scalar(out=acc[:, o0:o0 + n], in0=pbuf[:, p0:p0 + n], scalar1=cw[:, 1, c:c + 1],
                                                                                             scalar2=None, op0=ALU.mult), reads=["pbuf", "cw"], writes=["acc"])
                                S.op("dve", lambda e, c=c, o0=o0, p0=p0, n=n: e.scalar_tensor_tensor(out=acc[:, o0:o0 + n], in0=pbuf[:, p0 - 1:p0 - 1 + n], scalar=cw[:, 0, c:c + 1],
                                                                                                      in1=acc[:, o0:o0 + n], op0=ALU.mult, op1=ALU.add),
                                     reads=["pbuf", "cw", "acc"], writes=["acc"])
                                S.op("dve", lambda e, c=c, o0=o0, p0=p0, n=n: e.scalar_tensor_tensor(out=acc[:, o0:o0 + n], in0=pbuf[:, p0 + 1:p0 + 1 + n], scalar=cw[:, 2, c:c + 1],
                                                                                                     in1=acc[:, o0:o0 + n], op0=ALU.mult, op1=ALU.add),
                                     reads=["pbuf", "cw", "acc"], writes=["acc"])
                            S.op("pool", lambda e, c=c: e.tensor_tensor(out=zT[:, c, :], in0=acc[:], in1=gbuf[:], op=ALU.mult),
                                 reads=["acc", "gbuf"], writes=["zT"])
                        S.flush()
                    with ExitStack() as es2:
                        def sb2(name, shape, dt):
                            return es2.enter_context(nc.sbuf_tensor(un(name), list(shape), dt))
                        M = mixer_common_alloc(es2, i)
                        wob = sb2("cv_wob", [128, 8, D], BF16)
                        wstg = Rot(es2, "cv_wst2_", 2, [128, 8, 512], F32)
                        wo = conv_w_out[j].rearrange("(c p) n -> p c n", p=128)
                        for g in range(2):
                            wst, wst_id = wstg.next()
                            S.dma("pool", lambda e, g=g: e.dma_start(out=wob[:, :, g * 512:(g + 1) * 512], in_=wo[:, :, g * 512:(g + 1) * 512]), "ldwob", writes=["wob"])
                        qtiles = list(range(16)) + ([] if last else [16, 17])
                        po = P.ptf[:].rearrange("p c t -> p (c t)")
                        for jq in qtiles:
                            tt = b * TPB + jq
                            load_variant(M, i, b if jq < 16 else 2)
                            for hf in range(2):
                                for c in range(8):
                                    S.op("pe", lambda e, c=c, hf=hf, jq=jq: e.matmul(po[:, hf * 512:(hf + 1) * 512], lhsT=zT[:, c, jq * 128:(jq + 1) * 128],
                                                                                     rhs=wob[:, c, hf * 512:(hf + 1) * 512], start=(c == 0), stop=(c == 7)),
                                         reads=["zT", "wob"], writes=["ptf"])
                            out_stage(M, P, i, tt, po, ["ptf"])
                        S.flush()

        def phase_experts(i):
            NTB = CAP // 128
            NNT = CAP // 512
            with ExitStack() as es:
                def ps(name, shape, dt):
                    return es.enter_context(nc.psum_tensor(un(name), list(shape), dt))
                wg = Rot(es, "ex_wg", 3, [128, 8, 512], BF16)
                wu = Rot(es, "ex_wu", 3, [128, 8, 512], BF16)
                wd = Rot(es, "ex_wd", 3, [128, 4, D], BF16)
                xs = Rot(es, "ex_xs", 3, [128, NTB, D], BF16)
                xsT = Rot(es, "ex_xsT", 2, [128, 8, CAP], BF16)
                sg = Rot(es, "ex_sg", 2, [128, 512], F32)
                aT = Rot(es, "ex_aT", 2, [128, 4, CAP], BF16)
                ys = Rot(es, "ex_ys", 2, [128, D], F32)
                ptb = [(ps("ex_ptb%d" % k, [128, 8, 128], BF16), "ptb%d" % k) for k in range(2)]
                pg = [(ps("ex_pg%d" % k, [128, 512], F32), "pg%d" % k) for k in range(2)]
                pu = [(ps("ex_pu%d" % k, [128, 512], F32), "pu%d" % k) for k in range(2)]
                py = ps("ex_py", [128, D], F32)
                st8 = {"nptb": 0, "npg": 0}

                def prep(ex):
                    wgb, wg_id = wg.next()
                    wub, wu_id = wu.next()
                    wdb, wd_id = wd.next()
                    xsb, xs_id = xs.next()
                    for (src, dstb, dst_id) in ((w_gate[i, ex].rearrange("(c p) n -> p c n", p=128), wgb, wg_id),
                                                (w_up[i, ex].rearrange("(c p) n -> p c n", p=128), wub, wu_id),
                                                (w_down[i, ex].rearrange("(c p) n -> p c n", p=128), wdb, wd_id)):
                        S.dma("pool", lambda e, dstb=dstb, src=src: e.dma_start(out=dstb[:], in_=src), "ld" + dst_id, writes=[dst_id])
                    for q in range(CAP // 256):
                        S.cur_group = (ex, q * 256 + 1) if q > 0 else None
                        S.dma("sp", lambda e, xsb=xsb, ex=ex, q=q: e.dma_start(
                            out=xsb[:, 2 * q:2 * q + 2, :], in_=XS[ex * CAP + q * 256:ex * CAP + (q + 1) * 256, :].rearrange("(j p) d -> p j d", p=128)),
                            xs_id + "q%d" % q, reads=["XS"], writes=[xs_id + "q%d" % q])
                    S.cur_group = None
                    return dict(wgb=wgb, wg_id=wg_id, wub=wub, wu_id=wu_id, wdb=wdb, wd_id=wd_id, xsb=xsb, xs_id=xs_id)

                def late_cast(pr):
                    return

                def compute(ex, pr):
                    wgb, wg_id, wub, wu_id, wdb, wd_id, xsb, xs_id = (pr[k] for k in ("wgb", "wg_id", "wub", "wu_id", "wdb", "wd_id", "xsb", "xs_id"))
                    xTb, xT_id = xsT.next()
                    aTb, aT_id = aT.next()
                    QR = 256
                    for q in range(CAP // QR):
                        S.cur_group = (ex, q * QR + 1) if q > 0 else None
                        c0 = q * QR
                        for jt in range(2 * q, 2 * q + 2):
                            pt, pt_id = ptb[st8["nptb"] % 2]
                            st8["nptb"] += 1
                            for c in range(8):
                                S.op("pe", lambda e, pt=pt, jt=jt, c=c: e.transpose(out=pt[:, c, :], in_=xsb[:, jt, c * 128:(c + 1) * 128], identity=identb[:]),
                                     reads=[xs_id + "q%d" % q, "identb"], writes=[pt_id])
                            S.op("act", lambda e, pt=pt, jt=jt: e.copy(out=xTb[:, :, jt * 128:(jt + 1) * 128], in_=pt[:]), reads=[pt_id], writes=[xT_id])
                        for m_ in range(4):
                            pgb, pg_id = pg[st8["npg"] % 2]
                            pub, pu_id = pu[st8["npg"] % 2]
                            st8["npg"] += 1
                            for c in range(8):
                                S.op("pe", lambda e, pgb=pgb, c=c, m_=m_, c0=c0: e.matmul(pgb[:, 0:QR], lhsT=wgb[:, c, m_ * 128:(m_ + 1) * 128], rhs=xTb[:, c, c0:c0 + QR],
                                                                                          start=(c == 0), stop=(c == 7)), reads=[wg_id, xT_id], writes=[pg_id])
                            for c in range(8):
                                S.op("pe", lambda e, pub=pub, c=c, m_=m_, c0=c0: e.matmul(pub[:, 0:QR], lhsT=wub[:, c, m_ * 128:(m_ + 1) * 128], rhs=xTb[:, c, c0:c0 + QR],
                                                                                          start=(c == 0), stop=(c == 7)), reads=[wu_id, xT_id], writes=[pu_id])
                            sgb, sg_id = sg.next()
                            S.op("act", lambda e, sgb=sgb, pgb=pgb: e.activation(out=sgb[:, 0:QR], in_=pgb[:, 0:QR], func=AF.Silu), reads=[pg_id], writes=[sg_id])
                            S.op("dve", lambda e, sgb=sgb, pub=pub, m_=m_, c0=c0: e.tensor_tensor(out=aTb[:, m_, c0:c0 + QR], in0=pub[:, 0:QR], in1=sgb[:, 0:QR], op=ALU.mult),
                                 reads=[pu_id, sg_id], writes=[aT_id])
                        for jt in range(2 * q, 2 * q + 2):
                            for hf in range(2):
                                for m_ in range(4):
                                    S.op("pe", lambda e, jt=jt, hf=hf, m_=m_: e.matmul(py[:, hf * 512:(hf + 1) * 512], lhsT=aTb[:, m_, jt * 128:(jt + 1) * 128],
                                                                                       rhs=wdb[:, m_, hf * 512:(hf + 1) * 512], start=(m_ == 0), stop=(m_ == 3)),
                                         reads=[aT_id, wd_id], writes=["py"])
                            ysb, ys_id = ys.next()
                            if jt % 2 == 0:
                                S.op("act", lambda e, ysb=ysb: e.copy(out=ysb[:], in_=py[:]), reads=["py"], writes=[ys_id])
                            else:
                                S.op("dve", lambda e, ysb=ysb: e.tensor_copy(out=ysb[:], in_=py[:]), reads=["py"], writes=[ys_id])
                            r0 = ex * CAP + jt * 128
                            S.dma("sp", lambda e, ysb=ysb, r0=r0: e.dma_start(out=YS[r0:r0 + 128, :], in_=ysb[:]), "st" + ys_id, reads=[ys_id], writes=["YS"])
                    S.cur_group = None

                preps = {0: prep(0), 1: prep(1)}
                for ex in range(NEXP):
                    if ex + 2 < NEXP:
                        preps[ex + 2] = prep(ex + 2)
                    compute(ex, preps.pop(ex))
                S.flush()

        def phase_combine(i, last):
            with ExitStack() as es:
                def sb(name, shape, dt):
                    return es.enter_context(nc.sbuf_tensor(un(name), list(shape), dt))
                G2 = sb("cb_G2", [128, D], F32)
                lg = sb("cb_lg", [128, D], F32)
                lb = sb("cb_lb", [128, D], F32)
                y0 = Rot(es, "cb_y0", 2, [128, D], F32)
                y1 = Rot(es, "cb_y1", 2, [128, D], F32)
                xm = Rot(es, "cb_xm", 2, [128, D], F32)
                tb = Rot(es, "cb_t", 2, [128, D], F32)
                xn = Rot(es, "cb_xn", 2, [128, D], F32)
                xo = Rot(es, "cb_xo", 2, [128, D], F32)
                st = Rot(es, "cb_st", 2, [128, 2, 6], F32)
                mv = Rot(es, "cb_mv", 2, [128, 2], F32)
                rs = Rot(es, "cb_rs", 2, [128, 2], F32)
                S.dma("sp", lambda e: e.dma_start(out=lg[:], in_=ln2_g[i, :].partition_broadcast(128)), "clg", writes=["lg"])
                S.dma("sp", lambda e: e.dma_start(out=lb[:], in_=ln2_b[i, :].partition_broadcast(128)), "clb", writes=["lb"])
                cur_v = None
                for tt in range(NT):
                    b, j = divmod(tt, TPB)
                    if last and j >= 16:
                        continue
                    v = b if j < 16 else 2
                    if v != cur_v:
                        cur_v = v
                        S.dma("sp", lambda e, v=v: e.dma_start(out=G2[:], in_=MODR[i, v, 5, :].partition_broadcast(128)), "cG2", writes=["G2"])
                    y0b, y0_id = y0.next()
                    y1b, y1_id = y1.next()
                    xmb, xm_id = xm.next()
                    t, t_id = tb.next()
                    xnb, xn_id = xn.next()
                    xob, xo_id = xo.next()
                    for k, (yb, y_id) in enumerate(((y0b, y0_id), (y1b, y1_id))):
                        S.dma("pool", lambda e, yb=yb, k=k, tt=tt: e.indirect_dma_start(
                            out=yb[:], out_offset=None, in_=YS, in_offset=bass.IndirectOffsetOnAxis(ap=destI[:, tt, k:k + 1], axis=0),
                            bounds_check=breg(e), oob_is_err=False), y_id, reads=["YS"], writes=[y_id])
                    S.dma("sp", lambda e, xmb=xmb, tt=tt: e.dma_start(out=xmb[:], in_=X[tt * 128:(tt + 1) * 128, :]), xm_id, reads=["X%d" % tt], writes=[xm_id])
                    S.op("act", lambda e, t=t, y0b=y0b, tt=tt: e.activation(out=t[:], in_=y0b[:], func=AF.Copy, scale=gates[:, tt, 0:1]),
                         reads=[y0_id], writes=[t_id])
                    S.op("dve", lambda e, t=t, y1b=y1b, tt=tt: e.scalar_tensor_tensor(out=t[:], in0=y1b[:], scalar=gates[:, tt, 1:2], in1=t[:], op0=ALU.mult, op1=ALU.add),
                         reads=[y1_id, t_id], writes=[t_id])
                    S.op("dve", lambda e, t=t: e.tensor_tensor(out=t[:], in0=t[:], in1=G2[:], op=ALU.mult), reads=[t_id, "G2"], writes=[t_id])
                    S.op("dve", lambda e, t=t, xmb=xmb: e.tensor_tensor(out=t[:], in0=t[:], in1=xmb[:], op=ALU.add), reads=[t_id, xm_id], writes=[t_id])
                    layer_norm_core((st, mv, rs), t, t_id, xnb, xn_id)
                    S.op("dve", lambda e, xob=xob, xnb=xnb: e.tensor_tensor(out=xob[:], in0=xnb[:], in1=lg[:], op=ALU.mult), reads=[xn_id, "lg"], writes=[xo_id])
                    S.op("dve", lambda e, xob=xob: e.tensor_tensor(out=xob[:], in0=xob[:], in1=lb[:], op=ALU.add), reads=[xo_id, "lb"], writes=[xo_id])
                    if last:
                        dst = out[b, j * 128:(j + 1) * 128, :]
                    else:
                        dst = X[tt * 128:(tt + 1) * 128, :]
                    S.dma("sp", lambda e, xob=xob, dst=dst: e.dma_start(out=dst, in_=xob[:]), "st" + xo_id, reads=[xo_id], writes=["X%d" % tt])
                S.flush()

        phase_init()
        phase_nat_table()
        for i in range(DEPTH):
            last = i == DEPTH - 1
            kind = i % 3
            j = i // 3
            phase_ada(i)
            S.op("pool", lambda e: e.memset(cnt[:], 0.0), writes=["cnt"])
            if kind == 0:
                phase_attn(i, j, 0, last)
            elif kind == 1:
                phase_conv(i, j, last)
            else:
                phase_attn(i, j, 2, last)
            S.op("dve", lambda e: e.tensor_copy(out=cnti[:], in_=cnt[:]), reads=["cnt"], writes=["cnti"])
            S.flush()
            phase_experts(i)
            phase_combine(i, last)
        if debug:
            S.limit = None
            dbgX = nc.dram_tensor("dbgX", [NT * 128, D], F32, kind="ExternalOutput").ap()
            dbgM = nc.dram_tensor("dbgM", [DEPTH, 3, 6, D], F32, kind="ExternalOutput").ap()
            dbgXS = nc.dram_tensor("dbgXS", [NSLOT, D], BF16, kind="ExternalOutput").ap()
            dbgYS = nc.dram_tensor("dbgYS", [NSLOT, D], F32, kind="ExternalOutput").ap()
            dbgQ = nc.dram_tensor("dbgQ", [128, 8, 2304], BF16, kind="ExternalOutput").ap()
            dbgD = nc.dram_tensor("dbgD", [128, NT, 2], I32, kind="ExternalOutput").ap()
            dbgG = nc.dram_tensor("dbgG", [128, NT, 2], F32, kind="ExternalOutput").ap()
            for r0 in range(0, NT * 128, 512):
                S.dma("sp", lambda e, r0=r0: e.dma_start(out=dbgX[r0:r0 + 512, :], in_=X[r0:r0 + 512, :]), "d0")
            S.dma("sp", lambda e: e.dma_start(out=dbgM, in_=MODR), "d1")
            for r0 in range(0, NSLOT, 512):
                S.dma("sp", lambda e, r0=r0: e.dma_start(out=dbgXS[r0:r0 + 512, :], in_=XS[r0:r0 + 512, :]), "d2")
                S.dma("sp", lambda e, r0=r0: e.dma_start(out=dbgYS[r0:r0 + 512, :], in_=YS[r0:r0 + 512, :]), "d3")
            S.dma("sp", lambda e: e.dma_start(out=dbgQ, in_=QT), "d4")
            S.dma("sp", lambda e: e.dma_start(out=dbgD, in_=destI[:]), "d5")
            S.dma("sp", lambda e: e.dma_start(out=dbgG, in_=gates[:]), "d6")
            S.flush()
    return nc


_NC_CACHE = {}


def _host_constants(inputs):
    cosT, sinT, Pm = _rope_tables()
    pats, chunks, dr, dc, mk, npat = _NATP
    rpb = np.asarray(inputs["nat_rpb"], np.float32)[0]
    nat_bias = np.ascontiguousarray(np.transpose(rpb[:, dr, dc], (1, 2, 0, 3))).astype(np.float32)
    aw = np.asarray(inputs["attn_w_qkv"], np.float32)
    kcols = []
    for g in range(4):
        kcols += list(range(1024 + g * 64, 1024 + (g + 1) * 64)) * 2
    attn_wqk = np.ascontiguousarray(np.concatenate([aw[:, :, :1024], aw[:, :, kcols]], axis=2))
    attn_wv = np.ascontiguousarray(aw[:, :, 1280:1536])
    nw = np.asarray(inputs["nat_w_qkv"], np.float32)
    router_w = np.ascontiguousarray(np.concatenate([inputs["router_w_group"], inputs["router_w_expert"]], axis=2)).astype(np.float32)
    router_b = np.ascontiguousarray(np.concatenate([inputs["router_b_group"], inputs["router_b_expert"]], axis=1)).astype(np.float32)
    kk = np.arange(128)
    consts = {
        "attn_wqk": attn_wqk, "attn_wv": attn_wv,
        "nat_wqk": np.ascontiguousarray(nw[:, :, :2048]), "nat_wv": np.ascontiguousarray(nw[:, :, 2048:]),
        "nat_bias": nat_bias, "router_w": router_w, "router_b": router_b,
        "k_ident": np.eye(128, dtype=np.float32), "k_cos": cosT, "k_sin": sinT, "k_pm": Pm,
        "k_band": _band_masks(), "k_natmask": mk,
        "k_ustrict": (kk[:, None] < kk[None, :]).astype(np.float32),
        "k_eoff": (np.arange(32, dtype=np.float32) * CAP)[None, :],
    }
    return consts


def kernel(**inputs):
    if "nc" not in _NC_CACHE:
        _NC_CACHE["nc"] = build_program()
    nc = _NC_CACHE["nc"]
    consts = _host_constants(inputs)
    shared = {}
    for k in ("w_ada", "b_ada", "ln1_g", "ln1_b", "ln2_g", "ln2_b", "attn_w_o", "attn_sink", "conv_w_in", "conv_w",
              "conv_w_out", "nat_w_o", "expert_w_gate", "expert_w_up", "expert_w_down"):
        shared[k] = np.ascontiguousarray(np.asarray(inputs[k], np.float32))
    shared.update(consts)
    shared["c_ctx"] = np.ascontiguousarray(np.asarray(inputs["c_ctx"], np.float32).reshape(1, D))
    x = np.asarray(inputs["x"], np.float32)
    c = np.asarray(inputs["c"], np.float32)
    ctx = np.asarray(inputs["ctx"], np.float32)
    in_maps = []
    for core in range(NCORES):
        m = dict(shared)
        m["x"] = np.ascontiguousarray(x[core * NB:(core + 1) * NB])
        m["c"] = np.ascontiguousarray(c[core * NB:(core + 1) * NB])
        m["ctx"] = np.ascontiguousarray(ctx[core * NB:(core + 1) * NB])
        in_maps.append(m)
    res = run_bass_kernel_spmd(nc, in_maps, core_ids=list(range(NCORES)))
    return np.concatenate([r["out"] for r in res.results], axis=0).astype(np.float32)
```

```python
import numpy as np
from contextlib import ExitStack
import ml_dtypes
import concourse.bass as bass
import concourse.mybir as mybir
from concourse.bass_utils import run_bass_kernel_spmd

F32 = mybir.dt.float32
BF16 = mybir.dt.bfloat16
I32 = mybir.dt.int32
AF = mybir.ActivationFunctionType
ALU = mybir.AluOpType
AX = mybir.AxisListType.X

NCORES = 8
D = 1024
SEQ = 2048
LCTX = 256
DEPTH = 4
NB = 2
TPB = 18
NT = NB * TPB
NEXP = 32
FF = 512
CAP = 1024
NSLOT = NEXP * CAP
ALPHA = (2 * DEPTH) ** 0.25
INV_ALPHA = 1.0 / ALPHA
EPS2 = 1e-5 / (ALPHA * ALPHA)
BIG = 1.0e30
ENGS = ("pe", "act", "dve", "pool", "sp")
import os as _os
KDBG = int(_os.environ.get("KDBG", "0"))


class _Op:
    __slots__ = ("idx", "eng", "fn", "is_dma", "key", "dma_val", "signal", "milestone", "waits", "group")

    def __init__(self, idx, eng, fn, is_dma, key):
        self.group = None
        self.idx = idx
        self.eng = eng
        self.fn = fn
        self.is_dma = is_dma
        self.key = key
        self.dma_val = 0
        self.signal = False
        self.milestone = 0
        self.waits = []


class Sched:
    def __init__(self, nc, es):
        self.nc = nc
        self.es = es
        self.esem = {e: es.enter_context(nc.semaphore("s_" + e)) for e in ENGS}
        self.ksem = {}
        self.key_count = {}
        self.eng_base = {e: 0 for e in ENGS}
        self._reset()

    def _reset(self):
        self.ops = []
        self.per_eng = {e: [] for e in ENGS}
        self.last_w = {}
        self.readers = {}
        self.seen = {e: {f: -1 for f in ENGS} for e in ENGS}
        self.seen_dma = {e: {} for e in ENGS}
        self.eng_group = {e: None for e in ENGS}
        self.seen_saved = {e: None for e in ENGS}

    cur_group = None
    cnt_ap = None

    def _add(self, eng, fn, reads, writes, is_dma, key, group=None):
        o = _Op(len(self.ops), eng, fn, is_dma, key)
        o.group = group
        if group != self.eng_group[eng]:
            if self.eng_group[eng] is not None:
                self.seen[eng], self.seen_dma[eng] = self.seen_saved[eng]
            if group is not None:
                self.seen_saved[eng] = (dict(self.seen[eng]), dict(self.seen_dma[eng]))
            self.eng_group[eng] = group
        deps = set()
        for b in reads:
            w = self.last_w.get(b)
            if w is not None:
                deps.add(w)
        for b in writes:
            w = self.last_w.get(b)
            if w is not None:
                deps.add(w)
            for r in self.readers.get(b, ()):
                deps.add(r)
        for b in reads:
            self.readers.setdefault(b, []).append(o.idx)
        for b in writes:
            self.last_w[b] = o.idx
            self.readers[b] = []
        for di in sorted(deps):
            d = self.ops[di]
            if d.is_dma:
                if self.seen_dma[eng].get(d.key, 0) >= d.dma_val:
                    continue
                self.seen_dma[eng][d.key] = d.dma_val
                o.waits.append(("dma", d.key, d.dma_val))
            else:
                if d.eng == eng and not is_dma and eng == "pe":
                    continue
                if self.seen[eng][d.eng] >= d.idx:
                    continue
                self.seen[eng][d.eng] = d.idx
                d.signal = True
                o.waits.append(("eng", d.eng, d.idx))
        if is_dma:
            if key not in self.ksem:
                self.ksem[key] = self.es.enter_context(self.nc.semaphore("k%d" % len(self.ksem)))
                self.key_count[key] = 0
            self.key_count[key] += 16
            o.dma_val = self.key_count[key]
        self.ops.append(o)
        self.per_eng[eng].append(o)
        return o

    _cap = None

    def op(self, eng, fn, reads=(), writes=()):
        if self._cap is not None:
            self._cap.append((eng, fn, tuple(reads), tuple(writes), False, None, self.cur_group))
            return None
        return self._add(eng, fn, tuple(reads), tuple(writes), False, None, self.cur_group)

    def dma(self, eng, fn, key, reads=(), writes=()):
        if self._cap is not None:
            self._cap.append((eng, fn, tuple(reads), tuple(writes), True, key, self.cur_group))
            return None
        return self._add(eng, fn, tuple(reads), tuple(writes), True, key, self.cur_group)

    def capture(self, f, *a, **kw):
        assert self._cap is None
        self._cap = []
        try:
            r = f(*a, **kw)
        finally:
            c = self._cap
            self._cap = None
        return c, r

    def replay(self, items):
        for it in items:
            self._add(*it)

    limit = None
    nflush = 0

    def flush(self):
        self.nflush += 1
        if self.limit is not None and self.nflush > self.limit:
            for o in self.ops:
                if o.is_dma:
                    self.key_count[o.key] -= 16
            self._reset()
            return
        nc = self.nc
        ops = self.ops
        final = {}
        self.eng_base_prev = dict(self.eng_base)
        for e in ENGS:
            lst = [o for o in self.per_eng[e] if not o.is_dma]
            if e != "sp" and lst:
                lst[-1].signal = True
            n = self.eng_base[e]
            for o in self.per_eng[e]:
                if o.signal and not o.is_dma:
                    n += 1
                    o.milestone = n
            self.eng_base[e] = n
            final[e] = n
        esem, ksem = self.esem, self.ksem
        keyvals = dict(self.key_count)
        per_eng = self.per_eng

        regs = self.__dict__.setdefault("_regs", {})

        def emit_one(ename, eng, o):
            for w in o.waits:
                if w[0] == "dma":
                    eng.wait_ge(ksem[w[1]], w[2])
                else:
                    eng.wait_ge(esem[w[1]], ops[w[2]].milestone)
            inst = o.fn(eng)
            if o.is_dma:
                inst.then_inc(ksem[o.key], 16)
            elif o.signal:
                inst.then_inc(esem[ename], 1)

        def emit(ename, eng):
            lst = per_eng[ename]
            ms = self.eng_base_prev[ename]
            k = 0
            while k < len(lst):
                o = lst[k]
                if o.group is None:
                    emit_one(ename, eng, o)
                    if o.signal and not o.is_dma:
                        ms = o.milestone
                    k += 1
                    continue
                k2 = k
                while k2 < len(lst) and lst[k2].group == o.group:
                    k2 += 1
                run = lst[k:k2]
                if ename not in regs:
                    regs[ename] = eng.alloc_register("cnt_" + ename)
                r = regs[ename]
                eng.reg_load(r, self.cnt_ap(o.group[0]))
                nsig = sum(1 for x in run if x.signal and not x.is_dma)
                with eng.If_lt(r, o.group[1]):
                    if nsig:
                        if ms > 0:
                            eng.wait_ge(esem[ename], ms)
                        eng.sem_inc(esem[ename], nsig)
                    for x in run:
                        if x.is_dma:
                            if x.dma_val > 16:
                                eng.wait_ge(ksem[x.key], x.dma_val - 16)
                            eng.sem_inc(ksem[x.key], 16)
                with eng.Else():
                    for x in run:
                        emit_one(ename, eng, x)
                ms += nsig
                k = k2
            for kk, v in keyvals.items():
                if v > 0:
                    eng.wait_ge(ksem[kk], v)
            for f in ENGS:
                if f != ename and f != "sp" and final[f] > 0:
                    eng.wait_ge(esem[f], final[f])

        with nc.Block() as block:
            @block.tensor
            def _(eng):
                emit("pe", eng)

            @block.scalar
            def _(eng):
                emit("act", eng)

            @block.vector
            def _(eng):
                emit("dve", eng)

            @block.gpsimd
            def _(eng):
                emit("pool", eng)

            @block.sync
            def _(eng):
                emit("sp", eng)
        self._reset()


def _rope_tables():
    p = np.arange(128)
    j = p % 16
    inv_freq = (10000.0 ** (-(j.astype(np.float64)) / 16.0))
    t = np.arange(SEQ)
    rows = t // 64
    cols = t % 64
    pos = np.where(((p % 64) < 32)[:, None], rows[None, :], cols[None, :]).astype(np.float64)
    ang = pos * inv_freq[:, None]
    ang = (pos.astype(np.float32) * inv_freq.astype(np.float32)[:, None]).astype(np.float32)
    cosT = np.cos(ang).astype(np.float32)
    sinT = np.sin(ang).astype(np.float32)
    sign = np.where((p % 32) < 16, -1.0, 1.0).astype(np.float32)[:, None]
    sinT = sinT * sign
    perm = np.where((p % 32) < 16, p + 16, p - 16)
    Pm = np.zeros((128, 128), np.float32)
    Pm[perm, p] = 1.0
    return cosT, sinT, Pm


def _band_masks():
    k = np.arange(128)[:, None]
    q = np.arange(128)[None, :]
    prev = (k >= q).astype(np.float32)
    nxt = (k <= q).astype(np.float32)
    return np.stack([prev, nxt], 0)


def _nat_patterns():
    pats = {}
    plist = []
    chunks = {}
    for qt in range(16):
        r0, r1 = 2 * qt, 2 * qt + 1
        rs0 = min(max(r0 - 4, 0), 24)
        rs1 = min(max(r1 - 4, 0), 24)
        lo = rs0 // 2
        hi = (rs1 + 7) // 2
        chunks[qt] = list(range(lo, hi + 1))
        for kc in chunks[qt]:
            interior = 2 <= qt <= 13
            key = ("i", kc - qt) if interior else ("b", qt, kc)
            if key not in pats:
                pats[key] = len(plist)
                plist.append((qt, kc))
            pats[(qt, kc)] = pats[key]
    npat = len(plist)
    dr = np.zeros((npat, 128, 128), np.int64)
    dc = np.zeros((npat, 128, 128), np.int64)
    mk = np.zeros((npat, 128, 128), np.float32)
    kk = np.arange(128)
    for pi, (qt, kc) in enumerate(plist):
        kr = 2 * kc + kk // 64
        kcol = kk % 64
        qr = 2 * qt + kk // 64
        qcol = kk % 64
        rs = np.clip(qr - 4, 0, 24)
        qs = np.clip(qcol - 8, 0, 48)
        vrow = (kr[:, None] >= rs[None, :]) & (kr[:, None] < rs[None, :] + 8)
        vcol = (kcol[:, None] >= qs[None, :]) & (kcol[:, None] < qs[None, :] + 16)
        mk[pi] = (vrow & vcol).astype(np.float32)
        dr[pi] = np.clip(kr[:, None] - qr[None, :] + 7, 0, 14)
        dc[pi] = np.clip(kcol[:, None] - qcol[None, :] + 15, 0, 30)
    return pats, chunks, dr, dc, mk, npat


_NATP = _nat_patterns()
NPAT = _NATP[5]


def build_program(limit=None, debug=False):
    nc = bass.Bass("TRN2", target_bir_lowering=False)

    _uid = [0]

    def un(name):
        _uid[0] += 1
        return "%s_u%d" % (name, _uid[0])

    def din(name, shape, dt=F32):
        return nc.dram_tensor(name, list(shape), dt, kind="ExternalInput").ap()

    x_in = din("x", [NB, SEQ, D])
    ctx_in = din("ctx", [NB, LCTX, D])
    c_in = din("c", [NB, D])
    cctx_in = din("c_ctx", [1, D])
    w_ada = din("w_ada", [DEPTH, D, 6 * D])
    b_ada = din("b_ada", [DEPTH, 6 * D])
    ln1_g = din("ln1_g", [DEPTH, D])
    ln1_b = din("ln1_b", [DEPTH, D])
    ln2_g = din("ln2_g", [DEPTH, D])
    ln2_b = din("ln2_b", [DEPTH, D])
    attn_wqk = din("attn_wqk", [2, D, 1536])
    attn_wv = din("attn_wv", [2, D, 256])
    attn_wo = din("attn_w_o", [2, D, D])
    attn_sink = din("attn_sink", [2, 16])
    conv_w_in = din("conv_w_in", [1, D, 3 * D])
    conv_w = din("conv_w", [1, 3, D])
    conv_w_out = din("conv_w_out", [1, D, D])
    nat_wqk = din("nat_wqk", [1, D, 2048])
    nat_wv = din("nat_wv", [1, D, 1024])
    nat_wo = din("nat_w_o", [1, D, D])
    nat_bias = din("nat_bias", [NPAT, 128, 16, 128])
    router_w = din("router_w", [DEPTH, D, 36])
    router_b = din("router_b", [DEPTH, 36])
    w_gate = din("expert_w_gate", [DEPTH, NEXP, D, FF])
    w_up = din("expert_w_up", [DEPTH, NEXP, D, FF])
    w_down = din("expert_w_down", [DEPTH, NEXP, FF, D])
    k_ident = din("k_ident", [128, 128])
    k_cos = din("k_cos", [128, SEQ])
    k_sin = din("k_sin", [128, SEQ])
    k_pm = din("k_pm", [128, 128])
    k_band = din("k_band", [2, 128, 128])
    k_natmask = din("k_natmask", [NPAT, 128, 128])
    k_ustrict = din("k_ustrict", [128, 128])
    k_eoff = din("k_eoff", [1, 32])

    out = nc.dram_tensor("out", [NB, SEQ, D], F32, kind="ExternalOutput").ap()

    X = nc.dram_tensor("X_scr", [NT * 128, D], F32).ap()
    MODR = nc.dram_tensor("MODR_scr", [DEPTH, 3, 6, D], F32).ap()
    XS = nc.dram_tensor("XS_scr", [NSLOT, D], BF16).ap()
    YS = nc.dram_tensor("YS_scr", [NSLOT, D], F32).ap()
    ETAB = nc.dram_tensor("ETAB_scr", [NPAT, 128, 16, 128], BF16).ap()
    QT = nc.dram_tensor("QT_scr", [128, 8, 2304], BF16).ap()

    DBG = {}
    if debug:
        DBG["T"] = nc.dram_tensor("dbgT", [4, 128, D], F32, kind="ExternalOutput").ap()
        DBG["O"] = nc.dram_tensor("dbgO", [128, D], BF16, kind="ExternalOutput").ap()
        DBG["Den"] = nc.dram_tensor("dbgDen", [128, 16], F32, kind="ExternalOutput").ap()
        DBG["H2"] = nc.dram_tensor("dbgH2", [128, D], BF16, kind="ExternalOutput").ap()
        DBG["RT"] = nc.dram_tensor("dbgRT", [128, 256], F32, kind="ExternalOutput").ap()
        DBG["tile"] = 0
        DBG["OA"] = nc.dram_tensor("dbgOA", [128, 3, 512], F32, kind="ExternalOutput").ap()
        DBG["SK"] = nc.dram_tensor("dbgSK", [128, 16], F32, kind="ExternalOutput").ap()

    def dbg_store(key, idx, ap, bid, tt):
        if not DBG or tt != DBG["tile"] or DBG.get("done_" + key + str(idx)):
            return
        DBG["done_" + key + str(idx)] = True
        dst = DBG[key][idx] if idx is not None else DBG[key]
        S.dma("sp", lambda e: e.dma_start(out=dst, in_=ap), "dbg" + key + str(idx), reads=[bid])

    with ExitStack() as ges:
        S = Sched(nc, ges)
        S.limit = limit

        def gsb(name, shape, dt):
            return ges.enter_context(nc.sbuf_tensor(un(name), list(shape), dt))

        identf = gsb("identf", [128, 128], F32)
        identb = gsb("identb", [128, 128], BF16)
        ustrict = gsb("ustrict", [128, 128], BF16)
        onesb = gsb("onesb", [128, 128], BF16)
        zerob = gsb("zerob", [128, 512], BF16)
        ones13 = gsb("ones13", [1, 4], F32)
        csT = gsb("csT", [128, 8, 3], F32)
        eoff = gsb("eoff", [128, 32], F32)
        destI = gsb("destI", [128, NT, 2], I32)
        gates = gsb("gates", [128, NT, 2], F32)
        cnt = gsb("cnt", [128, 32], F32)
        cnti = gsb("cnti", [128, 32], I32)
        S.cnt_ap = lambda idx: cnti[0:1, idx:idx + 1]

        _preg = {}

        def breg(e):
            if "r" not in _preg:
                _preg["r"] = e.to_reg(NSLOT - 1)
            return _preg["r"]

        def tile_src(layer, tt):
            b, j = divmod(tt, TPB)
            if layer == 0:
                if j < 16:
                    return x_in[b, j * 128:(j + 1) * 128, :]
                return ctx_in[b, (j - 16) * 128:(j - 15) * 128, :]
            return X[tt * 128:(tt + 1) * 128, :]

        def phase_init():
            with ExitStack() as es:
                tmpf = es.enter_context(nc.sbuf_tensor(un("init_tmp"), [128, 128], F32))
                S.dma("sp", lambda e: e.dma_start(out=identf[:], in_=k_ident), "c0", writes=["identf"])
                S.op("dve", lambda e: e.tensor_copy(out=identb[:], in_=identf[:]), reads=["identf"], writes=["identb"])
                S.dma("sp", lambda e: e.dma_start(out=tmpf[:], in_=k_ustrict), "c1", writes=["tmpf"])
                S.op("dve", lambda e: e.tensor_copy(out=ustrict[:], in_=tmpf[:]), reads=["tmpf"], writes=["ustrict"])
                S.op("pool", lambda e: e.memset(onesb[:], 1.0), writes=["onesb"])
                S.op("pool", lambda e: e.memset(zerob[:], 0.0), writes=["zerob"])
                S.op("pool", lambda e: e.memset(ones13[:], 1.0), writes=["ones13"])
                for v in range(3):
                    srcv = c_in[v, :] if v < 2 else cctx_in[0, :]
                    S.dma("sp", lambda e, v=v, srcv=srcv: e.dma_start(out=csT[:, :, v], in_=srcv.rearrange("(c p) -> p c", p=128),
                                                                       allow_slow_non_contiguous=True), "c2", writes=["csT"])
                S.op("act", lambda e: e.activation(out=csT[:], in_=csT[:], func=AF.Silu), reads=["csT"], writes=["csT"])
                S.dma("sp", lambda e: e.dma_start(out=eoff[:], in_=k_eoff[0, :].partition_broadcast(128)), "c4", writes=["eoff"])
                S.flush()

        def phase_ada(i):
            with ExitStack() as es:
                def sb(name, shape, dt):
                    return es.enter_context(nc.sbuf_tensor(un(name), list(shape), dt))
                wst = [sb("ada_w%d" % k, [128, 8, 512], F32) for k in range(2)]
                brow = sb("ada_b", [1, 6 * D], F32)
                modrow = sb("ada_mod", [3, 6 * D], F32)
                lng = sb("ada_lng", [3, 2, D], F32)
                outrow = sb("ada_out", [3, 6, D], F32)
                tmp = sb("ada_tmp", [3, D], F32)
                pm = [es.enter_context(nc.psum_tensor(un("ada_ps%d" % k), [3, 512], F32)) for k in range(2)]
                S.dma("sp", lambda e: e.dma_start(out=brow[:], in_=b_ada[i:i + 1, :]), "ab", writes=["brow"])
                S.dma("sp", lambda e: e.dma_start(out=lng[:, 0, :], in_=ln1_g[i, :].partition_broadcast(3)), "al", writes=["lng"])
                S.dma("sp", lambda e: e.dma_start(out=lng[:, 1, :], in_=ln1_b[i, :].partition_broadcast(3)), "al", writes=["lng"])
                wv = w_ada[i].rearrange("(c p) n -> p c n", p=128)
                for n in range(12):
                    k = n % 2
                    S.dma("sp", lambda e, n=n, k=k: e.dma_start(out=wst[k][:], in_=wv[:, :, n * 512:(n + 1) * 512]),
                          "aw%d" % k, writes=["wst%d" % k])
                    for c in range(8):
                        S.op("pe", lambda e, c=c, k=k: e.matmul(pm[k][:], lhsT=csT[:, c, :], rhs=wst[k][:, c, :],
                                                                 start=(c == 0), stop=False),
                             reads=["csT", "wst%d" % k], writes=["pm%d" % k])
                    S.op("pe", lambda e, n=n, k=k: e.matmul(pm[k][:], lhsT=ones13[0:1, 0:3], rhs=brow[0:1, n * 512:(n + 1) * 512],
                                                             start=False, stop=True),
                         reads=["ones13", "brow"], writes=["pm%d" % k])
                    S.op("act", lambda e, n=n, k=k: e.copy(out=modrow[:, n * 512:(n + 1) * 512], in_=pm[k][:]),
                         reads=["pm%d" % k], writes=["modrow"])

                def m(k):
                    return modrow[:, k * D:(k + 1) * D]
                S.op("dve", lambda e: e.tensor_copy(out=outrow[:, 0, :], in_=m(0)), reads=["modrow"], writes=["o0"])
                S.op("dve", lambda e: e.tensor_scalar_add(out=outrow[:, 1, :], in0=m(1), scalar1=1.0), reads=["modrow"], writes=["o1"])
                S.op("dve", lambda e: e.tensor_scalar_mul(out=outrow[:, 2, :], in0=m(2), scalar1=INV_ALPHA), reads=["modrow"], writes=["o2"])
                S.op("dve", lambda e: e.tensor_scalar_add(out=tmp[:], in0=m(4), scalar1=1.0), reads=["modrow"], writes=["tmp"])
                S.op("dve", lambda e: e.tensor_tensor(out=outrow[:, 3, :], in0=tmp[:], in1=lng[:, 0, :], op=ALU.mult),
                     reads=["tmp", "lng"], writes=["o3"])
                S.op("dve", lambda e: e.tensor_tensor(out=outrow[:, 4, :], in0=tmp[:], in1=lng[:, 1, :], op=ALU.mult),
                     reads=["tmp", "lng"], writes=["o4"])
                S.op("dve", lambda e: e.tensor_tensor(out=outrow[:, 4, :], in0=outrow[:, 4, :], in1=m(3), op=ALU.add),
                     reads=["o4", "modrow"], writes=["o4"])
                S.op("dve", lambda e: e.tensor_scalar_mul(out=outrow[:, 5, :], in0=m(5), scalar1=INV_ALPHA), reads=["modrow"], writes=["o5"])
                S.dma("sp", lambda e: e.dma_start(out=MODR[i], in_=outrow[:]), "ao",
                      reads=["o0", "o1", "o2", "o3", "o4", "o5"], writes=["MODR"])
                S.flush()

        class Rot:
            def __init__(self, es, name, n, shape, dt, psum=False):
                self.bufs = []
                for k in range(n):
                    if psum:
                        t = es.enter_context(nc.psum_tensor(un("%s%d" % (name, k)), list(shape), dt))
                    else:
                        t = es.enter_context(nc.sbuf_tensor(un("%s%d" % (name, k)), list(shape), dt))
                    self.bufs.append((t, "%s%d" % (name, k)))
                self.i = 0

            def next(self):
                r = self.bufs[self.i % len(self.bufs)]
                self.i += 1
                return r

        def layer_norm_core(es_bufs, t, t_id, xn, xn_id):
            st, mv, rs = es_bufs
            stt, st_id = st.next()
            mvt, mv_id = mv.next()
            rst, rs_id = rs.next()
            S.op("dve", lambda e: e.bn_stats(out=stt[:, 0, :], in_=t[:, 0:512]), reads=[t_id], writes=[st_id])
            S.op("dve", lambda e: e.bn_stats(out=stt[:, 1, :], in_=t[:, 512:1024]), reads=[t_id], writes=[st_id])
            S.op("dve", lambda e: e.bn_aggr(out=mvt[:], in_=stt[:].rearrange("p a b -> p (a b)")), reads=[st_id], writes=[mv_id])
            S.op("dve", lambda e: e.tensor_scalar_add(out=rst[:, 0:1], in0=mvt[:, 1:2], scalar1=EPS2), reads=[mv_id], writes=[rs_id])
            S.op("act", lambda e: e.activation(out=rst[:, 0:1], in_=rst[:, 0:1], func=AF.Ln), reads=[rs_id], writes=[rs_id])
            S.op("act", lambda e: e.activation(out=rst[:, 0:1], in_=rst[:, 0:1], func=AF.Exp, scale=-0.5), reads=[rs_id], writes=[rs_id])
            S.op("dve", lambda e: e.scalar_tensor_tensor(out=rst[:, 1:2], in0=mvt[:, 0:1], scalar=-1.0, in1=rst[:, 0:1],
                                                         op0=ALU.mult, op1=ALU.mult), reads=[mv_id, rs_id], writes=[rs_id])
            S.op("act", lambda e: e.activation(out=xn[:], in_=t[:], func=AF.Identity, scale=rst[:, 0:1], bias=rst[:, 1:2]),
                 reads=[t_id, rs_id], writes=[xn_id])

        class MixCtx:
            pass

        def mixer_common_alloc(es, i):
            M = MixCtx()

            def sb(name, shape, dt):
                return es.enter_context(nc.sbuf_tensor(un(name), list(shape), dt))
            M.G1 = sb("mx_G1", [128, D], F32)
            M.A2 = sb("mx_A2", [128, D], F32)
            M.S2 = sb("mx_S2", [128, D], F32)
            M.lg = sb("mx_lg", [128, D], F32)
            M.lb = sb("mx_lb", [128, D], F32)
            M.wr = sb("mx_wr", [128, 8, 36], BF16)
            M.wrf = sb("mx_wrf", [128, 8, 36], F32)
            M.rb = sb("mx_rb", [128, 36], F32)
            M.xt = Rot(es, "mx_xt", 2, [128, D], F32)
            M.t = Rot(es, "mx_t", 2, [128, D], F32)
            M.xn = Rot(es, "mx_xn", 2, [128, D], F32)
            M.h2 = Rot(es, "mx_h2", 2, [128, D], BF16)
            M.h2T = Rot(es, "mx_h2T", 2, [128, 8, 128], BF16)
            M.st = Rot(es, "mx_st", 2, [128, 2, 6], F32)
            M.mv = Rot(es, "mx_mv", 2, [128, 2], F32)
            M.rs = Rot(es, "mx_rs", 2, [128, 2], F32)
            M.rt = Rot(es, "mx_rt", 2, [128, 256], F32)
            M.selb = Rot(es, "mx_sel", 2, [128, 32], BF16)
            M.cur_v = None
            S.dma("pool", lambda e: e.dma_start(out=M.wr[:], in_=router_w[i].rearrange("(c p) n -> p c n", p=128)), "mwr", writes=["wr"])
            S.dma("sp", lambda e: e.dma_start(out=M.rb[:], in_=router_b[i, :].partition_broadcast(128)), "mrb", writes=["rb"])
            S.dma("sp", lambda e: e.dma_start(out=M.lg[:], in_=ln1_g[i, :].partition_broadcast(128)), "mlg", writes=["lg"])
            S.dma("sp", lambda e: e.dma_start(out=M.lb[:], in_=ln1_b[i, :].partition_broadcast(128)), "mlb", writes=["lb"])
            return M

        def load_variant(M, i, v):
            if M.cur_v == v:
                return
            M.cur_v = v
            S.dma("sp", lambda e: e.dma_start(out=M.G1[:], in_=MODR[i, v, 2, :].partition_broadcast(128)), "mG1", writes=["G1"])
            S.dma("sp", lambda e: e.dma_start(out=M.A2[:], in_=MODR[i, v, 3, :].partition_broadcast(128)), "mA2", writes=["A2"])
            S.dma("sp", lambda e: e.dma_start(out=M.S2[:], in_=MODR[i, v, 4, :].partition_broadcast(128)), "mS2", writes=["S2"])

        def out_stage(M, P, i, tt, po, po_ids):
            xt, xt_id = M.xt.next()
            t, t_id = M.t.next()
            xn, xn_id = M.xn.next()
            h2, h2_id = M.h2.next()
            h2T, h2T_id = M.h2T.next()
            rt, rt_id = M.rt.next()
            selb, selb_id = M.selb.next()
            src = tile_src(i, tt)
            S.dma("sp", lambda e: e.dma_start(out=xt[:], in_=src), xt_id, writes=[xt_id])
            S.op("dve", lambda e: e.tensor_tensor(out=t[:], in0=po, in1=M.G1[:], op=ALU.mult), reads=list(po_ids) + ["G1"], writes=[t_id])
            dbg_store("T", 0, t[:], t_id, tt)
            S.op("dve", lambda e: e.tensor_tensor(out=t[:], in0=t[:], in1=xt[:], op=ALU.add), reads=[t_id, xt_id], writes=[t_id])
            dbg_store("T", 1, t[:], t_id, tt)
            layer_norm_core((M.st, M.mv, M.rs), t, t_id, xn, xn_id)
            dbg_store("T", 2, xn[:], xn_id, tt)
            S.op("dve", lambda e: e.tensor_tensor(out=t[:], in0=xn[:], in1=M.A2[:], op=ALU.mult), reads=[xn_id, "A2"], writes=[t_id])
            S.op("dve", lambda e: e.tensor_tensor(out=h2[:], in0=t[:], in1=M.S2[:], op=ALU.add), reads=[t_id, "S2"], writes=[h2_id])
            dbg_store("H2", None, h2[:], h2_id, tt)
            S.op("dve", lambda e: e.tensor_tensor(out=xn[:], in0=xn[:], in1=M.lg[:], op=ALU.mult), reads=[xn_id, "lg"], writes=[xn_id])
            S.op("dve", lambda e: e.tensor_tensor(out=xn[:], in0=xn[:], in1=M.lb[:], op=ALU.add), reads=[xn_id, "lb"], writes=[xn_id])
            S.dma("sp", lambda e: e.dma_start(out=X[tt * 128:(tt + 1) * 128, :], in_=xn[:]), "st_" + xn_id, reads=[xn_id], writes=["X%d" % tt])
            for hf4 in range(2):
                for c in range(4):
                    S.op("pe", lambda e, c=c, hf4=hf4: e.transpose(out=P.ptb[:, c, :], in_=h2[:, (hf4 * 4 + c) * 128:(hf4 * 4 + c + 1) * 128], identity=identb[:]),
                         reads=[h2_id, "identb"], writes=["ptb"])
                S.op("act", lambda e, hf4=hf4: e.copy(out=h2T[:, hf4 * 4:(hf4 + 1) * 4, :], in_=P.ptb), reads=["ptb"], writes=[h2T_id])
            for c in range(8):
                S.op("pe", lambda e, c=c: e.matmul(P.plg[:, 0:36], lhsT=h2T[:, c, :], rhs=M.wr[:, c, :], start=(c == 0), stop=(c == 7)),
                     reads=[h2T_id, "wr"], writes=["plg"])
            if not (KDBG & 2):
                route(M, P, tt, rt, rt_id, selb, selb_id)
            dbg_store("RT", None, rt[:], rt_id, tt)
            for k in range(2 if not (KDBG & 6) else 0):
                S.dma("pool", lambda e, k=k: e.indirect_dma_start(
                    out=XS, out_offset=bass.IndirectOffsetOnAxis(ap=destI[:, tt, k:k + 1], axis=0),
                    in_=h2[:], in_offset=None, bounds_check=breg(e), oob_is_err=False),
                    "sc_" + h2_id + str(k), reads=[h2_id, "destI%d" % tt], writes=["XS"])

        def route(M, P, tt, rt, rt_id, selb, selb_id):
            lgt = rt[:, 0:36]
            gl = rt[:, 0:4]
            el = rt[:, 4:36]
            gm = rt[:, 40:44]
            pen = rt[:, 44:48]
            em = rt[:, 48:80]
            oh1 = rt[:, 80:112]
            em2 = rt[:, 112:144]
            oh2 = rt[:, 144:176]
            slot = rt[:, 176:208]
            prod = rt[:, 208:240]
            sc = rt[:, 240:256]
            gex = rt[:, 36:40]
            W = [rt_id]
            R = [rt_id]

            def dv(fn, extra_r=(), extra_w=()):
                S.op("dve", fn, reads=R + list(extra_r), writes=W + list(extra_w))
            dv(lambda e: e.tensor_tensor(out=lgt, in0=P.plg[:, 0:36], in1=M.rb[:], op=ALU.add), extra_r=["plg", "rb"])
            dv(lambda e: e.reduce_max(out=sc[:, 0:1], in_=gl, axis=AX))
            dv(lambda e: e.tensor_scalar(out=gm, in0=gl, scalar1=sc[:, 0:1], scalar2=None, op0=ALU.is_ge))
            dv(lambda e: e.tensor_scalar_mul(out=sc[:, 1:2], in0=sc[:, 0:1], scalar1=-1.0))
            S.op("act", lambda e: e.activation(out=gex, in_=gl, func=AF.Exp, bias=sc[:, 1:2], scale=1.0, accum_out=sc[:, 2:3]),
                 reads=R, writes=W)
            dv(lambda e: e.reciprocal(out=sc[:, 3:4], in_=sc[:, 2:3]))
            dv(lambda e: e.tensor_scalar(out=pen, in0=gm, scalar1=BIG, scalar2=-BIG, op0=ALU.mult, op1=ALU.add))
            dv(lambda e: e.tensor_tensor(out=em.rearrange("p (g k) -> p g k", k=8), in0=el.rearrange("p (g k) -> p g k", k=8),
                                         in1=pen.unsqueeze(2).to_broadcast([128, 4, 8]), op=ALU.add))
            dv(lambda e: e.reduce_max(out=sc[:, 4:5], in_=em, axis=AX))
            dv(lambda e: e.tensor_scalar(out=oh1, in0=em, scalar1=sc[:, 4:5], scalar2=None, op0=ALU.is_ge))
            dv(lambda e: e.scalar_tensor_tensor(out=em2, in0=oh1, scalar=-BIG, in1=em, op0=ALU.mult, op1=ALU.add))
            dv(lambda e: e.reduce_max(out=sc[:, 5:6], in_=em2, axis=AX))
            dv(lambda e: e.tensor_scalar(out=oh2, in0=em2, scalar1=sc[:, 5:6], scalar2=None, op0=ALU.is_ge))
            dv(lambda e: e.tensor_tensor(out=sc[:, 6:7], in0=sc[:, 5:6], in1=sc[:, 4:5], op=ALU.subtract))
            S.op("act", lambda e: e.activation(out=sc[:, 7:8], in_=sc[:, 6:7], func=AF.Exp), reads=R, writes=W)
            dv(lambda e: e.tensor_scalar_add(out=sc[:, 7:8], in0=sc[:, 7:8], scalar1=1.0))
            dv(lambda e: e.reciprocal(out=sc[:, 8:9], in_=sc[:, 7:8]))
            dv(lambda e: e.tensor_tensor(out=gates[:, tt, 0:1], in0=sc[:, 8:9], in1=sc[:, 3:4], op=ALU.mult), extra_w=["gates%d" % tt])
            dv(lambda e: e.tensor_tensor(out=gates[:, tt, 1:2], in0=sc[:, 3:4], in1=gates[:, tt, 0:1], op=ALU.subtract),
               extra_r=["gates%d" % tt], extra_w=["gates%d" % tt])
            dv(lambda e: e.tensor_tensor(out=selb[:], in0=oh1, in1=oh2, op=ALU.add), extra_w=[selb_id])
            S.op("pe", lambda e: e.matmul(P.plg[:, 64:96], lhsT=ustrict[:], rhs=selb[:], start=True, stop=True),
                 reads=[selb_id, "ustrict"], writes=["prk"])
            S.op("pe", lambda e: e.matmul(P.plg[:, 128:160], lhsT=onesb[:], rhs=selb[:], start=True, stop=True),
                 reads=[selb_id, "onesb"], writes=["ptot"])
            dv(lambda e: e.tensor_tensor(out=slot, in0=P.plg[:, 64:96], in1=cnt[:], op=ALU.add), extra_r=["prk", "cnt"])
            dv(lambda e: e.tensor_scalar(out=prod, in0=slot, scalar1=float(CAP), scalar2=4.0e6, op0=ALU.is_ge, op1=ALU.mult))
            dv(lambda e: e.tensor_tensor(out=slot, in0=slot, in1=prod, op=ALU.add))
            dv(lambda e: e.tensor_tensor(out=slot, in0=slot, in1=eoff[:], op=ALU.add), extra_r=["eoff"])
            dv(lambda e: e.tensor_tensor(out=cnt[:], in0=cnt[:], in1=P.plg[:, 128:160], op=ALU.add), extra_r=["ptot", "cnt"], extra_w=["cnt"])
            dv(lambda e: e.tensor_tensor(out=prod, in0=oh1, in1=slot, op=ALU.mult))
            dv(lambda e: e.reduce_sum(out=sc[:, 9:10], in_=prod, axis=AX))
            dv(lambda e: e.tensor_tensor(out=prod, in0=oh2, in1=slot, op=ALU.mult))
            dv(lambda e: e.reduce_sum(out=sc[:, 10:11], in_=prod, axis=AX))
            dv(lambda e: e.tensor_copy(out=destI[:, tt, :], in_=sc[:, 9:11]), extra_w=["destI%d" % tt])

        def make_hT(es, P, i, b, hT, scal):
            xr = Rot(es, "hx_x", 3, [128, D], F32)
            for seg, v in ((0, b), (1, 2)):
                S.dma("sp", lambda e, v=v: e.dma_start(out=scal[:], in_=MODR[i, v, 0:2, :].rearrange("k (c p) -> p k c", p=128),
                                                        allow_slow_non_contiguous=True), "hsc", writes=["scal"])
                tiles = range(16) if seg == 0 else range(16, 18)
                for j in tiles:
                    tt = b * TPB + j
                    xt, xid = xr.next()
                    src = tile_src(i, tt)
                    S.dma("sp", lambda e, xt=xt, src=src: e.dma_start(out=xt[:], in_=src), xid, reads=["X%d" % tt], writes=[xid])
                    for c in range(8):
                        S.op("pe", lambda e, c=c, xt=xt: e.transpose(out=P.ptf[:, c, :], in_=xt[:, c * 128:(c + 1) * 128], identity=identf[:]),
                             reads=[xid, "identf"], writes=["ptf"])
                    for c in range(8):
                        if c % 2 == 0:
                            S.op("act", lambda e, c=c, j=j: e.activation(out=hT[:, c, j * 128:(j + 1) * 128], in_=P.ptf[:, c, :], func=AF.Identity,
                                                                          scale=scal[:, 1, c:c + 1], bias=scal[:, 0, c:c + 1]),
                                 reads=["ptf", "scal"], writes=["hT"])
                        else:
                            S.op("dve", lambda e, c=c, j=j: e.tensor_scalar(out=hT[:, c, j * 128:(j + 1) * 128], in0=P.ptf[:, c, :],
                                                                             scalar1=scal[:, 1, c:c + 1], scalar2=scal[:, 0, c:c + 1],
                                                                             op0=ALU.mult, op1=ALU.add),
                                 reads=["ptf", "scal"], writes=["hT"])

        HB = [(0, h) if h < 6 else ((1, h - 6) if h < 11 else (2, h - 11)) for h in range(16)]
        TOKG = [(0, 512), (512, 512), (1024, 512), (1536, 512), (2048, 256)]

        class PsumSet:
            pass

        def alloc_psum(es):
            P = PsumSet()

            def ps(name, shape, dt):
                return es.enter_context(nc.psum_tensor(un(name), list(shape), dt))
            P.ptf = ps("ps_ptf", [128, 8, 128], F32)
            P.pA = ps("ps_A", [128, 512], F32)
            P.pB = ps("ps_B", [128, 512], F32)
            P.oacc = ps("ps_oacc", [128, 3, 512], F32)
            P.b7 = ps("ps_b7", [128, 512], F32)
            P.ptb = P.b7[:, 0:256].bitcast(BF16).rearrange("p (c t) -> p c t", t=128)
            P.plg = P.b7[:, 256:512]
            P.pC = P.oacc[:, 0, :]
            return P

        def phase_attn(i, j, kind, last):
            is_a = kind == 0
            nkv = 4 if is_a else 16
            nqk_cols = 1536 if is_a else 2048
            nk_chunks = 4 if is_a else 8
            nv_cols = 256 if is_a else 1024
            wqk = attn_wqk[j] if is_a else nat_wqk[j]
            wv = attn_wv[j] if is_a else nat_wv[j]
            wo = attn_wo[j] if is_a else nat_wo[j]
            scale = 0.125
            if not is_a:
                pats, nat_chunks = _NATP[0], _NATP[1]
            for b in range(NB):
                with ExitStack() as es:
                    def sb(name, shape, dt):
                        return es.enter_context(nc.sbuf_tensor(un(name), list(shape), dt))
                    P = alloc_psum(es)
                    kT = sb("at_kT", [128, nk_chunks, 2304], BF16)
                    va = sb("at_va", [128, TPB, nkv, 65], BF16)
                    S.op("pool", lambda e: e.memset(va[:], 1.0), writes=["va"])
                    with ExitStack() as es1:
                        def sb1(name, shape, dt):
                            return es1.enter_context(nc.sbuf_tensor(un(name), list(shape), dt))
                        hT = sb1("at_hT", [128, 8, 2304], BF16)
                        scal = sb1("at_scal", [128, 2, 8], F32)
                        wstg = Rot(es1, "at_wst", 2, [128, 8, 256], F32)
                        wbf = Rot(es1, "at_wbf", 2, [128, 8, 256], BF16)
                        qst = Rot(es1, "at_qst", 2, [128, 512], BF16)
                        if is_a:
                            raw = Rot(es1, "at_raw", 2, [128, 512], BF16)
                            tm1 = Rot(es1, "at_tm1", 2, [128, 512], F32)
                            tm2 = Rot(es1, "at_tm2", 2, [128, 512], F32)
                            cosT = sb1("at_cos", [128, SEQ], F32)
                            sinT = sb1("at_sin", [128, SEQ], F32)
                            pmf = sb1("at_pmf", [128, 128], F32)
                            pmb = sb1("at_pmb", [128, 128], BF16)
                            S.dma("sp", lambda e: e.dma_start(out=cosT[:], in_=k_cos), "rc", writes=["cosT"])
                            S.dma("sp", lambda e: e.dma_start(out=sinT[:], in_=k_sin), "rs", writes=["sinT"])
                            S.dma("sp", lambda e: e.dma_start(out=pmf[:], in_=k_pm), "rp", writes=["pmf"])
                            S.op("dve", lambda e: e.tensor_copy(out=pmb[:], in_=pmf[:]), reads=["pmf"], writes=["pmb"])
                        make_hT(es1, P, i, b, hT, scal)
                        wqk_v = wqk.rearrange("(c p) n -> p c n", p=128)
                        wv_v = wv.rearrange("(c p) n -> p c n", p=128)
                        pab = [(P.pA, "pA"), (P.pB, "pB")]
                        pcount = 0
                        for g in range(nqk_cols // 256):
                            wst, wst_id = wstg.next()
                            wb, wb_id = wbf.next()
                            S.dma("pool", lambda e, wb=wb, g=g: e.dma_start(out=wb[:], in_=wqk_v[:, :, g * 256:(g + 1) * 256]),
                                  "ld" + wb_id, writes=[wb_id])
                            for cc in range(2):
                                chunk = g * 2 + cc
                                isq = chunk < 8
                                dchunk = chunk if isq else chunk - 8
                                for (t0, tn) in TOKG:
                                    if isq:
                                        qs_, dst_id = qst.next()
                                        dst_ap = qs_[:, 0:tn]
                                    else:
                                        dst_ap = kT[:, dchunk, t0:t0 + tn]
                                        dst_id = "kT"
                                    pp, pp_id = pab[pcount % 2]
                                    pcount += 1
                                    for c in range(8):
                                        S.op("pe", lambda e, pp=pp, wb=wb, cc=cc, c=c, t0=t0, tn=tn: e.matmul(
                                            pp[:, 0:tn], lhsT=wb[:, c, cc * 128:(cc + 1) * 128], rhs=hT[:, c, t0:t0 + tn],
                                            start=(c == 0), stop=(c == 7)), reads=[wb_id, "hT"], writes=[pp_id])
                                    rope = is_a and t0 < SEQ
                                    if not rope:
                                        S.op("act", lambda e, pp=pp, dst_ap=dst_ap, tn=tn: e.copy(
                                            out=dst_ap, in_=pp[:, 0:tn]), reads=[pp_id], writes=[dst_id])
                                    else:
                                        rw, rw_id = raw.next()
                                        a1, a1_id = tm1.next()
                                        a2, a2_id = tm2.next()
                                        S.op("act", lambda e, pp=pp, rw=rw: e.copy(out=rw[:], in_=pp[:]), reads=[pp_id], writes=[rw_id])
                                        S.op("pe", lambda e, rw=rw: e.matmul(P.pC, lhsT=pmb[:], rhs=rw[:], start=True, stop=True),
                                             reads=[rw_id, "pmb"], writes=["pC"])
                                        S.op("dve", lambda e, a1=a1, t0=t0: e.tensor_tensor(out=a1[:], in0=P.pC, in1=sinT[:, t0:t0 + 512], op=ALU.mult),
                                             reads=["pC", "sinT"], writes=[a1_id])
                                        S.op("dve", lambda e, a2=a2, rw=rw, t0=t0: e.tensor_tensor(out=a2[:], in0=rw[:], in1=cosT[:, t0:t0 + 512], op=ALU.mult),
                                             reads=[rw_id, "cosT"], writes=[a2_id])
                                        S.op("dve", lambda e, a1=a1, a2=a2, dst_ap=dst_ap: e.tensor_tensor(
                                            out=dst_ap, in0=a1[:], in1=a2[:], op=ALU.add),
                                            reads=[a1_id, a2_id], writes=[dst_id])
                                    if isq:
                                        S.dma("sp", lambda e, dst_ap=dst_ap, dchunk=dchunk, t0=t0, tn=tn: e.dma_start(
                                            out=QT[:, dchunk, t0:t0 + tn], in_=dst_ap), "st" + dst_id, reads=[dst_id], writes=["QT"])
                        for g in range(nv_cols // 256):
                            ncol = 256
                            wst, wst_id = wstg.next()
                            wb, wb_id = wbf.next()
                            S.dma("pool", lambda e, wb=wb, g=g, ncol=ncol: e.dma_start(out=wb[:, :, 0:ncol], in_=wv_v[:, :, g * 256:g * 256 + ncol]),
                                  "ld" + wb_id, writes=[wb_id])
                            nh = ncol // 64
                            for jt in range(TPB):
                                pp, pp_id = pab[pcount % 2]
                                pcount += 1
                                for c in range(8):
                                    S.op("pe", lambda e, pp=pp, wb=wb, c=c, jt=jt, ncol=ncol: e.matmul(
                                        pp[:, 0:ncol], lhsT=hT[:, c, jt * 128:(jt + 1) * 128], rhs=wb[:, c, 0:ncol],
                                        start=(c == 0), stop=(c == 7)), reads=[wb_id, "hT"], writes=[pp_id])
                                S.op("act", lambda e, pp=pp, jt=jt, g=g, nh=nh, ncol=ncol: e.copy(
                                    out=va[:, jt, g * 4:g * 4 + nh, 0:64], in_=pp[:, 0:ncol].rearrange("p (h d) -> p h d", d=64)),
                                    reads=[pp_id], writes=["va"])
                        S.flush()
                    with ExitStack() as es2:
                        def sb2(name, shape, dt):
                            return es2.enter_context(nc.sbuf_tensor(un(name), list(shape), dt))
                        M = mixer_common_alloc(es2, i)
                        wob = sb2("at_wob", [128, 8, D], BF16)
                        wstg = Rot(es2, "at_wst2_", 2, [128, 8, 256], F32)
                        for g in range(4):
                            wst, wst_id = wstg.next()
                            S.dma("pool", lambda e, g=g: e.dma_start(out=wob[:, :, g * 256:(g + 1) * 256], in_=wo.rearrange("(c p) n -> p c n", p=128)[:, :, g * 256:(g + 1) * 256]),
                                  "ldwob", writes=["wob"])
                        sinkx = sb2("at_sink", [128, 16], F32)
                        if is_a:
                            S.dma("sp", lambda e: e.dma_start(out=sinkx[:], in_=attn_sink[j, :].partition_broadcast(128)), "snk", writes=["sinkx"])
                            S.op("act", lambda e: e.activation(out=sinkx[:], in_=sinkx[:], func=AF.Exp), reads=["sinkx"], writes=["sinkx"])
                            bandf = sb2("at_bandf", [128, 2, 128], F32)
                            bandb = sb2("at_bandb", [128, 2, 128], BF16)
                            S.dma("sp", lambda e: e.dma_start(out=bandf[:], in_=k_band.rearrange("m k q -> k m q")), "bnd", writes=["bandf"])
                            S.op("dve", lambda e: e.tensor_copy(out=bandb[:], in_=bandf[:]), reads=["bandf"], writes=["bandb"])
                        else:
                            S.op("pool", lambda e: e.memset(sinkx[:], 0.0), writes=["sinkx"])
                            etr = Rot(es2, "at_et", 2, [128, 16, 128], BF16)
                        ptr_ = Rot(es2, "at_pt", 4, [128, 512], BF16)
                        otok = Rot(es2, "at_otok", 2, [128, D], BF16)
                        oTr = Rot(es2, "at_oT", 2, [128, 8, 128], BF16)
                        den = Rot(es2, "at_den", 2, [128, 16], F32)
                        pab = [(P.pA, "pA"), (P.pB, "pB")]
                        pcount = 0
                        qtiles = list(range(16)) + ([] if last else [16, 17])
                        qtr = Rot(es2, "at_qt", 2, [128, 16, 128], BF16)
                        for (qb_, qb_id) in qtr.bufs:
                            S.op("pool", lambda e, qb_=qb_: e.memset(qb_[:], 0.0), writes=[qb_id])
                        def kcs_for(jq):
                            if jq >= 16:
                                return [(16, None), (17, None)]
                            if is_a:
                                kcs = []
                                if jq > 0:
                                    kcs.append((jq - 1, ("band", 0)))
                                kcs.append((jq, None))
                                if jq < 15:
                                    kcs.append((jq + 1, ("band", 1)))
                                return kcs + [(16, None), (17, None)]
                            return [(kc, ("nat", pats[(jq, kc)])) for kc in nat_chunks[jq]] + [(16, None), (17, None)]

                        pstate = {"n": 0}

                        def rec_core(jq):
                            qT, qT_id = qtr.next()
                            qTv = qT[:].rearrange("p (j two) q -> p j two q", two=2)
                            kcs = kcs_for(jq)

                            def pro():
                                S.dma("sp", lambda e: e.dma_start(out=qTv[0:64, :, 0, :], in_=QT[0:64, :, jq * 128:(jq + 1) * 128]),
                                      qT_id, reads=["QT"], writes=[qT_id])
                                S.dma("sp", lambda e: e.dma_start(out=qTv[64:128, :, 1, :], in_=QT[64:128, :, jq * 128:(jq + 1) * 128]),
                                      qT_id, reads=["QT"], writes=[qT_id])
                                for bank in range(3):
                                    S.op("pe", lambda e, bank=bank: e.matmul(P.oacc[:, bank, :], lhsT=zerob[:, 0:128], rhs=zerob[:], start=True, stop=True),
                                         reads=["zerob"], writes=["oacc"])
                            prol, _ = S.capture(pro)
                            steps = []
                            for ci, (kc, msk) in enumerate(kcs):
                                et = None
                                et_id = None
                                if msk is not None and msk[0] == "nat":
                                    et, et_id = etr.next()
                                for hg in range(4):
                                    pp, pp_id = pab[pstate["n"] % 2]
                                    pstate["n"] += 1
                                    pt, pt_id = ptr_.next()

                                    def s_part(ci=ci, kc=kc, msk=msk, hg=hg, pp=pp, pp_id=pp_id, et=et, et_id=et_id):
                                        if et is not None and hg == 0:
                                            S.dma("sp", lambda e: e.dma_start(out=et[:], in_=ETAB[msk[1]]), et_id, reads=["ETAB"], writes=[et_id])
                                        for hh in range(4):
                                            h = hg * 4 + hh
                                            kch = hg if is_a else h // 2
                                            S.op("pe", lambda e, hh=hh, kch=kch, h=h: e.matmul(
                                                pp[:, hh * 128:(hh + 1) * 128], lhsT=kT[:, kch, kc * 128:(kc + 1) * 128],
                                                rhs=qT[:, h, :], start=True, stop=True),
                                                reads=[qT_id, "kT"], writes=[pp_id])

                                    def r_part(ci=ci, kc=kc, msk=msk, hg=hg, pp=pp, pp_id=pp_id, pt=pt, pt_id=pt_id, et=et, et_id=et_id, n=len(kcs)):
                                        S.op("act", lambda e: e.activation(out=pt[:], in_=pp[:], func=AF.Exp, scale=scale),
                                             reads=[pp_id], writes=[pt_id])
                                        if msk is not None:
                                            ptv = pt[:].rearrange("p (h q) -> p h q", q=128)
                                            if msk[0] == "band":
                                                S.op("pool", lambda e: e.tensor_tensor(out=ptv, in0=ptv,
                                                     in1=bandb[:, msk[1], :].unsqueeze(1).to_broadcast([128, 4, 128]), op=ALU.mult),
                                                     reads=[pt_id, "bandb"], writes=[pt_id])
                                            else:
                                                S.op("pool", lambda e: e.tensor_tensor(out=ptv, in0=ptv, in1=et[:, hg * 4:(hg + 1) * 4, :], op=ALU.mult),
                                                     reads=[pt_id, et_id], writes=[pt_id])
                                        for hh in range(4):
                                            h = hg * 4 + hh
                                            kvh = hg if is_a else h
                                            bank, off = HB[h]
                                            S.op("pe", lambda e, hh=hh, kvh=kvh, bank=bank, off=off: e.matmul(
                                                P.oacc[:, bank, off * 65:(off + 1) * 65], lhsT=pt[:, hh * 128:(hh + 1) * 128],
                                                rhs=va[:, kc, kvh, :], start=False, stop=(ci == n - 1)),
                                                reads=[pt_id, "va"], writes=["oacc"])
                                    sl, _ = S.capture(s_part)
                                    rl, _ = S.capture(r_part)
                                    steps.append((sl, rl))
                            return prol, steps

                        def core_units(steps):
                            units = []
                            n = len(steps)
                            for k in range(min(2, n)):
                                units.append(steps[k][0])
                            for k in range(n):
                                units.append(steps[k][1])
                                if k + 2 < n:
                                    units.append(steps[k + 2][0])
                            return units

                        def norm(jq):
                            tt = b * TPB + jq
                            dn, dn_id = den.next()
                            ot, ot_id = otok.next()
                            oT, oT_id = oTr.next()
                            for bank in range(3):
                                nh = (6, 5, 5)[bank]
                                h0 = (0, 6, 11)[bank]
                                S.op("dve", lambda e, bank=bank, nh=nh, h0=h0: e.tensor_tensor(
                                    out=dn[:, h0:h0 + nh], in0=P.oacc[:, bank, 0:nh * 65].rearrange("p (h d) -> p h d", d=65)[:, :, 64],
                                    in1=sinkx[:, h0:h0 + nh], op=ALU.add), reads=["oacc", "sinkx"], writes=[dn_id])
                            S.op("dve", lambda e: e.reciprocal(out=dn[:], in_=dn[:]), reads=[dn_id], writes=[dn_id])
                            for bank in range(3):
                                nh = (6, 5, 5)[bank]
                                h0 = (0, 6, 11)[bank]
                                S.op("dve", lambda e, bank=bank, nh=nh, h0=h0: e.tensor_tensor(
                                    out=ot[:, h0 * 64:(h0 + nh) * 64].rearrange("p (h d) -> p h d", d=64),
                                    in0=P.oacc[:, bank, 0:nh * 65].rearrange("p (h d) -> p h d", d=65)[:, :, 0:64],
                                    in1=dn[:, h0:h0 + nh].unsqueeze(2).to_broadcast([128, nh, 64]), op=ALU.mult),
                                    reads=["oacc", dn_id], writes=[ot_id])
                            return ot, ot_id, oT, oT_id

                        def tail(jq, ot, ot_id, oT, oT_id):
                            tt = b * TPB + jq
                            load_variant(M, i, b if jq < 16 else 2)
                            for hf4 in range(2):
                                for c in range(4):
                                    S.op("pe", lambda e, c=c, hf4=hf4: e.transpose(out=P.ptb[:, c, :], in_=ot[:, (hf4 * 4 + c) * 128:(hf4 * 4 + c + 1) * 128], identity=identb[:]),
                                         reads=[ot_id, "identb"], writes=["ptb"])
                                S.op("act", lambda e, hf4=hf4: e.copy(out=oT[:, hf4 * 4:(hf4 + 1) * 4, :], in_=P.ptb), reads=["ptb"], writes=[oT_id])
                            po = P.ptf[:].rearrange("p c t -> p (c t)")
                            for hf in range(2):
                                for c in range(8):
                                    S.op("pe", lambda e, c=c, hf=hf: e.matmul(po[:, hf * 512:(hf + 1) * 512], lhsT=oT[:, c, :],
                                                                              rhs=wob[:, c, hf * 512:(hf + 1) * 512],
                                                                              start=(c == 0), stop=(c == 7)),
                                         reads=[oT_id, "wob"], writes=["ptf"])
                            out_stage(M, P, i, tt, po, ["ptf"])

                        def interleave(units, tl):
                            out_ = []
                            nu = max(1, len(units))
                            per = -(-len(tl) // nu)
                            ti = 0
                            for u in units:
                                out_.extend(u)
                                out_.extend(tl[ti:ti + per])
                                ti += per
                            out_.extend(tl[ti:])
                            return out_

                        prol, steps = rec_core(qtiles[0])
                        S.replay(prol)
                        for u in core_units(steps):
                            S.replay(u)
                        for qi, jq in enumerate(qtiles):
                            nl, nr = S.capture(norm, jq)
                            S.replay(nl)
                            tl, _ = S.capture(tail, jq, *nr)
                            if qi + 1 < len(qtiles):
                                prol, steps = rec_core(qtiles[qi + 1])
                                S.replay(prol)
                                S.replay(interleave(core_units(steps), tl))
                            else:
                                S.replay(tl)
                        S.flush()

        def phase_nat_table():
            with ExitStack() as es:
                bt = Rot(es, "nt_b", 2, [128, 16, 128], F32)
                mt = Rot(es, "nt_m", 2, [128, 128], F32)
                eo = Rot(es, "nt_e", 2, [128, 16, 128], BF16)
                for pid in range(NPAT):
                    b_, b_id = bt.next()
                    m_, m_id = mt.next()
                    e_, e_id = eo.next()
                    S.dma("sp", lambda e, b_=b_, pid=pid: e.dma_start(out=b_[:], in_=nat_bias[pid]), b_id, writes=[b_id])
                    S.dma("sp", lambda e, m_=m_, pid=pid: e.dma_start(out=m_[:], in_=k_natmask[pid]), m_id, writes=[m_id])
                    S.op("act", lambda e, b_=b_: e.activation(out=b_[:], in_=b_[:], func=AF.Exp), reads=[b_id], writes=[b_id])
                    S.op("dve", lambda e, b_=b_, m_=m_, e_=e_: e.tensor_tensor(out=e_[:], in0=b_[:], in1=m_[:].unsqueeze(1).to_broadcast([128, 16, 128]),
                                                                              op=ALU.mult), reads=[b_id, m_id], writes=[e_id])
                    S.dma("sp", lambda e, e_=e_, pid=pid: e.dma_start(out=ETAB[pid], in_=e_[:]), "st" + e_id, reads=[e_id], writes=["ETAB"])
                S.flush()

        def phase_conv(i, j, last):
            win = conv_w_in[j].rearrange("(c p) n -> p c n", p=128)
            for b in range(NB):
                with ExitStack() as es:
                    def sb(name, shape, dt):
                        return es.enter_context(nc.sbuf_tensor(un(name), list(shape), dt))
                    P = alloc_psum(es)
                    zT = sb("cv_zT", [128, 8, 2304], BF16)
                    with ExitStack() as es1:
                        def sb1(name, shape, dt):
                            return es1.enter_context(nc.sbuf_tensor(un(name), list(shape), dt))
                        hT = sb1("cv_hT", [128, 8, 2304], BF16)
                        scal = sb1("cv_scal", [128, 2, 8], F32)
                        cw = sb1("cv_cw", [128, 3, 8], F32)
                        S.dma("sp", lambda e: e.dma_start(out=cw[:], in_=conv_w[j].rearrange("k (c p) -> p k c", p=128), allow_slow_non_contiguous=True),
                              "cw", writes=["cw"])
                        wstg = Rot(es1, "cv_wst", 2, [128, 8, 3, 128], F32)
                        wbf = Rot(es1, "cv_wbf", 2, [128, 8, 3, 128], BF16)
                        pbuf = sb1("cv_p", [128, 2308], F32)
                        gbuf = sb1("cv_g", [128, 2304], F32)
                        ubuf = Rot(es1, "cv_u", 2, [128, 512], F32)
                        acc = sb1("cv_acc", [128, 2304], F32)
                        S.op("pool", lambda e: e.memset(pbuf[:], 0.0), writes=["pbuf"])
                        make_hT(es1, P, i, b, hT, scal)
                        pbanks = [(P.pA, "pA"), (P.pB, "pB"), (P.pC, "pC")]
                        for c in range(8):
                            wst, wst_id = wstg.next()
                            wb, wb_id = wbf.next()
                            for k3 in range(3):
                                S.dma("pool", lambda e, wb=wb, k3=k3, c=c: e.dma_start(out=wb[:, :, k3, :], in_=win[:, :, k3 * D + c * 128:k3 * D + (c + 1) * 128]),
                                      "ld" + wb_id, writes=[wb_id])
                            for (t0, tn) in TOKG:
                                poff = 1 + t0 if t0 < SEQ else 2051
                                for k3 in range(3):
                                    pp, pp_id = pbanks[k3]
                                    for kk in range(8):
                                        S.op("pe", lambda e, pp=pp, wb=wb, k3=k3, kk=kk, t0=t0, tn=tn: e.matmul(
                                            pp[:, 0:tn], lhsT=wb[:, kk, k3, :], rhs=hT[:, kk, t0:t0 + tn], start=(kk == 0), stop=(kk == 7)),
                                            reads=[wb_id, "hT"], writes=[pp_id])
                                ub, ub_id = ubuf.next()
                                S.op("act", lambda e, ub=ub, tn=tn: e.copy(out=ub[:, 0:tn], in_=P.pC[:, 0:tn]), reads=["pC"], writes=[ub_id])
                                S.op("act", lambda e, t0=t0, tn=tn: e.copy(out=gbuf[:, t0:t0 + tn], in_=P.pA[:, 0:tn]), reads=["pA"], writes=["gbuf"])
                                S.op("dve", lambda e, ub=ub, tn=tn, poff=poff: e.tensor_tensor(out=pbuf[:, poff:poff + tn], in0=P.pB[:, 0:tn], in1=ub[:, 0:tn], op=ALU.mult),
                                     reads=["pB", ub_id], writes=["pbuf"])
                            for (o0, p0, n) in ((0, 1, SEQ), (SEQ, 2051, LCTX)):
                                S.op("dve", lambda e, c=c, o0=o0, p0=p0, n=n: e.tensor_scalar(out=acc[:, o0:o0 + n], in0=pbuf[:, p0:p0 + n], scalar1=cw[:, 1, c:c + 1],
                                                                                             scalar2=None, op0=ALU.mult), reads=["pbuf", "cw"], writes=["acc"])
                                S.op("dve", lambda e, c=c, o0=o0, p0=p0, n=n: e.scalar_tensor_tensor(out=acc[:, o0:o0 + n], in0=pbuf[:, p0 - 1:p0 - 1 + n], scalar=cw[:, 0, c:c + 1],
                                                                                                      in1=acc[:, o0:o0 + n], op0=ALU.mult, op1=ALU.add),
                                     reads=["pbuf", "cw", "acc"], writes=["acc"])
                                S.op("dve", lambda e, c=c, o0=o0, p0=p0, n=n: e.scalar_tensor_tensor(out=acc[:, o0:o0 + n], in0=pbuf[:, p0 + 1:p0 + 1 + n], scalar=cw[:, 2, c:c + 1],
                                                                                                     in1=acc[:, o0:o0 + n], op0=ALU.mult, op1=ALU.add),
                                     reads=["pbuf", "cw", "acc"], writes=["acc"])
                            S.op("pool", lambda e, c=c: e.tensor_tensor(out=zT[:, c, :], in0=acc[:], in1=gbuf[:], op=ALU.mult),
                                 reads=["acc", "gbuf"], writes=["zT"])
                        S.flush()
                    with ExitStack() as es2:
                        def sb2(name, shape, dt):
                            return es2.enter_context(nc.sbuf_tensor(un(name), list(shape), dt))
                        M = mixer_common_alloc(es2, i)
                        wob = sb2("cv_wob", [128, 8, D], BF16)
                        wstg = Rot(es2, "cv_wst2_", 2, [128, 8, 512], F32)
                        wo = conv_w_out[j].rearrange("(c p) n -> p c n", p=128)
                        for g in range(2):
                            wst, wst_id = wstg.next()
                            S.dma("pool", lambda e, g=g: e.dma_start(out=wob[:, :, g * 512:(g + 1) * 512], in_=wo[:, :, g * 512:(g + 1) * 512]), "ldwob", writes=["wob"])
                        qtiles = list(range(16)) + ([] if last else [16, 17])
                        po = P.ptf[:].rearrange("p c t -> p (c t)")
                        for jq in qtiles:
                            tt = b * TPB + jq
                            load_variant(M, i, b if jq < 16 else 2)
                            for hf in range(2):
                                for c in range(8):
                                    S.op("pe", lambda e, c=c, hf=hf, jq=jq: e.matmul(po[:, hf * 512:(hf + 1) * 512], lhsT=zT[:, c, jq * 128:(jq + 1) * 128],
                                                                                     rhs=wob[:, c, hf * 512:(hf + 1) * 512], start=(c == 0), stop=(c == 7)),
                                         reads=["zT", "wob"], writes=["ptf"])
                            out_stage(M, P, i, tt, po, ["ptf"])
                        S.flush()

        def phase_experts(i):
            NTB = CAP // 128
            NNT = CAP // 512
            with ExitStack() as es:
                def ps(name, shape, dt):
                    return es.enter_context(nc.psum_tensor(un(name), list(shape), dt))
                wg = Rot(es, "ex_wg", 3, [128, 8, 512], BF16)
                wu = Rot(es, "ex_wu", 3, [128, 8, 512], BF16)
                wd = Rot(es, "ex_wd", 3, [128, 4, D], BF16)
                xs = Rot(es, "ex_xs", 3, [128, NTB, D], BF16)
                xsT = Rot(es, "ex_xsT", 2, [128, 8, CAP], BF16)
                sg = Rot(es, "ex_sg", 2, [128, 512], F32)
                aT = Rot(es, "ex_aT", 2, [128, 4, CAP], BF16)
                ys = Rot(es, "ex_ys", 2, [128, D], F32)
                ptb = [(ps("ex_ptb%d" % k, [128, 8, 128], BF16), "ptb%d" % k) for k in range(2)]
                pg = [(ps("ex_pg%d" % k, [128, 512], F32), "pg%d" % k) for k in range(2)]
                pu = [(ps("ex_pu%d" % k, [128, 512], F32), "pu%d" % k) for k in range(2)]
                py = ps("ex_py", [128, D], F32)
                st8 = {"nptb": 0, "npg": 0}

                def prep(ex):
                    wgb, wg_id = wg.next()
                    wub, wu_id = wu.next()
                    wdb, wd_id = wd.next()
                    xsb, xs_id = xs.next()
                    for (src, dstb, dst_id) in ((w_gate[i, ex].rearrange("(c p) n -> p c n", p=128), wgb, wg_id),
                                                (w_up[i, ex].rearrange("(c p) n -> p c n", p=128), wub, wu_id),
                                                (w_down[i, ex].rearrange("(c p) n -> p c n", p=128), wdb, wd_id)):
                        S.dma("pool", lambda e, dstb=dstb, src=src: e.dma_start(out=dstb[:], in_=src), "ld" + dst_id, writes=[dst_id])
                    for q in range(CAP // 256):
                        S.cur_group = (ex, q * 256 + 1) if q > 0 else None
                        S.dma("sp", lambda e, xsb=xsb, ex=ex, q=q: e.dma_start(
                            out=xsb[:, 2 * q:2 * q + 2, :], in_=XS[ex * CAP + q * 256:ex * CAP + (q + 1) * 256, :].rearrange("(j p) d -> p j d", p=128)),
                            xs_id + "q%d" % q, reads=["XS"], writes=[xs_id + "q%d" % q])
                    S.cur_group = None
                    return dict(wgb=wgb, wg_id=wg_id, wub=wub, wu_id=wu_id, wdb=wdb, wd_id=wd_id, xsb=xsb, xs_id=xs_id)

                def late_cast(pr):
                    return

                def compute(ex, pr):
                    wgb, wg_id, wub, wu_id, wdb, wd_id, xsb, xs_id = (pr[k] for k in ("wgb", "wg_id", "wub", "wu_id", "wdb", "wd_id", "xsb", "xs_id"))
                    xTb, xT_id = xsT.next()
                    aTb, aT_id = aT.next()
                    QR = 256
                    for q in range(CAP // QR):
                        S.cur_group = (ex, q * QR + 1) if q > 0 else None
                        c0 = q * QR
                        for jt in range(2 * q, 2 * q + 2):
                            pt, pt_id = ptb[st8["nptb"] % 2]
                            st8["nptb"] += 1
                            for c in range(8):
                                S.op("pe", lambda e, pt=pt, jt=jt, c=c: e.transpose(out=pt[:, c, :], in_=xsb[:, jt, c * 128:(c + 1) * 128], identity=identb[:]),
                                     reads=[xs_id + "q%d" % q, "identb"], writes=[pt_id])
                            S.op("act", lambda e, pt=pt, jt=jt: e.copy(out=xTb[:, :, jt * 128:(jt + 1) * 128], in_=pt[:]), reads=[pt_id], writes=[xT_id])
                        for m_ in range(4):
                            pgb, pg_id = pg[st8["npg"] % 2]
                            pub, pu_id = pu[st8["npg"] % 2]
                            st8["npg"] += 1
                            for c in range(8):
                                S.op("pe", lambda e, pgb=pgb, c=c, m_=m_, c0=c0: e.matmul(pgb[:, 0:QR], lhsT=wgb[:, c, m_ * 128:(m_ + 1) * 128], rhs=xTb[:, c, c0:c0 + QR],
                                                                                          start=(c == 0), stop=(c == 7)), reads=[wg_id, xT_id], writes=[pg_id])
                            for c in range(8):
                                S.op("pe", lambda e, pub=pub, c=c, m_=m_, c0=c0: e.matmul(pub[:, 0:QR], lhsT=wub[:, c, m_ * 128:(m_ + 1) * 128], rhs=xTb[:, c, c0:c0 + QR],
                                                                                          start=(c == 0), stop=(c == 7)), reads=[wu_id, xT_id], writes=[pu_id])
                            sgb, sg_id = sg.next()
                            S.op("act", lambda e, sgb=sgb, pgb=pgb: e.activation(out=sgb[:, 0:QR], in_=pgb[:, 0:QR], func=AF.Silu), reads=[pg_id], writes=[sg_id])
                            S.op("dve", lambda e, sgb=sgb, pub=pub, m_=m_, c0=c0: e.tensor_tensor(out=aTb[:, m_, c0:c0 + QR], in0=pub[:, 0:QR], in1=sgb[:, 0:QR], op=ALU.mult),
                                 reads=[pu_id, sg_id], writes=[aT_id])
                        for jt in range(2 * q, 2 * q + 2):
                            for hf in range(2):
                                for m_ in range(4):
                                    S.op("pe", lambda e, jt=jt, hf=hf, m_=m_: e.matmul(py[:, hf * 512:(hf + 1) * 512], lhsT=aTb[:, m_, jt * 128:(jt + 1) * 128],
                                                                                       rhs=wdb[:, m_, hf * 512:(hf + 1) * 512], start=(m_ == 0), stop=(m_ == 3)),
                                         reads=[aT_id, wd_id], writes=["py"])
                            ysb, ys_id = ys.next()
                            if jt % 2 == 0:
                                S.op("act", lambda e, ysb=ysb: e.copy(out=ysb[:], in_=py[:]), reads=["py"], writes=[ys_id])
                            else:
                                S.op("dve", lambda e, ysb=ysb: e.tensor_copy(out=ysb[:], in_=py[:]), reads=["py"], writes=[ys_id])
                            r0 = ex * CAP + jt * 128
                            S.dma("sp", lambda e, ysb=ysb, r0=r0: e.dma_start(out=YS[r0:r0 + 128, :], in_=ysb[:]), "st" + ys_id, reads=[ys_id], writes=["YS"])
                    S.cur_group = None

                preps = {0: prep(0), 1: prep(1)}
                for ex in range(NEXP):
                    if ex + 2 < NEXP:
                        preps[ex + 2] = prep(ex + 2)
                    compute(ex, preps.pop(ex))
                S.flush()

        def phase_combine(i, last):
            with ExitStack() as es:
                def sb(name, shape, dt):
                    return es.enter_context(nc.sbuf_tensor(un(name), list(shape), dt))
                G2 = sb("cb_G2", [128, D], F32)
                lg = sb("cb_lg", [128, D], F32)
                lb = sb("cb_lb", [128, D], F32)
                y0 = Rot(es, "cb_y0", 2, [128, D], F32)
                y1 = Rot(es, "cb_y1", 2, [128, D], F32)
                xm = Rot(es, "cb_xm", 2, [128, D], F32)
                tb = Rot(es, "cb_t", 2, [128, D], F32)
                xn = Rot(es, "cb_xn", 2, [128, D], F32)
                xo = Rot(es, "cb_xo", 2, [128, D], F32)
                st = Rot(es, "cb_st", 2, [128, 2, 6], F32)
                mv = Rot(es, "cb_mv", 2, [128, 2], F32)
                rs = Rot(es, "cb_rs", 2, [128, 2], F32)
                S.dma("sp", lambda e: e.dma_start(out=lg[:], in_=ln2_g[i, :].partition_broadcast(128)), "clg", writes=["lg"])
                S.dma("sp", lambda e: e.dma_start(out=lb[:], in_=ln2_b[i, :].partition_broadcast(128)), "clb", writes=["lb"])
                cvar = {"v": None}

                def one_tile(tt):
                    b, j = divmod(tt, TPB)
                    v = b if j < 16 else 2
                    if v != cvar["v"]:
                        cvar["v"] = v
                        S.dma("sp", lambda e, v=v: e.dma_start(out=G2[:], in_=MODR[i, v, 5, :].partition_broadcast(128)), "cG2", writes=["G2"])
                    y0b, y0_id = y0.next()
                    y1b, y1_id = y1.next()
                    xmb, xm_id = xm.next()
                    t, t_id = tb.next()
                    xnb, xn_id = xn.next()
                    xob, xo_id = xo.next()
                    for k, (yb, y_id) in enumerate(((y0b, y0_id), (y1b, y1_id))):
                        S.dma("pool", lambda e, yb=yb, k=k, tt=tt: e.indirect_dma_start(
                            out=yb[:], out_offset=None, in_=YS, in_offset=bass.IndirectOffsetOnAxis(ap=destI[:, tt, k:k + 1], axis=0),
                            bounds_check=breg(e), oob_is_err=False), y_id, reads=["YS"], writes=[y_id])
                    S.dma("sp", lambda e, xmb=xmb, tt=tt: e.dma_start(out=xmb[:], in_=X[tt * 128:(tt + 1) * 128, :]), xm_id, reads=["X%d" % tt], writes=[xm_id])
                    S.op("act", lambda e, t=t, y0b=y0b, tt=tt: e.activation(out=t[:], in_=y0b[:], func=AF.Copy, scale=gates[:, tt, 0:1]),
                         reads=[y0_id], writes=[t_id])
                    S.op("dve", lambda e, t=t, y1b=y1b, tt=tt: e.scalar_tensor_tensor(out=t[:], in0=y1b[:], scalar=gates[:, tt, 1:2], in1=t[:], op0=ALU.mult, op1=ALU.add),
                         reads=[y1_id, t_id], writes=[t_id])
                    S.op("dve", lambda e, t=t: e.tensor_tensor(out=t[:], in0=t[:], in1=G2[:], op=ALU.mult), reads=[t_id, "G2"], writes=[t_id])
                    S.op("dve", lambda e, t=t, xmb=xmb: e.tensor_tensor(out=t[:], in0=t[:], in1=xmb[:], op=ALU.add), reads=[t_id, xm_id], writes=[t_id])
                    layer_norm_core((st, mv, rs), t, t_id, xnb, xn_id)
                    S.op("dve", lambda e, xob=xob, xnb=xnb: e.tensor_tensor(out=xob[:], in0=xnb[:], in1=lg[:], op=ALU.mult), reads=[xn_id, "lg"], writes=[xo_id])
                    S.op("dve", lambda e, xob=xob: e.tensor_tensor(out=xob[:], in0=xob[:], in1=lb[:], op=ALU.add), reads=[xo_id, "lb"], writes=[xo_id])
                    if last:
                        dst = out[b, j * 128:(j + 1) * 128, :]
                    else:
                        dst = X[tt * 128:(tt + 1) * 128, :]
                    S.dma("sp", lambda e, xob=xob, dst=dst: e.dma_start(out=dst, in_=xob[:]), "st" + xo_id, reads=[xo_id], writes=["X%d" % tt])

                tiles = [tt for tt in range(NT) if not (last and tt % TPB >= 16)]
                k = 0
                while k < len(tiles):
                    ta = tiles[k]
                    va_ = (ta // TPB) if ta % TPB < 16 else 2
                    tile_b = tiles[k + 1] if k + 1 < len(tiles) else None
                    vb_ = None if tile_b is None else ((tile_b // TPB) if tile_b % TPB < 16 else 2)
                    la, _ = S.capture(one_tile, ta)
                    if tile_b is not None and vb_ == va_:
                        lb_, _ = S.capture(one_tile, tile_b)
                        merged = []
                        for x in range(max(len(la), len(lb_))):
                            if x < len(la):
                                merged.append(la[x])
                            if x < len(lb_):
                                merged.append(lb_[x])
                        S.replay(merged)
                        k += 2
                    else:
                        S.replay(la)
                        k += 1
                S.flush()

        phase_init()
        phase_nat_table()
        for i in range(DEPTH):
            last = i == DEPTH - 1
            kind = i % 3
            j = i // 3
            phase_ada(i)
            S.op("pool", lambda e: e.memset(cnt[:], 0.0), writes=["cnt"])
            if kind == 0:
                phase_attn(i, j, 0, last)
            elif kind == 1:
                phase_conv(i, j, last)
            else:
                phase_attn(i, j, 2, last)
            S.op("dve", lambda e: e.tensor_copy(out=cnti[:], in_=cnt[:]), reads=["cnt"], writes=["cnti"])
            S.flush()
            phase_experts(i)
            phase_combine(i, last)
        if debug:
            S.limit = None
            dbgX = nc.dram_tensor("dbgX", [NT * 128, D], F32, kind="ExternalOutput").ap()
            dbgM = nc.dram_tensor("dbgM", [DEPTH, 3, 6, D], F32, kind="ExternalOutput").ap()
            dbgXS = nc.dram_tensor("dbgXS", [NSLOT, D], BF16, kind="ExternalOutput").ap()
            dbgYS = nc.dram_tensor("dbgYS", [NSLOT, D], F32, kind="ExternalOutput").ap()
            dbgQ = nc.dram_tensor("dbgQ", [128, 8, 2304], BF16, kind="ExternalOutput").ap()
            dbgD = nc.dram_tensor("dbgD", [128, NT, 2], I32, kind="ExternalOutput").ap()
            dbgG = nc.dram_tensor("dbgG", [128, NT, 2], F32, kind="ExternalOutput").ap()
            for r0 in range(0, NT * 128, 512):
                S.dma("sp", lambda e, r0=r0: e.dma_start(out=dbgX[r0:r0 + 512, :], in_=X[r0:r0 + 512, :]), "d0")
            S.dma("sp", lambda e: e.dma_start(out=dbgM, in_=MODR), "d1")
            for r0 in range(0, NSLOT, 512):
                S.dma("sp", lambda e, r0=r0: e.dma_start(out=dbgXS[r0:r0 + 512, :], in_=XS[r0:r0 + 512, :]), "d2")
                S.dma("sp", lambda e, r0=r0: e.dma_start(out=dbgYS[r0:r0 + 512, :], in_=YS[r0:r0 + 512, :]), "d3")
            S.dma("sp", lambda e: e.dma_start(out=dbgQ, in_=QT), "d4")
            S.dma("sp", lambda e: e.dma_start(out=dbgD, in_=destI[:]), "d5")
            S.dma("sp", lambda e: e.dma_start(out=dbgG, in_=gates[:]), "d6")
            S.flush()
    return nc


_NC_CACHE = {}


def _host_constants(inputs):
    cosT, sinT, Pm = _rope_tables()
    pats, chunks, dr, dc, mk, npat = _NATP
    rpb = np.asarray(inputs["nat_rpb"], np.float32)[0]
    nat_bias = np.ascontiguousarray(np.transpose(rpb[:, dr, dc], (1, 2, 0, 3))).astype(np.float32)
    aw = np.asarray(inputs["attn_w_qkv"], np.float32)
    kcols = []
    for g in range(4):
        kcols += list(range(1024 + g * 64, 1024 + (g + 1) * 64)) * 2
    attn_wqk = np.ascontiguousarray(np.concatenate([aw[:, :, :1024], aw[:, :, kcols]], axis=2))
    attn_wv = np.ascontiguousarray(aw[:, :, 1280:1536])
    nw = np.asarray(inputs["nat_w_qkv"], np.float32)
    router_w = np.ascontiguousarray(np.concatenate([inputs["router_w_group"], inputs["router_w_expert"]], axis=2)).astype(np.float32)
    router_b = np.ascontiguousarray(np.concatenate([inputs["router_b_group"], inputs["router_b_expert"]], axis=1)).astype(np.float32)
    kk = np.arange(128)
    consts = {
        "attn_wqk": attn_wqk, "attn_wv": attn_wv,
        "nat_wqk": np.ascontiguousarray(nw[:, :, :2048]), "nat_wv": np.ascontiguousarray(nw[:, :, 2048:]),
        "nat_bias": nat_bias, "router_w": router_w, "router_b": router_b,
        "k_ident": np.eye(128, dtype=np.float32), "k_cos": cosT, "k_sin": sinT, "k_pm": Pm,
        "k_band": _band_masks(), "k_natmask": mk,
        "k_ustrict": (kk[:, None] < kk[None, :]).astype(np.float32),
        "k_eoff": (np.arange(32, dtype=np.float32) * CAP)[None, :],
    }
    return consts


def kernel(**inputs):
    if "nc" not in _NC_CACHE:
        _NC_CACHE["nc"] = build_program()
    nc = _NC_CACHE["nc"]
    consts = _host_constants(inputs)
    shared = {}
    for k in ("w_ada", "b_ada", "ln1_g", "ln1_b", "ln2_g", "ln2_b", "attn_w_o", "attn_sink", "conv_w_in", "conv_w",
              "conv_w_out", "nat_w_o", "expert_w_gate", "expert_w_up", "expert_w_down"):
        shared[k] = np.ascontiguousarray(np.asarray(inputs[k], np.float32))
    shared.update(consts)
    shared["c_ctx"] = np.ascontiguousarray(np.asarray(inputs["c_ctx"], np.float32).reshape(1, D))
    x = np.asarray(inputs["x"], np.float32)
    c = np.asarray(inputs["c"], np.float32)
    ctx = np.asarray(inputs["ctx"], np.float32)
    in_maps = []
    for core in range(NCORES):
        m = dict(shared)
        m["x"] = np.ascontiguousarray(x[core * NB:(core + 1) * NB])
        m["c"] = np.ascontiguousarray(c[core * NB:(core + 1) * NB])
        m["ctx"] = np.ascontiguousarray(ctx[core * NB:(core + 1) * NB])
        in_maps.append(m)
    res = run_bass_kernel_spmd(nc, in_maps, core_ids=list(range(NCORES)))
    return np.concatenate([r["out"] for r in res.results], axis=0).astype(np.float32)
```

```python
import numpy as np
from contextlib import ExitStack
import ml_dtypes
import concourse.bass as bass
import concourse.mybir as mybir
from concourse.bass_utils import run_bass_kernel_spmd

F32 = mybir.dt.float32
BF16 = mybir.dt.bfloat16
I32 = mybir.dt.int32
AF = mybir.ActivationFunctionType
ALU = mybir.AluOpType
AX = mybir.AxisListType.X

NCORES = 8
D = 1024
SEQ = 2048
LCTX = 256
DEPTH = 4
NB = 2
TPB = 18
NT = NB * TPB
NEXP = 32
FF = 512
CAP = 1024
NSLOT = NEXP * CAP
ALPHA = (2 * DEPTH) ** 0.25
INV_ALPHA = 1.0 / ALPHA
EPS2 = 1e-5 / (ALPHA * ALPHA)
BIG = 1.0e30
ENGS = ("pe", "act", "dve", "pool", "sp")
import os as _os
KDBG = int(_os.environ.get("KDBG", "0"))


class _Op:
    __slots__ = ("idx", "eng", "fn", "is_dma", "key", "dma_val", "signal", "milestone", "waits", "group")

    def __init__(self, idx, eng, fn, is_dma, key):
        self.group = None
        self.idx = idx
        self.eng = eng
        self.fn = fn
        self.is_dma = is_dma
        self.key = key
        self.dma_val = 0
        self.signal = False
        self.milestone = 0
        self.waits = []


class Sched:
    def __init__(self, nc, es):
        self.nc = nc
        self.es = es
        self.esem = {e: es.enter_context(nc.semaphore("s_" + e)) for e in ENGS}
        self.ksem = {}
        self.key_count = {}
        self.eng_base = {e: 0 for e in ENGS}
        self._reset()

    def _reset(self):
        self.ops = []
        self.per_eng = {e: [] for e in ENGS}
        self.last_w = {}
        self.readers = {}
        self.seen = {e: {f: -1 for f in ENGS} for e in ENGS}
        self.seen_dma = {e: {} for e in ENGS}
        self.eng_group = {e: None for e in ENGS}
        self.seen_saved = {e: None for e in ENGS}

    cur_group = None
    cnt_ap = None

    def _add(self, eng, fn, reads, writes, is_dma, key, group=None):
        o = _Op(len(self.ops), eng, fn, is_dma, key)
        o.group = group
        if group != self.eng_group[eng]:
            if self.eng_group[eng] is not None:
                self.seen[eng], self.seen_dma[eng] = self.seen_saved[eng]
            if group is not None:
                self.seen_saved[eng] = (dict(self.seen[eng]), dict(self.seen_dma[eng]))
            self.eng_group[eng] = group
        deps = set()
        for b in reads:
            w = self.last_w.get(b)
            if w is not None:
                deps.add(w)
        for b in writes:
            w = self.last_w.get(b)
            if w is not None:
                deps.add(w)
            for r in self.readers.get(b, ()):
                deps.add(r)
        for b in reads:
            self.readers.setdefault(b, []).append(o.idx)
        for b in writes:
            self.last_w[b] = o.idx
            self.readers[b] = []
        for di in sorted(deps):
            d = self.ops[di]
            if d.is_dma:
                if self.seen_dma[eng].get(d.key, 0) >= d.dma_val:
                    continue
                self.seen_dma[eng][d.key] = d.dma_val
                o.waits.append(("dma", d.key, d.dma_val))
            else:
                if d.eng == eng and not is_dma and eng == "pe":
                    continue
                if self.seen[eng][d.eng] >= d.idx:
                    continue
                self.seen[eng][d.eng] = d.idx
                d.signal = True
                o.waits.append(("eng", d.eng, d.idx))
        if is_dma:
            if key not in self.ksem:
                self.ksem[key] = self.es.enter_context(self.nc.semaphore("k%d" % len(self.ksem)))
                self.key_count[key] = 0
            self.key_count[key] += 16
            o.dma_val = self.key_count[key]
        self.ops.append(o)
        self.per_eng[eng].append(o)
        return o

    _cap = None

    def op(self, eng, fn, reads=(), writes=()):
        if self._cap is not None:
            self._cap.append((eng, fn, tuple(reads), tuple(writes), False, None, self.cur_group))
            return None
        return self._add(eng, fn, tuple(reads), tuple(writes), False, None, self.cur_group)

    def dma(self, eng, fn, key, reads=(), writes=()):
        if self._cap is not None:
            self._cap.append((eng, fn, tuple(reads), tuple(writes), True, key, self.cur_group))
            return None
        return self._add(eng, fn, tuple(reads), tuple(writes), True, key, self.cur_group)

    def capture(self, f, *a, **kw):
        assert self._cap is None
        self._cap = []
        try:
            r = f(*a, **kw)
        finally:
            c = self._cap
            self._cap = None
        return c, r

    def replay(self, items):
        for it in items:
            self._add(*it)

    limit = None
    nflush = 0

    def flush(self):
        self.nflush += 1
        if self.limit is not None and self.nflush > self.limit:
            for o in self.ops:
                if o.is_dma:
                    self.key_count[o.key] -= 16
            self._reset()
            return
        nc = self.nc
        ops = self.ops
        final = {}
        self.eng_base_prev = dict(self.eng_base)
        for e in ENGS:
            lst = [o for o in self.per_eng[e] if not o.is_dma]
            if e != "sp" and lst:
                lst[-1].signal = True
            n = self.eng_base[e]
            for o in self.per_eng[e]:
                if o.signal and not o.is_dma:
                    n += 1
                    o.milestone = n
            self.eng_base[e] = n
            final[e] = n
        esem, ksem = self.esem, self.ksem
        keyvals = dict(self.key_count)
        per_eng = self.per_eng

        regs = self.__dict__.setdefault("_regs", {})

        def emit_one(ename, eng, o):
            for w in o.waits:
                if w[0] == "dma":
                    eng.wait_ge(ksem[w[1]], w[2])
                else:
                    eng.wait_ge(esem[w[1]], ops[w[2]].milestone)
            inst = o.fn(eng)
            if o.is_dma:
                inst.then_inc(ksem[o.key], 16)
            elif o.signal:
                inst.then_inc(esem[ename], 1)

        def emit(ename, eng):
            lst = per_eng[ename]
            ms = self.eng_base_prev[ename]
            k = 0
            while k < len(lst):
                o = lst[k]
                if o.group is None:
                    emit_one(ename, eng, o)
                    if o.signal and not o.is_dma:
                        ms = o.milestone
                    k += 1
                    continue
                k2 = k
                while k2 < len(lst) and lst[k2].group == o.group:
                    k2 += 1
                run = lst[k:k2]
                if ename not in regs:
                    regs[ename] = eng.alloc_register("cnt_" + ename)
                r = regs[ename]
                eng.reg_load(r, self.cnt_ap(o.group[0]))
                nsig = sum(1 for x in run if x.signal and not x.is_dma)
                with eng.If_lt(r, o.group[1]):
                    if nsig:
                        if ms > 0:
                            eng.wait_ge(esem[ename], ms)
                        eng.sem_inc(esem[ename], nsig)
                    for x in run:
                        if x.is_dma:
                            if x.dma_val > 16:
                                eng.wait_ge(ksem[x.key], x.dma_val - 16)
                            eng.sem_inc(ksem[x.key], 16)
                with eng.Else():
                    for x in run:
                        emit_one(ename, eng, x)
                ms += nsig
                k = k2
            for kk, v in keyvals.items():
                if v > 0:
                    eng.wait_ge(ksem[kk], v)
            for f in ENGS:
                if f != ename and f != "sp" and final[f] > 0:
                    eng.wait_ge(esem[f], final[f])

        with nc.Block() as block:
            @block.tensor
            def _(eng):
                emit("pe", eng)

            @block.scalar
            def _(eng):
                emit("act", eng)

            @block.vector
            def _(eng):
                emit("dve", eng)

            @block.gpsimd
            def _(eng):
                emit("pool", eng)

            @block.sync
            def _(eng):
                emit("sp", eng)
        self._reset()


def _rope_tables():
    p = np.arange(128)
    j = p % 16
    inv_freq = (10000.0 ** (-(j.astype(np.float64)) / 16.0))
    t = np.arange(SEQ)
    rows = t // 64
    cols = t % 64
    pos = np.where(((p % 64) < 32)[:, None], rows[None, :], cols[None, :]).astype(np.float64)
    ang = pos * inv_freq[:, None]
    ang = (pos.astype(np.float32) * inv_freq.astype(np.float32)[:, None]).astype(np.float32)
    cosT = np.cos(ang).astype(np.float32)
    sinT = np.sin(ang).astype(np.float32)
    sign = np.where((p % 32) < 16, -1.0, 1.0).astype(np.float32)[:, None]
    sinT = sinT * sign
    perm = np.where((p % 32) < 16, p + 16, p - 16)
    Pm = np.zeros((128, 128), np.float32)
    Pm[perm, p] = 1.0
    return cosT, sinT, Pm


def _band_masks():
    k = np.arange(128)[:, None]
    q = np.arange(128)[None, :]
    prev = (k >= q).astype(np.float32)
    nxt = (k <= q).astype(np.float32)
    return np.stack([prev, nxt], 0)


def _nat_patterns():
    pats = {}
    plist = []
    chunks = {}
    for qt in range(16):
        r0, r1 = 2 * qt, 2 * qt + 1
        rs0 = min(max(r0 - 4, 0), 24)
        rs1 = min(max(r1 - 4, 0), 24)
        lo = rs0 // 2
        hi = (rs1 + 7) // 2
        chunks[qt] = list(range(lo, hi + 1))
        for kc in chunks[qt]:
            interior = 2 <= qt <= 13
            key = ("i", kc - qt) if interior else ("b", qt, kc)
            if key not in pats:
                pats[key] = len(plist)
                plist.append((qt, kc))
            pats[(qt, kc)] = pats[key]
    npat = len(plist)
    dr = np.zeros((npat, 128, 128), np.int64)
    dc = np.zeros((npat, 128, 128), np.int64)
    mk = np.zeros((npat, 128, 128), np.float32)
    kk = np.arange(128)
    for pi, (qt, kc) in enumerate(plist):
        kr = 2 * kc + kk // 64
        kcol = kk % 64
        qr = 2 * qt + kk // 64
        qcol = kk % 64
        rs = np.clip(qr - 4, 0, 24)
        qs = np.clip(qcol - 8, 0, 48)
        vrow = (kr[:, None] >= rs[None, :]) & (kr[:, None] < rs[None, :] + 8)
        vcol = (kcol[:, None] >= qs[None, :]) & (kcol[:, None] < qs[None, :] + 16)
        mk[pi] = (vrow & vcol).astype(np.float32)
        dr[pi] = np.clip(kr[:, None] - qr[None, :] + 7, 0, 14)
        dc[pi] = np.clip(kcol[:, None] - qcol[None, :] + 15, 0, 30)
    return pats, chunks, dr, dc, mk, npat


_NATP = _nat_patterns()
NPAT = _NATP[5]


def build_program(limit=None, debug=False):
    nc = bass.Bass("TRN2", target_bir_lowering=False)

    _uid = [0]

    def un(name):
        _uid[0] += 1
        return "%s_u%d" % (name, _uid[0])

    def din(name, shape, dt=F32):
        return nc.dram_tensor(name, list(shape), dt, kind="ExternalInput").ap()

    x_in = din("x", [NB, SEQ, D])
    ctx_in = din("ctx", [NB, LCTX, D])
    c_in = din("c", [NB, D])
    cctx_in = din("c_ctx", [1, D])
    w_ada = din("w_ada", [DEPTH, D, 6 * D])
    b_ada = din("b_ada", [DEPTH, 6 * D])
    ln1_g = din("ln1_g", [DEPTH, D])
    ln1_b = din("ln1_b", [DEPTH, D])
    ln2_g = din("ln2_g", [DEPTH, D])
    ln2_b = din("ln2_b", [DEPTH, D])
    attn_wqk = din("attn_wqk", [2, D, 1536])
    attn_wv = din("attn_wv", [2, D, 256])
    attn_wo = din("attn_w_o", [2, D, D])
    attn_sink = din("attn_sink", [2, 16])
    conv_w_in = din("conv_w_in", [1, D, 3 * D])
    conv_w = din("conv_w", [1, 3, D])
    conv_w_out = din("conv_w_out", [1, D, D])
    nat_wqk = din("nat_wqk", [1, D, 2048])
    nat_wv = din("nat_wv", [1, D, 1024])
    nat_wo = din("nat_w_o", [1, D, D])
    nat_bias = din("nat_bias", [NPAT, 128, 16, 128])
    router_w = din("router_w", [DEPTH, D, 36])
    router_b = din("router_b", [DEPTH, 36])
    w_gate = din("expert_w_gate", [DEPTH, NEXP, D, FF])
    w_up = din("expert_w_up", [DEPTH, NEXP, D, FF])
    w_down = din("expert_w_down", [DEPTH, NEXP, FF, D])
    k_ident = din("k_ident", [128, 128])
    k_cos = din("k_cos", [128, SEQ])
    k_sin = din("k_sin", [128, SEQ])
    k_pm = din("k_pm", [128, 128])
    k_band = din("k_band", [2, 128, 128])
    k_natmask = din("k_natmask", [NPAT, 128, 128])
    k_ustrict = din("k_ustrict", [128, 128])
    k_eoff = din("k_eoff", [1, 32])

    out = nc.dram_tensor("out", [NB, SEQ, D], F32, kind="ExternalOutput").ap()

    X = nc.dram_tensor("X_scr", [NT * 128, D], F32).ap()
    MODR = nc.dram_tensor("MODR_scr", [DEPTH, 3, 6, D], F32).ap()
    XS = nc.dram_tensor("XS_scr", [NSLOT, D], BF16).ap()
    YS = nc.dram_tensor("YS_scr", [NSLOT, D], F32).ap()
    ETAB = nc.dram_tensor("ETAB_scr", [NPAT, 128, 16, 128], BF16).ap()
    QT = nc.dram_tensor("QT_scr", [128, 8, 2304], BF16).ap()

    DBG = {}
    if debug:
        DBG["T"] = nc.dram_tensor("dbgT", [4, 128, D], F32, kind="ExternalOutput").ap()
        DBG["O"] = nc.dram_tensor("dbgO", [128, D], BF16, kind="ExternalOutput").ap()
        DBG["Den"] = nc.dram_tensor("dbgDen", [128, 16], F32, kind="ExternalOutput").ap()
        DBG["H2"] = nc.dram_tensor("dbgH2", [128, D], BF16, kind="ExternalOutput").ap()
        DBG["RT"] = nc.dram_tensor("dbgRT", [128, 256], F32, kind="ExternalOutput").ap()
        DBG["tile"] = 0
        DBG["OA"] = nc.dram_tensor("dbgOA", [128, 3, 512], F32, kind="ExternalOutput").ap()
        DBG["SK"] = nc.dram_tensor("dbgSK", [128, 16], F32, kind="ExternalOutput").ap()

    def dbg_store(key, idx, ap, bid, tt):
        if not DBG or tt != DBG["tile"] or DBG.get("done_" + key + str(idx)):
            return
        DBG["done_" + key + str(idx)] = True
        dst = DBG[key][idx] if idx is not None else DBG[key]
        S.dma("sp", lambda e: e.dma_start(out=dst, in_=ap), "dbg" + key + str(idx), reads=[bid])

    with ExitStack() as ges:
        S = Sched(nc, ges)
        S.limit = limit

        def gsb(name, shape, dt):
            return ges.enter_context(nc.sbuf_tensor(un(name), list(shape), dt))

        identf = gsb("identf", [128, 128], F32)
        identb = gsb("identb", [128, 128], BF16)
        ustrict = gsb("ustrict", [128, 128], BF16)
        onesb = gsb("onesb", [128, 128], BF16)
        zerob = gsb("zerob", [128, 512], BF16)
        ones13 = gsb("ones13", [1, 4], F32)
        csT = gsb("csT", [128, 8, 3], F32)
        eoff = gsb("eoff", [128, 32], F32)
        destI = gsb("destI", [128, NT, 2], I32)
        gates = gsb("gates", [128, NT, 2], F32)
        cnt = gsb("cnt", [128, 32], F32)
        cnti = gsb("cnti", [128, 32], I32)
        S.cnt_ap = lambda idx: cnti[0:1, idx:idx + 1]

        _preg = {}

        def breg(e):
            if "r" not in _preg:
                _preg["r"] = e.to_reg(NSLOT - 1)
            return _preg["r"]

        def tile_src(layer, tt):
            b, j = divmod(tt, TPB)
            if layer == 0:
                if j < 16:
                    return x_in[b, j * 128:(j + 1) * 128, :]
                return ctx_in[b, (j - 16) * 128:(j - 15) * 128, :]
            return X[tt * 128:(tt + 1) * 128, :]

        def phase_init():
            with ExitStack() as es:
                tmpf = es.enter_context(nc.sbuf_tensor(un("init_tmp"), [128, 128], F32))
                S.dma("sp", lambda e: e.dma_start(out=identf[:], in_=k_ident), "c0", writes=["identf"])
                S.op("dve", lambda e: e.tensor_copy(out=identb[:], in_=identf[:]), reads=["identf"], writes=["identb"])
                S.dma("sp", lambda e: e.dma_start(out=tmpf[:], in_=k_ustrict), "c1", writes=["tmpf"])
                S.op("dve", lambda e: e.tensor_copy(out=ustrict[:], in_=tmpf[:]), reads=["tmpf"], writes=["ustrict"])
                S.op("pool", lambda e: e.memset(onesb[:], 1.0), writes=["onesb"])
                S.op("pool", lambda e: e.memset(zerob[:], 0.0), writes=["zerob"])
                S.op("pool", lambda e: e.memset(ones13[:], 1.0), writes=["ones13"])
                for v in range(3):
                    srcv = c_in[v, :] if v < 2 else cctx_in[0, :]
                    S.dma("sp", lambda e, v=v, srcv=srcv: e.dma_start(out=csT[:, :, v], in_=srcv.rearrange("(c p) -> p c", p=128),
                                                                       allow_slow_non_contiguous=True), "c2", writes=["csT"])
                S.op("act", lambda e: e.activation(out=csT[:], in_=csT[:], func=AF.Silu), reads=["csT"], writes=["csT"])
                S.dma("sp", lambda e: e.dma_start(out=eoff[:], in_=k_eoff[0, :].partition_broadcast(128)), "c4", writes=["eoff"])
                S.flush()

        def phase_ada(i, es_outer=None, do_flush=True):
            with ExitStack() as es_local:
                es = es_outer if es_outer is not None else es_local

                def sb(name, shape, dt):
                    return es.enter_context(nc.sbuf_tensor(un(name), list(shape), dt))
                wst = [sb("ada_w%d" % k, [128, 8, 512], F32) for k in range(2)]
                brow = sb("ada_b", [1, 6 * D], F32)
                modrow = sb("ada_mod", [3, 6 * D], F32)
                lng = sb("ada_lng", [3, 2, D], F32)
                outrow = sb("ada_out", [3, 6, D], F32)
                tmp = sb("ada_tmp", [3, D], F32)
                pm = [es.enter_context(nc.psum_tensor(un("ada_ps%d" % k), [3, 512], F32)) for k in range(2)]
                S.dma("sp", lambda e: e.dma_start(out=brow[:], in_=b_ada[i:i + 1, :]), "ab", writes=["brow"])
                S.dma("sp", lambda e: e.dma_start(out=lng[:, 0, :], in_=ln1_g[i, :].partition_broadcast(3)), "al", writes=["lng"])
                S.dma("sp", lambda e: e.dma_start(out=lng[:, 1, :], in_=ln1_b[i, :].partition_broadcast(3)), "al", writes=["lng"])
                wv = w_ada[i].rearrange("(c p) n -> p c n", p=128)
                for n in range(12):
                    k = n % 2
                    S.dma("sp", lambda e, n=n, k=k: e.dma_start(out=wst[k][:], in_=wv[:, :, n * 512:(n + 1) * 512]),
                          "aw%d" % k, writes=["wst%d" % k])
                    for c in range(8):
                        S.op("pe", lambda e, c=c, k=k: e.matmul(pm[k][:], lhsT=csT[:, c, :], rhs=wst[k][:, c, :],
                                                                 start=(c == 0), stop=False),
                             reads=["csT", "wst%d" % k], writes=["pm%d" % k])
                    S.op("pe", lambda e, n=n, k=k: e.matmul(pm[k][:], lhsT=ones13[0:1, 0:3], rhs=brow[0:1, n * 512:(n + 1) * 512],
                                                             start=False, stop=True),
                         reads=["ones13", "brow"], writes=["pm%d" % k])
                    S.op("act", lambda e, n=n, k=k: e.copy(out=modrow[:, n * 512:(n + 1) * 512], in_=pm[k][:]),
                         reads=["pm%d" % k], writes=["modrow"])

                def m(k):
                    return modrow[:, k * D:(k + 1) * D]
                S.op("dve", lambda e: e.tensor_copy(out=outrow[:, 0, :], in_=m(0)), reads=["modrow"], writes=["o0"])
                S.op("dve", lambda e: e.tensor_scalar_add(out=outrow[:, 1, :], in0=m(1), scalar1=1.0), reads=["modrow"], writes=["o1"])
                S.op("dve", lambda e: e.tensor_scalar_mul(out=outrow[:, 2, :], in0=m(2), scalar1=INV_ALPHA), reads=["modrow"], writes=["o2"])
                S.op("dve", lambda e: e.tensor_scalar_add(out=tmp[:], in0=m(4), scalar1=1.0), reads=["modrow"], writes=["tmp"])
                S.op("dve", lambda e: e.tensor_tensor(out=outrow[:, 3, :], in0=tmp[:], in1=lng[:, 0, :], op=ALU.mult),
                     reads=["tmp", "lng"], writes=["o3"])
                S.op("dve", lambda e: e.tensor_tensor(out=outrow[:, 4, :], in0=tmp[:], in1=lng[:, 1, :], op=ALU.mult),
                     reads=["tmp", "lng"], writes=["o4"])
                S.op("dve", lambda e: e.tensor_tensor(out=outrow[:, 4, :], in0=outrow[:, 4, :], in1=m(3), op=ALU.add),
                     reads=["o4", "modrow"], writes=["o4"])
                S.op("dve", lambda e: e.tensor_scalar_mul(out=outrow[:, 5, :], in0=m(5), scalar1=INV_ALPHA), reads=["modrow"], writes=["o5"])
                S.dma("sp", lambda e: e.dma_start(out=MODR[i], in_=outrow[:]), "ao",
                      reads=["o0", "o1", "o2", "o3", "o4", "o5"], writes=["MODR"])
                if do_flush:
                    S.flush()

        class Rot:
            def __init__(self, es, name, n, shape, dt, psum=False):
                self.bufs = []
                for k in range(n):
                    if psum:
                        t = es.enter_context(nc.psum_tensor(un("%s%d" % (name, k)), list(shape), dt))
                    else:
                        t = es.enter_context(nc.sbuf_tensor(un("%s%d" % (name, k)), list(shape), dt))
                    self.bufs.append((t, "%s%d" % (name, k)))
                self.i = 0

            def next(self):
                r = self.bufs[self.i % len(self.bufs)]
                self.i += 1
                return r

        def layer_norm_core(es_bufs, t, t_id, xn, xn_id):
            st, mv, rs = es_bufs
            stt, st_id = st.next()
            mvt, mv_id = mv.next()
            rst, rs_id = rs.next()
            S.op("dve", lambda e: e.bn_stats(out=stt[:, 0, :], in_=t[:, 0:512]), reads=[t_id], writes=[st_id])
            S.op("dve", lambda e: e.bn_stats(out=stt[:, 1, :], in_=t[:, 512:1024]), reads=[t_id], writes=[st_id])
            S.op("dve", lambda e: e.bn_aggr(out=mvt[:], in_=stt[:].rearrange("p a b -> p (a b)")), reads=[st_id], writes=[mv_id])
            S.op("dve", lambda e: e.tensor_scalar_add(out=rst[:, 0:1], in0=mvt[:, 1:2], scalar1=EPS2), reads=[mv_id], writes=[rs_id])
            S.op("act", lambda e: e.activation(out=rst[:, 0:1], in_=rst[:, 0:1], func=AF.Ln), reads=[rs_id], writes=[rs_id])
            S.op("act", lambda e: e.activation(out=rst[:, 0:1], in_=rst[:, 0:1], func=AF.Exp, scale=-0.5), reads=[rs_id], writes=[rs_id])
            S.op("dve", lambda e: e.scalar_tensor_tensor(out=rst[:, 1:2], in0=mvt[:, 0:1], scalar=-1.0, in1=rst[:, 0:1],
                                                         op0=ALU.mult, op1=ALU.mult), reads=[mv_id, rs_id], writes=[rs_id])
            S.op("act", lambda e: e.activation(out=xn[:], in_=t[:], func=AF.Identity, scale=rst[:, 0:1], bias=rst[:, 1:2]),
                 reads=[t_id, rs_id], writes=[xn_id])

        class MixCtx:
            pass

        def mixer_common_alloc(es, i):
            M = MixCtx()

            def sb(name, shape, dt):
                return es.enter_context(nc.sbuf_tensor(un(name), list(shape), dt))
            M.G1 = sb("mx_G1", [128, D], F32)
            M.A2 = sb("mx_A2", [128, D], F32)
            M.S2 = sb("mx_S2", [128, D], F32)
            M.lg = sb("mx_lg", [128, D], F32)
            M.lb = sb("mx_lb", [128, D], F32)
            M.wr = sb("mx_wr", [128, 8, 36], BF16)
            M.wrf = sb("mx_wrf", [128, 8, 36], F32)
            M.rb = sb("mx_rb", [128, 36], F32)
            M.xt = Rot(es, "mx_xt", 2, [128, D], F32)
            M.t = Rot(es, "mx_t", 2, [128, D], F32)
            M.xn = Rot(es, "mx_xn", 2, [128, D], F32)
            M.h2 = Rot(es, "mx_h2", 2, [128, D], BF16)
            M.h2T = Rot(es, "mx_h2T", 2, [128, 8, 128], BF16)
            M.st = Rot(es, "mx_st", 2, [128, 2, 6], F32)
            M.mv = Rot(es, "mx_mv", 2, [128, 2], F32)
            M.rs = Rot(es, "mx_rs", 2, [128, 2], F32)
            M.rt = Rot(es, "mx_rt", 2, [128, 256], F32)
            M.selb = Rot(es, "mx_sel", 2, [128, 32], BF16)
            M.cur_v = None
            S.dma("pool", lambda e: e.dma_start(out=M.wr[:], in_=router_w[i].rearrange("(c p) n -> p c n", p=128)), "mwr", writes=["wr"])
            S.dma("sp", lambda e: e.dma_start(out=M.rb[:], in_=router_b[i, :].partition_broadcast(128)), "mrb", writes=["rb"])
            S.dma("sp", lambda e: e.dma_start(out=M.lg[:], in_=ln1_g[i, :].partition_broadcast(128)), "mlg", writes=["lg"])
            S.dma("sp", lambda e: e.dma_start(out=M.lb[:], in_=ln1_b[i, :].partition_broadcast(128)), "mlb", writes=["lb"])
            return M

        def load_variant(M, i, v):
            if M.cur_v == v:
                return
            M.cur_v = v
            S.dma("sp", lambda e: e.dma_start(out=M.G1[:], in_=MODR[i, v, 2, :].partition_broadcast(128)), "mG1", writes=["G1"])
            S.dma("sp", lambda e: e.dma_start(out=M.A2[:], in_=MODR[i, v, 3, :].partition_broadcast(128)), "mA2", writes=["A2"])
            S.dma("sp", lambda e: e.dma_start(out=M.S2[:], in_=MODR[i, v, 4, :].partition_broadcast(128)), "mS2", writes=["S2"])

        def out_stage(M, P, i, tt, po, po_ids):
            xt, xt_id = M.xt.next()
            t, t_id = M.t.next()
            xn, xn_id = M.xn.next()
            h2, h2_id = M.h2.next()
            h2T, h2T_id = M.h2T.next()
            rt, rt_id = M.rt.next()
            selb, selb_id = M.selb.next()
            src = tile_src(i, tt)
            S.dma("sp", lambda e: e.dma_start(out=xt[:], in_=src), xt_id, writes=[xt_id])
            S.op("dve", lambda e: e.tensor_tensor(out=t[:], in0=po, in1=M.G1[:], op=ALU.mult), reads=list(po_ids) + ["G1"], writes=[t_id])
            dbg_store("T", 0, t[:], t_id, tt)
            S.op("dve", lambda e: e.tensor_tensor(out=t[:], in0=t[:], in1=xt[:], op=ALU.add), reads=[t_id, xt_id], writes=[t_id])
            dbg_store("T", 1, t[:], t_id, tt)
            layer_norm_core((M.st, M.mv, M.rs), t, t_id, xn, xn_id)
            dbg_store("T", 2, xn[:], xn_id, tt)
            S.op("dve", lambda e: e.tensor_tensor(out=t[:], in0=xn[:], in1=M.A2[:], op=ALU.mult), reads=[xn_id, "A2"], writes=[t_id])
            S.op("dve", lambda e: e.tensor_tensor(out=h2[:], in0=t[:], in1=M.S2[:], op=ALU.add), reads=[t_id, "S2"], writes=[h2_id])
            dbg_store("H2", None, h2[:], h2_id, tt)
            S.op("dve", lambda e: e.tensor_tensor(out=xn[:], in0=xn[:], in1=M.lg[:], op=ALU.mult), reads=[xn_id, "lg"], writes=[xn_id])
            S.op("dve", lambda e: e.tensor_tensor(out=xn[:], in0=xn[:], in1=M.lb[:], op=ALU.add), reads=[xn_id, "lb"], writes=[xn_id])
            S.dma("sp", lambda e: e.dma_start(out=X[tt * 128:(tt + 1) * 128, :], in_=xn[:]), "st_" + xn_id, reads=[xn_id], writes=["X%d" % tt])
            for hf4 in range(2):
                for c in range(4):
                    S.op("pe", lambda e, c=c, hf4=hf4: e.transpose(out=P.ptb[:, c, :], in_=h2[:, (hf4 * 4 + c) * 128:(hf4 * 4 + c + 1) * 128], identity=identb[:]),
                         reads=[h2_id, "identb"], writes=["ptb"])
                S.op("act", lambda e, hf4=hf4: e.copy(out=h2T[:, hf4 * 4:(hf4 + 1) * 4, :], in_=P.ptb), reads=["ptb"], writes=[h2T_id])
            for c in range(8):
                S.op("pe", lambda e, c=c: e.matmul(P.plg[:, 0:36], lhsT=h2T[:, c, :], rhs=M.wr[:, c, :], start=(c == 0), stop=(c == 7)),
                     reads=[h2T_id, "wr"], writes=["plg"])
            if not (KDBG & 2):
                route(M, P, tt, rt, rt_id, selb, selb_id)
            dbg_store("RT", None, rt[:], rt_id, tt)
            for k in range(2 if not (KDBG & 6) else 0):
                S.dma("pool", lambda e, k=k: e.indirect_dma_start(
                    out=XS, out_offset=bass.IndirectOffsetOnAxis(ap=destI[:, tt, k:k + 1], axis=0),
                    in_=h2[:], in_offset=None, bounds_check=breg(e), oob_is_err=False),
                    "sc_" + h2_id + str(k), reads=[h2_id, "destI%d" % tt], writes=["XS"])

        def route(M, P, tt, rt, rt_id, selb, selb_id):
            lgt = rt[:, 0:36]
            gl = rt[:, 0:4]
            el = rt[:, 4:36]
            gm = rt[:, 40:44]
            pen = rt[:, 44:48]
            em = rt[:, 48:80]
            oh1 = rt[:, 80:112]
            em2 = rt[:, 112:144]
            oh2 = rt[:, 144:176]
            slot = rt[:, 176:208]
            prod = rt[:, 208:240]
            sc = rt[:, 240:256]
            gex = rt[:, 36:40]
            W = [rt_id]
            R = [rt_id]

            def dv(fn, extra_r=(), extra_w=()):
                S.op("dve", fn, reads=R + list(extra_r), writes=W + list(extra_w))
            dv(lambda e: e.tensor_tensor(out=lgt, in0=P.plg[:, 0:36], in1=M.rb[:], op=ALU.add), extra_r=["plg", "rb"])
            dv(lambda e: e.reduce_max(out=sc[:, 0:1], in_=gl, axis=AX))
            dv(lambda e: e.tensor_scalar(out=gm, in0=gl, scalar1=sc[:, 0:1], scalar2=None, op0=ALU.is_ge))
            dv(lambda e: e.tensor_scalar_mul(out=sc[:, 1:2], in0=sc[:, 0:1], scalar1=-1.0))
            S.op("act", lambda e: e.activation(out=gex, in_=gl, func=AF.Exp, bias=sc[:, 1:2], scale=1.0, accum_out=sc[:, 2:3]),
                 reads=R, writes=W)
            dv(lambda e: e.reciprocal(out=sc[:, 3:4], in_=sc[:, 2:3]))
            dv(lambda e: e.tensor_scalar(out=pen, in0=gm, scalar1=BIG, scalar2=-BIG, op0=ALU.mult, op1=ALU.add))
            dv(lambda e: e.tensor_tensor(out=em.rearrange("p (g k) -> p g k", k=8), in0=el.rearrange("p (g k) -> p g k", k=8),
                                         in1=pen.unsqueeze(2).to_broadcast([128, 4, 8]), op=ALU.add))
            dv(lambda e: e.reduce_max(out=sc[:, 4:5], in_=em, axis=AX))
            dv(lambda e: e.tensor_scalar(out=oh1, in0=em, scalar1=sc[:, 4:5], scalar2=None, op0=ALU.is_ge))
            dv(lambda e: e.scalar_tensor_tensor(out=em2, in0=oh1, scalar=-BIG, in1=em, op0=ALU.mult, op1=ALU.add))
            dv(lambda e: e.reduce_max(out=sc[:, 5:6], in_=em2, axis=AX))
            dv(lambda e: e.tensor_scalar(out=oh2, in0=em2, scalar1=sc[:, 5:6], scalar2=None, op0=ALU.is_ge))
            dv(lambda e: e.tensor_tensor(out=sc[:, 6:7], in0=sc[:, 5:6], in1=sc[:, 4:5], op=ALU.subtract))
            S.op("act", lambda e: e.activation(out=sc[:, 7:8], in_=sc[:, 6:7], func=AF.Exp), reads=R, writes=W)
            dv(lambda e: e.tensor_scalar_add(out=sc[:, 7:8], in0=sc[:, 7:8], scalar1=1.0))
            dv(lambda e: e.reciprocal(out=sc[:, 8:9], in_=sc[:, 7:8]))
            dv(lambda e: e.tensor_tensor(out=gates[:, tt, 0:1], in0=sc[:, 8:9], in1=sc[:, 3:4], op=ALU.mult), extra_w=["gates%d" % tt])
            dv(lambda e: e.tensor_tensor(out=gates[:, tt, 1:2], in0=sc[:, 3:4], in1=gates[:, tt, 0:1], op=ALU.subtract),
               extra_r=["gates%d" % tt], extra_w=["gates%d" % tt])
            dv(lambda e: e.tensor_tensor(out=selb[:], in0=oh1, in1=oh2, op=ALU.add), extra_w=[selb_id])
            S.op("pe", lambda e: e.matmul(P.plg[:, 64:96], lhsT=ustrict[:], rhs=selb[:], start=True, stop=True),
                 reads=[selb_id, "ustrict"], writes=["prk"])
            S.op("pe", lambda e: e.matmul(P.plg[:, 128:160], lhsT=onesb[:], rhs=selb[:], start=True, stop=True),
                 reads=[selb_id, "onesb"], writes=["ptot"])
            dv(lambda e: e.tensor_tensor(out=slot, in0=P.plg[:, 64:96], in1=cnt[:], op=ALU.add), extra_r=["prk", "cnt"])
            dv(lambda e: e.tensor_scalar(out=prod, in0=slot, scalar1=float(CAP), scalar2=4.0e6, op0=ALU.is_ge, op1=ALU.mult))
            dv(lambda e: e.tensor_tensor(out=slot, in0=slot, in1=prod, op=ALU.add))
            dv(lambda e: e.tensor_tensor(out=slot, in0=slot, in1=eoff[:], op=ALU.add), extra_r=["eoff"])
            dv(lambda e: e.tensor_tensor(out=cnt[:], in0=cnt[:], in1=P.plg[:, 128:160], op=ALU.add), extra_r=["ptot", "cnt"], extra_w=["cnt"])
            dv(lambda e: e.tensor_tensor(out=prod, in0=oh1, in1=slot, op=ALU.mult))
            dv(lambda e: e.reduce_sum(out=sc[:, 9:10], in_=prod, axis=AX))
            dv(lambda e: e.tensor_tensor(out=prod, in0=oh2, in1=slot, op=ALU.mult))
            dv(lambda e: e.reduce_sum(out=sc[:, 10:11], in_=prod, axis=AX))
            dv(lambda e: e.tensor_copy(out=destI[:, tt, :], in_=sc[:, 9:11]), extra_w=["destI%d" % tt])

        def make_hT(es, P, i, b, hT, scal):
            xr = Rot(es, "hx_x", 3, [128, D], F32)
            for seg, v in ((0, b), (1, 2)):
                S.dma("sp", lambda e, v=v: e.dma_start(out=scal[:], in_=MODR[i, v, 0:2, :].rearrange("k (c p) -> p k c", p=128),
                                                        allow_slow_non_contiguous=True), "hsc", writes=["scal"])
                tiles = range(16) if seg == 0 else range(16, 18)
                for j in tiles:
                    tt = b * TPB + j
                    xt, xid = xr.next()
                    src = tile_src(i, tt)
                    S.dma("sp", lambda e, xt=xt, src=src: e.dma_start(out=xt[:], in_=src), xid, reads=["X%d" % tt], writes=[xid])
                    for c in range(8):
                        S.op("pe", lambda e, c=c, xt=xt: e.transpose(out=P.ptf[:, c, :], in_=xt[:, c * 128:(c + 1) * 128], identity=identf[:]),
                             reads=[xid, "identf"], writes=["ptf"])
                    for c in range(8):
                        if c % 2 == 0:
                            S.op("act", lambda e, c=c, j=j: e.activation(out=hT[:, c, j * 128:(j + 1) * 128], in_=P.ptf[:, c, :], func=AF.Identity,
                                                                          scale=scal[:, 1, c:c + 1], bias=scal[:, 0, c:c + 1]),
                                 reads=["ptf", "scal"], writes=["hT"])
                        else:
                            S.op("dve", lambda e, c=c, j=j: e.tensor_scalar(out=hT[:, c, j * 128:(j + 1) * 128], in0=P.ptf[:, c, :],
                                                                             scalar1=scal[:, 1, c:c + 1], scalar2=scal[:, 0, c:c + 1],
                                                                             op0=ALU.mult, op1=ALU.add),
                                 reads=["ptf", "scal"], writes=["hT"])

        HB = [(0, h) if h < 6 else ((1, h - 6) if h < 11 else (2, h - 11)) for h in range(16)]
        TOKG = [(0, 512), (512, 512), (1024, 512), (1536, 512), (2048, 256)]

        class PsumSet:
            pass

        def alloc_psum(es):
            P = PsumSet()

            def ps(name, shape, dt):
                return es.enter_context(nc.psum_tensor(un(name), list(shape), dt))
            P.ptf = ps("ps_ptf", [128, 8, 128], F32)
            P.pA = ps("ps_A", [128, 512], F32)
            P.pB = ps("ps_B", [128, 512], F32)
            P.oacc = ps("ps_oacc", [128, 3, 512], F32)
            P.b7 = ps("ps_b7", [128, 512], F32)
            P.ptb = P.b7[:, 0:256].bitcast(BF16).rearrange("p (c t) -> p c t", t=128)
            P.plg = P.b7[:, 256:512]
            P.pC = P.oacc[:, 0, :]
            return P

        def phase_attn(i, j, kind, last):
            is_a = kind == 0
            nkv = 4 if is_a else 16
            nqk_cols = 1536 if is_a else 2048
            nk_chunks = 4 if is_a else 8
            nv_cols = 256 if is_a else 1024
            wqk = attn_wqk[j] if is_a else nat_wqk[j]
            wv = attn_wv[j] if is_a else nat_wv[j]
            wo = attn_wo[j] if is_a else nat_wo[j]
            scale = 0.125
            if not is_a:
                pats, nat_chunks = _NATP[0], _NATP[1]
            for b in range(NB):
                with ExitStack() as es:
                    def sb(name, shape, dt):
                        return es.enter_context(nc.sbuf_tensor(un(name), list(shape), dt))
                    P = alloc_psum(es)
                    kT = sb("at_kT", [128, nk_chunks, 2304], BF16)
                    va = sb("at_va", [128, TPB, nkv, 65], BF16)
                    S.op("pool", lambda e: e.memset(va[:], 1.0), writes=["va"])
                    with ExitStack() as es1:
                        def sb1(name, shape, dt):
                            return es1.enter_context(nc.sbuf_tensor(un(name), list(shape), dt))
                        hT = sb1("at_hT", [128, 8, 2304], BF16)
                        scal = sb1("at_scal", [128, 2, 8], F32)
                        wstg = Rot(es1, "at_wst", 2, [128, 8, 256], F32)
                        wbf = Rot(es1, "at_wbf", 2, [128, 8, 256], BF16)
                        qst = Rot(es1, "at_qst", 2, [128, 512], BF16)
                        if is_a:
                            raw = Rot(es1, "at_raw", 2, [128, 512], BF16)
                            tm1 = Rot(es1, "at_tm1", 2, [128, 512], F32)
                            tm2 = Rot(es1, "at_tm2", 2, [128, 512], F32)
                            cosT = sb1("at_cos", [128, SEQ], F32)
                            sinT = sb1("at_sin", [128, SEQ], F32)
                            pmf = sb1("at_pmf", [128, 128], F32)
                            pmb = sb1("at_pmb", [128, 128], BF16)
                            S.dma("sp", lambda e: e.dma_start(out=cosT[:], in_=k_cos), "rc", writes=["cosT"])
                            S.dma("sp", lambda e: e.dma_start(out=sinT[:], in_=k_sin), "rs", writes=["sinT"])
                            S.dma("sp", lambda e: e.dma_start(out=pmf[:], in_=k_pm), "rp", writes=["pmf"])
                            S.op("dve", lambda e: e.tensor_copy(out=pmb[:], in_=pmf[:]), reads=["pmf"], writes=["pmb"])
                        make_hT(es1, P, i, b, hT, scal)
                        wqk_v = wqk.rearrange("(c p) n -> p c n", p=128)
                        wv_v = wv.rearrange("(c p) n -> p c n", p=128)
                        pab = [(P.pA, "pA"), (P.pB, "pB")]
                        pcount = 0
                        for g in range(nqk_cols // 256):
                            wst, wst_id = wstg.next()
                            wb, wb_id = wbf.next()
                            S.dma("pool", lambda e, wb=wb, g=g: e.dma_start(out=wb[:], in_=wqk_v[:, :, g * 256:(g + 1) * 256]),
                                  "ld" + wb_id, writes=[wb_id])
                            for cc in range(2):
                                chunk = g * 2 + cc
                                isq = chunk < 8
                                dchunk = chunk if isq else chunk - 8
                                for (t0, tn) in TOKG:
                                    if isq:
                                        qs_, dst_id = qst.next()
                                        dst_ap = qs_[:, 0:tn]
                                    else:
                                        dst_ap = kT[:, dchunk, t0:t0 + tn]
                                        dst_id = "kT"
                                    pp, pp_id = pab[pcount % 2]
                                    pcount += 1
                                    for c in range(8):
                                        S.op("pe", lambda e, pp=pp, wb=wb, cc=cc, c=c, t0=t0, tn=tn: e.matmul(
                                            pp[:, 0:tn], lhsT=wb[:, c, cc * 128:(cc + 1) * 128], rhs=hT[:, c, t0:t0 + tn],
                                            start=(c == 0), stop=(c == 7)), reads=[wb_id, "hT"], writes=[pp_id])
                                    rope = is_a and t0 < SEQ
                                    if not rope:
                                        S.op("act", lambda e, pp=pp, dst_ap=dst_ap, tn=tn: e.copy(
                                            out=dst_ap, in_=pp[:, 0:tn]), reads=[pp_id], writes=[dst_id])
                                    else:
                                        rw, rw_id = raw.next()
                                        a1, a1_id = tm1.next()
                                        a2, a2_id = tm2.next()
                                        S.op("act", lambda e, pp=pp, rw=rw: e.copy(out=rw[:], in_=pp[:]), reads=[pp_id], writes=[rw_id])
                                        S.op("pe", lambda e, rw=rw: e.matmul(P.pC, lhsT=pmb[:], rhs=rw[:], start=True, stop=True),
                                             reads=[rw_id, "pmb"], writes=["pC"])
                                        S.op("dve", lambda e, a1=a1, t0=t0: e.tensor_tensor(out=a1[:], in0=P.pC, in1=sinT[:, t0:t0 + 512], op=ALU.mult),
                                             reads=["pC", "sinT"], writes=[a1_id])
                                        S.op("dve", lambda e, a2=a2, rw=rw, t0=t0: e.tensor_tensor(out=a2[:], in0=rw[:], in1=cosT[:, t0:t0 + 512], op=ALU.mult),
                                             reads=[rw_id, "cosT"], writes=[a2_id])
                                        S.op("dve", lambda e, a1=a1, a2=a2, dst_ap=dst_ap: e.tensor_tensor(
                                            out=dst_ap, in0=a1[:], in1=a2[:], op=ALU.add),
                                            reads=[a1_id, a2_id], writes=[dst_id])
                                    if isq:
                                        S.dma("sp", lambda e, dst_ap=dst_ap, dchunk=dchunk, t0=t0, tn=tn: e.dma_start(
                                            out=QT[:, dchunk, t0:t0 + tn], in_=dst_ap), "st" + dst_id, reads=[dst_id], writes=["QT"])
                        for g in range(nv_cols // 256):
                            ncol = 256
                            wst, wst_id = wstg.next()
                            wb, wb_id = wbf.next()
                            S.dma("pool", lambda e, wb=wb, g=g, ncol=ncol: e.dma_start(out=wb[:, :, 0:ncol], in_=wv_v[:, :, g * 256:g * 256 + ncol]),
                                  "ld" + wb_id, writes=[wb_id])
                            nh = ncol // 64
                            for jt in range(TPB):
                                pp, pp_id = pab[pcount % 2]
                                pcount += 1
                                for c in range(8):
                                    S.op("pe", lambda e, pp=pp, wb=wb, c=c, jt=jt, ncol=ncol: e.matmul(
                                        pp[:, 0:ncol], lhsT=hT[:, c, jt * 128:(jt + 1) * 128], rhs=wb[:, c, 0:ncol],
                                        start=(c == 0), stop=(c == 7)), reads=[wb_id, "hT"], writes=[pp_id])
                                S.op("act", lambda e, pp=pp, jt=jt, g=g, nh=nh, ncol=ncol: e.copy(
                                    out=va[:, jt, g * 4:g * 4 + nh, 0:64], in_=pp[:, 0:ncol].rearrange("p (h d) -> p h d", d=64)),
                                    reads=[pp_id], writes=["va"])
                        S.flush()
                    with ExitStack() as es2:
                        def sb2(name, shape, dt):
                            return es2.enter_context(nc.sbuf_tensor(un(name), list(shape), dt))
                        M = mixer_common_alloc(es2, i)
                        wob = sb2("at_wob", [128, 8, D], BF16)
                        wstg = Rot(es2, "at_wst2_", 2, [128, 8, 256], F32)
                        for g in range(4):
                            wst, wst_id = wstg.next()
                            S.dma("pool", lambda e, g=g: e.dma_start(out=wob[:, :, g * 256:(g + 1) * 256], in_=wo.rearrange("(c p) n -> p c n", p=128)[:, :, g * 256:(g + 1) * 256]),
                                  "ldwob", writes=["wob"])
                        sinkx = sb2("at_sink", [128, 16], F32)
                        if is_a:
                            S.dma("sp", lambda e: e.dma_start(out=sinkx[:], in_=attn_sink[j, :].partition_broadcast(128)), "snk", writes=["sinkx"])
                            S.op("act", lambda e: e.activation(out=sinkx[:], in_=sinkx[:], func=AF.Exp), reads=["sinkx"], writes=["sinkx"])
                            bandf = sb2("at_bandf", [128, 2, 128], F32)
                            bandb = sb2("at_bandb", [128, 2, 128], BF16)
                            S.dma("sp", lambda e: e.dma_start(out=bandf[:], in_=k_band.rearrange("m k q -> k m q")), "bnd", writes=["bandf"])
                            S.op("dve", lambda e: e.tensor_copy(out=bandb[:], in_=bandf[:]), reads=["bandf"], writes=["bandb"])
                        else:
                            S.op("pool", lambda e: e.memset(sinkx[:], 0.0), writes=["sinkx"])
                            etr = Rot(es2, "at_et", 2, [128, 16, 128], BF16)
                        ptr_ = Rot(es2, "at_pt", 4, [128, 512], BF16)
                        otok = Rot(es2, "at_otok", 2, [128, D], BF16)
                        oTr = Rot(es2, "at_oT", 2, [128, 8, 128], BF16)
                        den = Rot(es2, "at_den", 2, [128, 16], F32)
                        pab = [(P.pA, "pA"), (P.pB, "pB")]
                        pcount = 0
                        qtiles = list(range(16)) + ([] if last else [16, 17])
                        qtr = Rot(es2, "at_qt", 2, [128, 16, 128], BF16)
                        for (qb_, qb_id) in qtr.bufs:
                            S.op("pool", lambda e, qb_=qb_: e.memset(qb_[:], 0.0), writes=[qb_id])
                        def kcs_for(jq):
                            if jq >= 16:
                                return [(16, None), (17, None)]
                            if is_a:
                                kcs = []
                                if jq > 0:
                                    kcs.append((jq - 1, ("band", 0)))
                                kcs.append((jq, None))
                                if jq < 15:
                                    kcs.append((jq + 1, ("band", 1)))
                                return kcs + [(16, None), (17, None)]
                            return [(kc, ("nat", pats[(jq, kc)])) for kc in nat_chunks[jq]] + [(16, None), (17, None)]

                        pstate = {"n": 0}

                        def rec_core(jq):
                            qT, qT_id = qtr.next()
                            qTv = qT[:].rearrange("p (j two) q -> p j two q", two=2)
                            kcs = kcs_for(jq)

                            def pro():
                                S.dma("sp", lambda e: e.dma_start(out=qTv[0:64, :, 0, :], in_=QT[0:64, :, jq * 128:(jq + 1) * 128]),
                                      qT_id, reads=["QT"], writes=[qT_id])
                                S.dma("sp", lambda e: e.dma_start(out=qTv[64:128, :, 1, :], in_=QT[64:128, :, jq * 128:(jq + 1) * 128]),
                                      qT_id, reads=["QT"], writes=[qT_id])
                                for bank in range(3):
                                    S.op("pe", lambda e, bank=bank: e.matmul(P.oacc[:, bank, :], lhsT=zerob[:, 0:128], rhs=zerob[:], start=True, stop=True),
                                         reads=["zerob"], writes=["oacc"])
                            prol, _ = S.capture(pro)
                            steps = []
                            for ci, (kc, msk) in enumerate(kcs):
                                et = None
                                et_id = None
                                if msk is not None and msk[0] == "nat":
                                    et, et_id = etr.next()
                                for hg in range(4):
                                    pp, pp_id = pab[pstate["n"] % 2]
                                    pstate["n"] += 1
                                    pt, pt_id = ptr_.next()

                                    def s_part(ci=ci, kc=kc, msk=msk, hg=hg, pp=pp, pp_id=pp_id, et=et, et_id=et_id):
                                        if et is not None and hg == 0:
                                            S.dma("sp", lambda e: e.dma_start(out=et[:], in_=ETAB[msk[1]]), et_id, reads=["ETAB"], writes=[et_id])
                                        for hh in range(4):
                                            h = hg * 4 + hh
                                            kch = hg if is_a else h // 2
                                            S.op("pe", lambda e, hh=hh, kch=kch, h=h: e.matmul(
                                                pp[:, hh * 128:(hh + 1) * 128], lhsT=kT[:, kch, kc * 128:(kc + 1) * 128],
                                                rhs=qT[:, h, :], start=True, stop=True),
                                                reads=[qT_id, "kT"], writes=[pp_id])

                                    def r_part(ci=ci, kc=kc, msk=msk, hg=hg, pp=pp, pp_id=pp_id, pt=pt, pt_id=pt_id, et=et, et_id=et_id, n=len(kcs)):
                                        S.op("act", lambda e: e.activation(out=pt[:], in_=pp[:], func=AF.Exp, scale=scale),
                                             reads=[pp_id], writes=[pt_id])
                                        if msk is not None:
                                            ptv = pt[:].rearrange("p (h q) -> p h q", q=128)
                                            if msk[0] == "band":
                                                S.op("pool", lambda e: e.tensor_tensor(out=ptv, in0=ptv,
                                                     in1=bandb[:, msk[1], :].unsqueeze(1).to_broadcast([128, 4, 128]), op=ALU.mult),
                                                     reads=[pt_id, "bandb"], writes=[pt_id])
                                            else:
                                                S.op("pool", lambda e: e.tensor_tensor(out=ptv, in0=ptv, in1=et[:, hg * 4:(hg + 1) * 4, :], op=ALU.mult),
                                                     reads=[pt_id, et_id], writes=[pt_id])
                                        for hh in range(4):
                                            h = hg * 4 + hh
                                            kvh = hg if is_a else h
                                            bank, off = HB[h]
                                            S.op("pe", lambda e, hh=hh, kvh=kvh, bank=bank, off=off: e.matmul(
                                                P.oacc[:, bank, off * 65:(off + 1) * 65], lhsT=pt[:, hh * 128:(hh + 1) * 128],
                                                rhs=va[:, kc, kvh, :], start=False, stop=(ci == n - 1)),
                                                reads=[pt_id, "va"], writes=["oacc"])
                                    sl, _ = S.capture(s_part)
                                    rl, _ = S.capture(r_part)
                                    steps.append((sl, rl))
                            return prol, steps

                        def core_units(steps):
                            units = []
                            n = len(steps)
                            for k in range(min(2, n)):
                                units.append(steps[k][0])
                            for k in range(n):
                                units.append(steps[k][1])
                                if k + 2 < n:
                                    units.append(steps[k + 2][0])
                            return units

                        def norm(jq):
                            tt = b * TPB + jq
                            dn, dn_id = den.next()
                            ot, ot_id = otok.next()
                            oT, oT_id = oTr.next()
                            for bank in range(3):
                                nh = (6, 5, 5)[bank]
                                h0 = (0, 6, 11)[bank]
                                S.op("dve", lambda e, bank=bank, nh=nh, h0=h0: e.tensor_tensor(
                                    out=dn[:, h0:h0 + nh], in0=P.oacc[:, bank, 0:nh * 65].rearrange("p (h d) -> p h d", d=65)[:, :, 64],
                                    in1=sinkx[:, h0:h0 + nh], op=ALU.add), reads=["oacc", "sinkx"], writes=[dn_id])
                            S.op("dve", lambda e: e.reciprocal(out=dn[:], in_=dn[:]), reads=[dn_id], writes=[dn_id])
                            for bank in range(3):
                                nh = (6, 5, 5)[bank]
                                h0 = (0, 6, 11)[bank]
                                S.op("dve", lambda e, bank=bank, nh=nh, h0=h0: e.tensor_tensor(
                                    out=ot[:, h0 * 64:(h0 + nh) * 64].rearrange("p (h d) -> p h d", d=64),
                                    in0=P.oacc[:, bank, 0:nh * 65].rearrange("p (h d) -> p h d", d=65)[:, :, 0:64],
                                    in1=dn[:, h0:h0 + nh].unsqueeze(2).to_broadcast([128, nh, 64]), op=ALU.mult),
                                    reads=["oacc", dn_id], writes=[ot_id])
                            return ot, ot_id, oT, oT_id

                        def tail(jq, ot, ot_id, oT, oT_id):
                            tt = b * TPB + jq
                            load_variant(M, i, b if jq < 16 else 2)
                            for hf4 in range(2):
                                for c in range(4):
                                    S.op("pe", lambda e, c=c, hf4=hf4: e.transpose(out=P.ptb[:, c, :], in_=ot[:, (hf4 * 4 + c) * 128:(hf4 * 4 + c + 1) * 128], identity=identb[:]),
                                         reads=[ot_id, "identb"], writes=["ptb"])
                                S.op("act", lambda e, hf4=hf4: e.copy(out=oT[:, hf4 * 4:(hf4 + 1) * 4, :], in_=P.ptb), reads=["ptb"], writes=[oT_id])
                            po = P.ptf[:].rearrange("p c t -> p (c t)")
                            for hf in range(2):
                                for c in range(8):
                                    S.op("pe", lambda e, c=c, hf=hf: e.matmul(po[:, hf * 512:(hf + 1) * 512], lhsT=oT[:, c, :],
                                                                              rhs=wob[:, c, hf * 512:(hf + 1) * 512],
                                                                              start=(c == 0), stop=(c == 7)),
                                         reads=[oT_id, "wob"], writes=["ptf"])
                            out_stage(M, P, i, tt, po, ["ptf"])

                        def interleave(units, tl):
                            out_ = []
                            nu = max(1, len(units))
                            per = -(-len(tl) // nu)
                            ti = 0
                            for u in units:
                                out_.extend(u)
                                out_.extend(tl[ti:ti + per])
                                ti += per
                            out_.extend(tl[ti:])
                            return out_

                        prol, steps = rec_core(qtiles[0])
                        S.replay(prol)
                        for u in core_units(steps):
                            S.replay(u)
                        for qi, jq in enumerate(qtiles):
                            nl, nr = S.capture(norm, jq)
                            S.replay(nl)
                            tl, _ = S.capture(tail, jq, *nr)
                            if qi + 1 < len(qtiles):
                                prol, steps = rec_core(qtiles[qi + 1])
                                S.replay(prol)
                                S.replay(interleave(core_units(steps), tl))
                            else:
                                S.replay(tl)
                        S.flush()

        def phase_nat_table():
            with ExitStack() as es:
                bt = Rot(es, "nt_b", 2, [128, 16, 128], F32)
                mt = Rot(es, "nt_m", 2, [128, 128], F32)
                eo = Rot(es, "nt_e", 2, [128, 16, 128], BF16)
                for pid in range(NPAT):
                    b_, b_id = bt.next()
                    m_, m_id = mt.next()
                    e_, e_id = eo.next()
                    S.dma("sp", lambda e, b_=b_, pid=pid: e.dma_start(out=b_[:], in_=nat_bias[pid]), b_id, writes=[b_id])
                    S.dma("sp", lambda e, m_=m_, pid=pid: e.dma_start(out=m_[:], in_=k_natmask[pid]), m_id, writes=[m_id])
                    S.op("act", lambda e, b_=b_: e.activation(out=b_[:], in_=b_[:], func=AF.Exp), reads=[b_id], writes=[b_id])
                    S.op("dve", lambda e, b_=b_, m_=m_, e_=e_: e.tensor_tensor(out=e_[:], in0=b_[:], in1=m_[:].unsqueeze(1).to_broadcast([128, 16, 128]),
                                                                              op=ALU.mult), reads=[b_id, m_id], writes=[e_id])
                    S.dma("sp", lambda e, e_=e_, pid=pid: e.dma_start(out=ETAB[pid], in_=e_[:]), "st" + e_id, reads=[e_id], writes=["ETAB"])
                S.flush()

        def phase_conv(i, j, last):
            win = conv_w_in[j].rearrange("(c p) n -> p c n", p=128)
            for b in range(NB):
                with ExitStack() as es:
                    def sb(name, shape, dt):
                        return es.enter_context(nc.sbuf_tensor(un(name), list(shape), dt))
                    P = alloc_psum(es)
                    zT = sb("cv_zT", [128, 8, 2304], BF16)
                    with ExitStack() as es1:
                        def sb1(name, shape, dt):
                            return es1.enter_context(nc.sbuf_tensor(un(name), list(shape), dt))
                        hT = sb1("cv_hT", [128, 8, 2304], BF16)
                        scal = sb1("cv_scal", [128, 2, 8], F32)
                        cw = sb1("cv_cw", [128, 3, 8], F32)
                        S.dma("sp", lambda e: e.dma_start(out=cw[:], in_=conv_w[j].rearrange("k (c p) -> p k c", p=128), allow_slow_non_contiguous=True),
                              "cw", writes=["cw"])
                        wstg = Rot(es1, "cv_wst", 2, [128, 8, 3, 128], F32)
                        wbf = Rot(es1, "cv_wbf", 2, [128, 8, 3, 128], BF16)
                        pbuf = sb1("cv_p", [128, 2308], F32)
                        gbuf = sb1("cv_g", [128, 2304], F32)
                        ubuf = Rot(es1, "cv_u", 2, [128, 512], F32)
                        acc = sb1("cv_acc", [128, 2304], F32)
                        S.op("pool", lambda e: e.memset(pbuf[:], 0.0), writes=["pbuf"])
                        make_hT(es1, P, i, b, hT, scal)
                        pbanks = [(P.pA, "pA"), (P.pB, "pB"), (P.pC, "pC")]
                        for c in range(8):
                            wst, wst_id = wstg.next()
                            wb, wb_id = wbf.next()
                            for k3 in range(3):
                                S.dma("pool", lambda e, wb=wb, k3=k3, c=c: e.dma_start(out=wb[:, :, k3, :], in_=win[:, :, k3 * D + c * 128:k3 * D + (c + 1) * 128]),
                                      "ld" + wb_id, writes=[wb_id])
                            for (t0, tn) in TOKG:
                                poff = 1 + t0 if t0 < SEQ else 2051
                                for k3 in range(3):
                                    pp, pp_id = pbanks[k3]
                                    for kk in range(8):
                                        S.op("pe", lambda e, pp=pp, wb=wb, k3=k3, kk=kk, t0=t0, tn=tn: e.matmul(
                                            pp[:, 0:tn], lhsT=wb[:, kk, k3, :], rhs=hT[:, kk, t0:t0 + tn], start=(kk == 0), stop=(kk == 7)),
                                            reads=[wb_id, "hT"], writes=[pp_id])
                                ub, ub_id = ubuf.next()
                                S.op("act", lambda e, ub=ub, tn=tn: e.copy(out=ub[:, 0:tn], in_=P.pC[:, 0:tn]), reads=["pC"], writes=[ub_id])
                                S.op("act", lambda e, t0=t0, tn=tn: e.copy(out=gbuf[:, t0:t0 + tn], in_=P.pA[:, 0:tn]), reads=["pA"], writes=["gbuf"])
                                S.op("dve", lambda e, ub=ub, tn=tn, poff=poff: e.tensor_tensor(out=pbuf[:, poff:poff + tn], in0=P.pB[:, 0:tn], in1=ub[:, 0:tn], op=ALU.mult),
                                     reads=["pB", ub_id], writes=["pbuf"])
                            for (o0, p0, n) in ((0, 1, SEQ), (SEQ, 2051, LCTX)):
                                S.op("dve", lambda e, c=c, o0=o0, p0=p0, n=n: e.tensor_scalar(out=acc[:, o0:o0 + n], in0=pbuf[:, p0:p0 + n], scalar1=cw[:, 1, c:c + 1],
                                                                                             scalar2=None, op0=ALU.mult), reads=["pbuf", "cw"], writes=["acc"])
                                S.op("dve", lambda e, c=c, o0=o0, p0=p0, n=n: e.scalar_tensor_tensor(out=acc[:, o0:o0 + n], in0=pbuf[:, p0 - 1:p0 - 1 + n], scalar=cw[:, 0, c:c + 1],
                                                                                                      in1=acc[:, o0:o0 + n], op0=ALU.mult, op1=ALU.add),
                                     reads=["pbuf", "cw", "acc"], writes=["acc"])
                                S.op("dve", lambda e, c=c, o0=o0, p0=p0, n=n: e.scalar_tensor_tensor(out=acc[:, o0:o0 + n], in0=pbuf[:, p0 + 1:p0 + 1 + n], scalar=cw[:, 2, c:c + 1],
                                                                                                     in1=acc[:, o0:o0 + n], op0=ALU.mult, op1=ALU.add),
                                     reads=["pbuf", "cw", "acc"], writes=["acc"])
                            S.op("pool", lambda e, c=c: e.tensor_tensor(out=zT[:, c, :], in0=acc[:], in1=gbuf[:], op=ALU.mult),
                                 reads=["acc", "gbuf"], writes=["zT"])
                        S.flush()
                    with ExitStack() as es2:
                        def sb2(name, shape, dt):
                            return es2.enter_context(nc.sbuf_tensor(un(name), list(shape), dt))
                        M = mixer_common_alloc(es2, i)
                        wob = sb2("cv_wob", [128, 8, D], BF16)
                        wstg = Rot(es2, "cv_wst2_", 2, [128, 8, 512], F32)
                        wo = conv_w_out[j].rearrange("(c p) n -> p c n", p=128)
                        for g in range(2):
                            wst, wst_id = wstg.next()
                            S.dma("pool", lambda e, g=g: e.dma_start(out=wob[:, :, g * 512:(g + 1) * 512], in_=wo[:, :, g * 512:(g + 1) * 512]), "ldwob", writes=["wob"])
                        qtiles = list(range(16)) + ([] if last else [16, 17])
                        po = P.ptf[:].rearrange("p c t -> p (c t)")
                        for jq in qtiles:
                            tt = b * TPB + jq
                            load_variant(M, i, b if jq < 16 else 2)
                            for hf in range(2):
                                for c in range(8):
                                    S.op("pe", lambda e, c=c, hf=hf, jq=jq: e.matmul(po[:, hf * 512:(hf + 1) * 512], lhsT=zT[:, c, jq * 128:(jq + 1) * 128],
                                                                                     rhs=wob[:, c, hf * 512:(hf + 1) * 512], start=(c == 0), stop=(c == 7)),
                                         reads=["zT", "wob"], writes=["ptf"])
                            out_stage(M, P, i, tt, po, ["ptf"])
                        S.flush()

        def phase_experts(i):
            NTB = CAP // 128
            NNT = CAP // 512
            with ExitStack() as es:
                def ps(name, shape, dt):
                    return es.enter_context(nc.psum_tensor(un(name), list(shape), dt))
                wg = Rot(es, "ex_wg", 3, [128, 8, 512], BF16)
                wu = Rot(es, "ex_wu", 3, [128, 8, 512], BF16)
                wd = Rot(es, "ex_wd", 3, [128, 4, D], BF16)
                xs = Rot(es, "ex_xs", 3, [128, NTB, D], BF16)
                xsT = Rot(es, "ex_xsT", 2, [128, 8, CAP], BF16)
                sg = Rot(es, "ex_sg", 2, [128, 512], F32)
                aT = Rot(es, "ex_aT", 2, [128, 4, CAP], BF16)
                ys = Rot(es, "ex_ys", 2, [128, D], F32)
                ptb = [(ps("ex_ptb%d" % k, [128, 8, 128], BF16), "ptb%d" % k) for k in range(2)]
                pg = [(ps("ex_pg%d" % k, [128, 512], F32), "pg%d" % k) for k in range(2)]
                pu = [(ps("ex_pu%d" % k, [128, 512], F32), "pu%d" % k) for k in range(2)]
                py = ps("ex_py", [128, D], F32)
                st8 = {"nptb": 0, "npg": 0}

                def prep(ex):
                    wgb, wg_id = wg.next()
                    wub, wu_id = wu.next()
                    wdb, wd_id = wd.next()
                    xsb, xs_id = xs.next()
                    for (src, dstb, dst_id) in ((w_gate[i, ex].rearrange("(c p) n -> p c n", p=128), wgb, wg_id),
                                                (w_up[i, ex].rearrange("(c p) n -> p c n", p=128), wub, wu_id),
                                                (w_down[i, ex].rearrange("(c p) n -> p c n", p=128), wdb, wd_id)):
                        S.dma("pool", lambda e, dstb=dstb, src=src: e.dma_start(out=dstb[:], in_=src), "ld" + dst_id, writes=[dst_id])
                    for q in range(CAP // 256):
                        S.cur_group = (ex, q * 256 + 1) if q > 0 else None
                        S.dma("sp", lambda e, xsb=xsb, ex=ex, q=q: e.dma_start(
                            out=xsb[:, 2 * q:2 * q + 2, :], in_=XS[ex * CAP + q * 256:ex * CAP + (q + 1) * 256, :].rearrange("(j p) d -> p j d", p=128)),
                            xs_id + "q%d" % q, reads=["XS"], writes=[xs_id + "q%d" % q])
                    S.cur_group = None
                    return dict(wgb=wgb, wg_id=wg_id, wub=wub, wu_id=wu_id, wdb=wdb, wd_id=wd_id, xsb=xsb, xs_id=xs_id)

                def late_cast(pr):
                    return

                def compute(ex, pr):
                    wgb, wg_id, wub, wu_id, wdb, wd_id, xsb, xs_id = (pr[k] for k in ("wgb", "wg_id", "wub", "wu_id", "wdb", "wd_id", "xsb", "xs_id"))
                    xTb, xT_id = xsT.next()
                    aTb, aT_id = aT.next()
                    QR = 256
                    for q in range(CAP // QR):
                        S.cur_group = (ex, q * QR + 1) if q > 0 else None
                        c0 = q * QR
                        for jt in range(2 * q, 2 * q + 2):
                            pt, pt_id = ptb[st8["nptb"] % 2]
                            st8["nptb"] += 1
                            for c in range(8):
                                S.op("pe", lambda e, pt=pt, jt=jt, c=c: e.transpose(out=pt[:, c, :], in_=xsb[:, jt, c * 128:(c + 1) * 128], identity=identb[:]),
                                     reads=[xs_id + "q%d" % q, "identb"], writes=[pt_id])
                            S.op("act", lambda e, pt=pt, jt=jt: e.copy(out=xTb[:, :, jt * 128:(jt + 1) * 128], in_=pt[:]), reads=[pt_id], writes=[xT_id])
                        for m_ in range(4):
                            pgb, pg_id = pg[st8["npg"] % 2]
                            pub, pu_id = pu[st8["npg"] % 2]
                            st8["npg"] += 1
                            for c in range(8):
                                S.op("pe", lambda e, pgb=pgb, c=c, m_=m_, c0=c0: e.matmul(pgb[:, 0:QR], lhsT=wgb[:, c, m_ * 128:(m_ + 1) * 128], rhs=xTb[:, c, c0:c0 + QR],
                                                                                          start=(c == 0), stop=(c == 7)), reads=[wg_id, xT_id], writes=[pg_id])
                            for c in range(8):
                                S.op("pe", lambda e, pub=pub, c=c, m_=m_, c0=c0: e.matmul(pub[:, 0:QR], lhsT=wub[:, c, m_ * 128:(m_ + 1) * 128], rhs=xTb[:, c, c0:c0 + QR],
                                                                                          start=(c == 0), stop=(c == 7)), reads=[wu_id, xT_id], writes=[pu_id])
                            sgb, sg_id = sg.next()
                            S.op("act", lambda e, sgb=sgb, pgb=pgb: e.activation(out=sgb[:, 0:QR], in_=pgb[:, 0:QR], func=AF.Silu), reads=[pg_id], writes=[sg_id])
                            S.op("dve", lambda e, sgb=sgb, pub=pub, m_=m_, c0=c0: e.tensor_tensor(out=aTb[:, m_, c0:c0 + QR], in0=pub[:, 0:QR], in1=sgb[:, 0:QR], op=ALU.mult),
                                 reads=[pu_id, sg_id], writes=[aT_id])
                        for jt in range(2 * q, 2 * q + 2):
                            for hf in range(2):
                                for m_ in range(4):
                                    S.op("pe", lambda e, jt=jt, hf=hf, m_=m_: e.matmul(py[:, hf * 512:(hf + 1) * 512], lhsT=aTb[:, m_, jt * 128:(jt + 1) * 128],
                                                                                       rhs=wdb[:, m_, hf * 512:(hf + 1) * 512], start=(m_ == 0), stop=(m_ == 3)),
                                         reads=[aT_id, wd_id], writes=["py"])
                            ysb, ys_id = ys.next()
                            if jt % 2 == 0:
                                S.op("act", lambda e, ysb=ysb: e.copy(out=ysb[:], in_=py[:]), reads=["py"], writes=[ys_id])
                            else:
                                S.op("dve", lambda e, ysb=ysb: e.tensor_copy(out=ysb[:], in_=py[:]), reads=["py"], writes=[ys_id])
                            r0 = ex * CAP + jt * 128
                            S.dma("sp", lambda e, ysb=ysb, r0=r0: e.dma_start(out=YS[r0:r0 + 128, :], in_=ysb[:]), "st" + ys_id, reads=[ys_id], writes=["YS"])
                    S.cur_group = None

                preps = {0: prep(0), 1: prep(1)}
                for ex in range(NEXP):
                    if ex + 2 < NEXP:
                        preps[ex + 2] = prep(ex + 2)
                    compute(ex, preps.pop(ex))
                S.flush()

        def phase_combine(i, last, ada_next=None):
            with ExitStack() as es:
                def sb(name, shape, dt):
                    return es.enter_context(nc.sbuf_tensor(un(name), list(shape), dt))
                G2 = sb("cb_G2", [128, D], F32)
                lg = sb("cb_lg", [128, D], F32)
                lb = sb("cb_lb", [128, D], F32)
                y0 = Rot(es, "cb_y0", 2, [128, D], F32)
                y1 = Rot(es, "cb_y1", 2, [128, D], F32)
                xm = Rot(es, "cb_xm", 2, [128, D], F32)
                tb = Rot(es, "cb_t", 2, [128, D], F32)
                xn = Rot(es, "cb_xn", 2, [128, D], F32)
                xo = Rot(es, "cb_xo", 2, [128, D], F32)
                st = Rot(es, "cb_st", 2, [128, 2, 6], F32)
                mv = Rot(es, "cb_mv", 2, [128, 2], F32)
                rs = Rot(es, "cb_rs", 2, [128, 2], F32)
                S.dma("sp", lambda e: e.dma_start(out=lg[:], in_=ln2_g[i, :].partition_broadcast(128)), "clg", writes=["lg"])
                S.dma("sp", lambda e: e.dma_start(out=lb[:], in_=ln2_b[i, :].partition_broadcast(128)), "clb", writes=["lb"])
                cvar = {"v": None}

                def one_tile(tt):
                    b, j = divmod(tt, TPB)
                    v = b if j < 16 else 2
                    if v != cvar["v"]:
                        cvar["v"] = v
                        S.dma("sp", lambda e, v=v: e.dma_start(out=G2[:], in_=MODR[i, v, 5, :].partition_broadcast(128)), "cG2", writes=["G2"])
                    y0b, y0_id = y0.next()
                    y1b, y1_id = y1.next()
                    xmb, xm_id = xm.next()
                    t, t_id = tb.next()
                    xnb, xn_id = xn.next()
                    xob, xo_id = xo.next()
                    for k, (yb, y_id) in enumerate(((y0b, y0_id), (y1b, y1_id))):
                        S.dma("pool", lambda e, yb=yb, k=k, tt=tt: e.indirect_dma_start(
                            out=yb[:], out_offset=None, in_=YS, in_offset=bass.IndirectOffsetOnAxis(ap=destI[:, tt, k:k + 1], axis=0),
                            bounds_check=breg(e), oob_is_err=False), y_id, reads=["YS"], writes=[y_id])
                    S.dma("sp", lambda e, xmb=xmb, tt=tt: e.dma_start(out=xmb[:], in_=X[tt * 128:(tt + 1) * 128, :]), xm_id, reads=["X%d" % tt], writes=[xm_id])
                    S.op("act", lambda e, t=t, y0b=y0b, tt=tt: e.activation(out=t[:], in_=y0b[:], func=AF.Copy, scale=gates[:, tt, 0:1]),
                         reads=[y0_id], writes=[t_id])
                    S.op("dve", lambda e, t=t, y1b=y1b, tt=tt: e.scalar_tensor_tensor(out=t[:], in0=y1b[:], scalar=gates[:, tt, 1:2], in1=t[:], op0=ALU.mult, op1=ALU.add),
                         reads=[y1_id, t_id], writes=[t_id])
                    S.op("dve", lambda e, t=t: e.tensor_tensor(out=t[:], in0=t[:], in1=G2[:], op=ALU.mult), reads=[t_id, "G2"], writes=[t_id])
                    S.op("dve", lambda e, t=t, xmb=xmb: e.tensor_tensor(out=t[:], in0=t[:], in1=xmb[:], op=ALU.add), reads=[t_id, xm_id], writes=[t_id])
                    layer_norm_core((st, mv, rs), t, t_id, xnb, xn_id)
                    S.op("dve", lambda e, xob=xob, xnb=xnb: e.tensor_tensor(out=xob[:], in0=xnb[:], in1=lg[:], op=ALU.mult), reads=[xn_id, "lg"], writes=[xo_id])
                    S.op("dve", lambda e, xob=xob: e.tensor_tensor(out=xob[:], in0=xob[:], in1=lb[:], op=ALU.add), reads=[xo_id, "lb"], writes=[xo_id])
                    if last:
                        dst = out[b, j * 128:(j + 1) * 128, :]
                    else:
                        dst = X[tt * 128:(tt + 1) * 128, :]
                    S.dma("sp", lambda e, xob=xob, dst=dst: e.dma_start(out=dst, in_=xob[:]), "st" + xo_id, reads=[xo_id], writes=["X%d" % tt])

                tiles = [tt for tt in range(NT) if not (last and tt % TPB >= 16)]
                ada_list = []
                if ada_next is not None:
                    ada_list, _ = S.capture(phase_ada, ada_next, es, False)
                ada_per = -(-len(ada_list) // max(1, len(tiles) // 2))
                ada_pos = 0
                k = 0
                while k < len(tiles):
                    ta = tiles[k]
                    va_ = (ta // TPB) if ta % TPB < 16 else 2
                    tile_b = tiles[k + 1] if k + 1 < len(tiles) else None
                    vb_ = None if tile_b is None else ((tile_b // TPB) if tile_b % TPB < 16 else 2)
                    la, _ = S.capture(one_tile, ta)
                    if tile_b is not None and vb_ == va_:
                        lb_, _ = S.capture(one_tile, tile_b)
                        merged = []
                        for x in range(max(len(la), len(lb_))):
                            if x < len(la):
                                merged.append(la[x])
                            if x < len(lb_):
                                merged.append(lb_[x])
                        S.replay(merged)
                        k += 2
                    else:
                        S.replay(la)
                        k += 1
                    S.replay(ada_list[ada_pos:ada_pos + ada_per])
                    ada_pos += ada_per
                S.replay(ada_list[ada_pos:])
                S.flush()

        phase_init()
        phase_nat_table()
        phase_ada(0)
        for i in range(DEPTH):
            last = i == DEPTH - 1
            kind = i % 3
            j = i // 3
            S.op("pool", lambda e: e.memset(cnt[:], 0.0), writes=["cnt"])
            if kind == 0:
                phase_attn(i, j, 0, last)
            elif kind == 1:
                phase_conv(i, j, last)
            else:
                phase_attn(i, j, 2, last)
            S.op("dve", lambda e: e.tensor_copy(out=cnti[:], in_=cnt[:]), reads=["cnt"], writes=["cnti"])
            S.flush()
            phase_experts(i)
            phase_combine(i, last, None if last else i + 1)
        if debug:
            S.limit = None
            dbgX = nc.dram_tensor("dbgX", [NT * 128, D], F32, kind="ExternalOutput").ap()
            dbgM = nc.dram_tensor("dbgM", [DEPTH, 3, 6, D], F32, kind="ExternalOutput").ap()
            dbgXS = nc.dram_tensor("dbgXS", [NSLOT, D], BF16, kind="ExternalOutput").ap()
            dbgYS = nc.dram_tensor("dbgYS", [NSLOT, D], F32, kind="ExternalOutput").ap()
            dbgQ = nc.dram_tensor("dbgQ", [128, 8, 2304], BF16, kind="ExternalOutput").ap()
            dbgD = nc.dram_tensor("dbgD", [128, NT, 2], I32, kind="ExternalOutput").ap()
            dbgG = nc.dram_tensor("dbgG", [128, NT, 2], F32, kind="ExternalOutput").ap()
            for r0 in range(0, NT * 128, 512):
                S.dma("sp", lambda e, r0=r0: e.dma_start(out=dbgX[r0:r0 + 512, :], in_=X[r0:r0 + 512, :]), "d0")
            S.dma("sp", lambda e: e.dma_start(out=dbgM, in_=MODR), "d1")
            for r0 in range(0, NSLOT, 512):
                S.dma("sp", lambda e, r0=r0: e.dma_start(out=dbgXS[r0:r0 + 512, :], in_=XS[r0:r0 + 512, :]), "d2")
                S.dma("sp", lambda e, r0=r0: e.dma_start(out=dbgYS[r0:r0 + 512, :], in_=YS[r0:r0 + 512, :]), "d3")
            S.dma("sp", lambda e: e.dma_start(out=dbgQ, in_=QT), "d4")
            S.dma("sp", lambda e: e.dma_start(out=dbgD, in_=destI[:]), "d5")
            S.dma("sp", lambda e: e.dma_start(out=dbgG, in_=gates[:]), "d6")
            S.flush()
    return nc


_NC_CACHE = {}


def _host_constants(inputs):
    cosT, sinT, Pm = _rope_tables()
    pats, chunks, dr, dc, mk, npat = _NATP
    rpb = np.asarray(inputs["nat_rpb"], np.float32)[0]
    nat_bias = np.ascontiguousarray(np.transpose(rpb[:, dr, dc], (1, 2, 0, 3))).astype(np.float32)
    aw = np.asarray(inputs["attn_w_qkv"], np.float32)
    kcols = []
    for g in range(4):
        kcols += list(range(1024 + g * 64, 1024 + (g + 1) * 64)) * 2
    attn_wqk = np.ascontiguousarray(np.concatenate([aw[:, :, :1024], aw[:, :, kcols]], axis=2))
    attn_wv = np.ascontiguousarray(aw[:, :, 1280:1536])
    nw = np.asarray(inputs["nat_w_qkv"], np.float32)
    router_w = np.ascontiguousarray(np.concatenate([inputs["router_w_group"], inputs["router_w_expert"]], axis=2)).astype(np.float32)
    router_b = np.ascontiguousarray(np.concatenate([inputs["router_b_group"], inputs["router_b_expert"]], axis=1)).astype(np.float32)
    kk = np.arange(128)
    consts = {
        "attn_wqk": attn_wqk, "attn_wv": attn_wv,
        "nat_wqk": np.ascontiguousarray(nw[:, :, :2048]), "nat_wv": np.ascontiguousarray(nw[:, :, 2048:]),
        "nat_bias": nat_bias, "router_w": router_w, "router_b": router_b,
        "k_ident": np.eye(128, dtype=np.float32), "k_cos": cosT, "k_sin": sinT, "k_pm": Pm,
        "k_band": _band_masks(), "k_natmask": mk,
        "k_ustrict": (kk[:, None] < kk[None, :]).astype(np.float32),
        "k_eoff": (np.arange(32, dtype=np.float32) * CAP)[None, :],
    }
    return consts


def kernel(**inputs):
    if "nc" not in _NC_CACHE:
        _NC_CACHE["nc"] = build_program()
    nc = _NC_CACHE["nc"]
    consts = _host_constants(inputs)
    shared = {}
    for k in ("w_ada", "b_ada", "ln1_g", "ln1_b", "ln2_g", "ln2_b", "attn_w_o", "attn_sink", "conv_w_in", "conv_w",
              "conv_w_out", "nat_w_o", "expert_w_gate", "expert_w_up", "expert_w_down"):
        shared[k] = np.ascontiguousarray(np.asarray(inputs[k], np.float32))
    shared.update(consts)
    shared["c_ctx"] = np.ascontiguousarray(np.asarray(inputs["c_ctx"], np.float32).reshape(1, D))
    x = np.asarray(inputs["x"], np.float32)
    c = np.asarray(inputs["c"], np.float32)
    ctx = np.asarray(inputs["ctx"], np.float32)
    in_maps = []
    for core in range(NCORES):
        m = dict(shared)
        m["x"] = np.ascontiguousarray(x[core * NB:(core + 1) * NB])
        m["c"] = np.ascontiguousarray(c[core * NB:(core + 1) * NB])
        m["ctx"] = np.ascontiguousarray(ctx[core * NB:(core + 1) * NB])
        in_maps.append(m)
    res = run_bass_kernel_spmd(nc, in_maps, core_ids=list(range(NCORES)))
    return np.concatenate([r["out"] for r in res.results], axis=0).astype(np.float32)
```
